# Optimizing a Trainium2 kernel written in Bass

```python
import jax, jax.numpy as jnp
from jax import lax
import numpy as np

D_MODEL = 2048
BATCH = 4
SEQ = 2048
DEPTH = 1

CONV_CH = D_MODEL // 2
CONV_K = 3
RET_HEADS = 8
RET_DK = D_MODEL // (2 * RET_HEADS)
RET_DV = 2 * RET_DK
RET_QK = RET_HEADS * RET_DK
RET_V = RET_HEADS * RET_DV
CHUNK = 128
ROPE_BASE = 10000.0
N_GROUPS = 4
EXPERTS_PER_GROUP = 8
N_EXPERTS = N_GROUPS * EXPERTS_PER_GROUP
TOP_K = 2
D_EXPERT = D_MODEL // 2
EXPERT_BLOCK = 128
EPS = 1e-6
IN_COLS = 3 * CONV_CH + 2 * RET_QK + 2 * RET_V + 2 * D_MODEL

kernel_name = "hybrid_conv_retention_hmoe_adaln"


def rmsnorm(x, g):
    xf = x.astype(jnp.float32)
    xf = xf * lax.rsqrt(jnp.mean(xf * xf, axis=-1, keepdims=True) + EPS)
    return (xf * g.astype(jnp.float32)).astype(x.dtype)


def modulate(h, shift, scale):
    return h * (1 + scale[:, None, :]) + shift[:, None, :]


def causal_dwconv(z, w):
    S = z.shape[1]
    zp = jnp.pad(z, ((0, 0), (CONV_K - 1, 0), (0, 0)))
    out = zp[:, 0:S] * w[0]
    for tap in range(1, CONV_K):
        out = out + zp[:, tap:tap + S] * w[tap]
    return out


def rotary(t):
    S, d = t.shape[-2], t.shape[-1]
    inv = ROPE_BASE ** (-jnp.arange(0, d, 2, dtype=jnp.float32) / d)
    ang = jnp.arange(S, dtype=jnp.float32)[:, None] * inv[None, :]
    cos, sin = jnp.cos(ang).astype(t.dtype), jnp.sin(ang).astype(t.dtype)
    t1, t2 = t[..., : d // 2], t[..., d // 2:]
    return jnp.concatenate([t1 * cos - t2 * sin, t1 * sin + t2 * cos], axis=-1)


def retention_chunkwise(q, k, v):
    B, H, S, dk = q.shape
    dv = v.shape[-1]
    N = S // CHUNK
    dt = q.dtype
    log_g = jnp.log1p(-(2.0 ** (-5.0 - jnp.arange(H, dtype=jnp.float32))))
    i = jnp.arange(CHUNK, dtype=jnp.float32)
    diff = i[:, None] - i[None, :]
    inner_decay = jnp.where(diff >= 0, jnp.exp(log_g[:, None, None] * jnp.maximum(diff, 0.0)), 0.0).astype(dt)
    q_decay = jnp.exp(log_g[:, None] * (i + 1.0)).astype(dt)
    k_decay = jnp.exp(log_g[:, None] * (CHUNK - 1.0 - i)).astype(dt)
    chunk_decay = jnp.exp(log_g * CHUNK).astype(dt)[:, None, None]
    qc = q.reshape(B, H, N, CHUNK, dk)
    kc = k.reshape(B, H, N, CHUNK, dk)
    vc = v.reshape(B, H, N, CHUNK, dv)
    scores = jnp.einsum('bhnid,bhnjd->bhnij', qc, kc) * inner_decay[None, :, None]
    inner = jnp.einsum('bhnij,bhnje->bhnie', scores, vc)
    kv = jnp.einsum('bhnjd,hj,bhnje->nbhde', kc, k_decay, vc)

    def step(state, kv_n):
        return state * chunk_decay + kv_n, state

    _, s_prev = lax.scan(step, jnp.zeros((B, H, dk, dv), kv.dtype), kv)
    cross = jnp.einsum('bhnid,hi,nbhde->bhnie', qc, q_decay, s_prev)
    return (inner + cross).reshape(B, H, S, dv)


def hier_moe(h, w_router_group, b_router_group, w_router_expert, b_router_expert, w_gate, w_up, w_down):
    T, D = h.shape
    g_logits = (h @ w_router_group).astype(jnp.float32) + b_router_group.astype(jnp.float32)
    g_prob = jax.nn.softmax(g_logits, axis=-1)
    g_sel = jnp.argmax(g_logits, axis=-1)
    tok = jnp.arange(T)
    g_w = g_prob[tok, g_sel][:, None]
    e_logits = ((h @ w_router_expert).astype(jnp.float32) + b_router_expert.astype(jnp.float32)).reshape(T, N_GROUPS, EXPERTS_PER_GROUP)
    e_prob = jax.nn.softmax(e_logits[tok, g_sel], axis=-1)
    top_p, top_i = lax.top_k(e_prob, TOP_K)
    top_p = top_p / jnp.sum(top_p, axis=-1, keepdims=True)
    combine = g_w * top_p
    expert_id = g_sel[:, None] * EXPERTS_PER_GROUP + top_i

    A = T * TOP_K
    flat_e = expert_id.reshape(A)
    flat_tok = jnp.repeat(tok, TOP_K)
    flat_w = combine.reshape(A)
    order = jnp.argsort(flat_e)
    se, stok, sw = flat_e[order], flat_tok[order], flat_w[order]
    counts = jnp.bincount(flat_e, length=N_EXPERTS)
    start = jnp.cumsum(counts) - counts
    padded = (counts + EXPERT_BLOCK - 1) // EXPERT_BLOCK * EXPERT_BLOCK
    pend = jnp.cumsum(padded)
    pstart = pend - padded
    dest = pstart[se] + (jnp.arange(A) - start[se])
    R = A + N_EXPERTS * EXPERT_BLOCK
    n_blk = R // EXPERT_BLOCK
    x_buf = jnp.zeros((R, D), h.dtype).at[dest].set(h[stok])
    tok_buf = jnp.zeros((R,), jnp.int32).at[dest].set(stok.astype(jnp.int32))
    w_buf = jnp.zeros((R,), h.dtype).at[dest].set(sw.astype(h.dtype))
    blk_e = jnp.minimum(jnp.searchsorted(pend, jnp.arange(n_blk) * EXPERT_BLOCK, side='right'), N_EXPERTS - 1)

    def run_block(args):
        xb, e = args
        return (jax.nn.silu(xb @ w_gate[e]) * (xb @ w_up[e])) @ w_down[e]

    y_buf = lax.map(run_block, (x_buf.reshape(n_blk, EXPERT_BLOCK, D), blk_e)).reshape(R, D)
    return jnp.zeros((T, D), h.dtype).at[tok_buf].add(y_buf * w_buf[:, None])


def setup_inputs(seed: int = 0) -> dict:
    key = jax.random.key(seed)
    ks = jax.random.split(key, 20)
    D, L = D_MODEL, DEPTH
    nrm = lambda k, shape, fan_in, s=1.0: (jax.random.normal(k, shape, jnp.float32) * (s * fan_in ** -0.5))
    return {
        "x": jax.random.normal(ks[0], (BATCH, SEQ, D), jnp.float32),
        "c": jax.random.normal(ks[1], (BATCH, D), jnp.float32),
        "w_ada": nrm(ks[2], (L, D, 6 * D), D, 0.5),
        "b_ada": 0.01 * jax.random.normal(ks[3], (L, 6 * D), jnp.float32),
        "norm1_g": 1.0 + 0.05 * jax.random.normal(ks[4], (L, D), jnp.float32),
        "w_in": nrm(ks[5], (L, D, IN_COLS), D),
        "conv_w": nrm(ks[6], (L, CONV_K, CONV_CH), CONV_K),
        "w_conv_out": nrm(ks[7], (L, CONV_CH, D), CONV_CH),
        "w_ret_out": nrm(ks[8], (L, RET_V, D), RET_V),
        "w_o": nrm(ks[9], (L, D, D), D),
        "norm2_g": 1.0 + 0.05 * jax.random.normal(ks[10], (L, D), jnp.float32),
        "w_router_group": nrm(ks[11], (L, D, N_GROUPS), D),
        "b_router_group": 0.01 * jax.random.normal(ks[12], (L, N_GROUPS), jnp.float32),
        "w_router_expert": nrm(ks[13], (L, D, N_EXPERTS), D),
        "b_router_expert": 0.01 * jax.random.normal(ks[14], (L, N_EXPERTS), jnp.float32),
        "w_gate": nrm(ks[15], (L, N_EXPERTS, D, D_EXPERT), D),
        "w_up": nrm(ks[16], (L, N_EXPERTS, D, D_EXPERT), D),
        "w_down": nrm(ks[17], (L, N_EXPERTS, D_EXPERT, D), D_EXPERT),
        "norm_f_g": 1.0 + 0.05 * jax.random.normal(ks[18], (D,), jnp.float32),
    }


def reference(x, c, w_ada, b_ada, norm1_g, w_in, conv_w, w_conv_out, w_ret_out, w_o, norm2_g,
              w_router_group, b_router_group, w_router_expert, b_router_expert,
              w_gate, w_up, w_down, norm_f_g):
    B, S, D = x.shape
    sizes = (CONV_CH, CONV_CH, CONV_CH, RET_QK, RET_QK, RET_V, RET_V, D_MODEL, D_MODEL)
    split_at = tuple(int(s) for s in np.cumsum(sizes)[:-1])
    c_act = jax.nn.silu(c)
    for l in range(DEPTH):
        mod = c_act @ w_ada[l] + b_ada[l]
        sh1, sc1, g1, sh2, sc2, g2 = jnp.split(mod, 6, axis=-1)

        h = modulate(rmsnorm(x, norm1_g[l]), sh1, sc1)
        u = h @ w_in[l]
        cb, cc, cx, q, k, v, rg, gate_a, gate_b = jnp.split(u, split_at, axis=-1)

        y_a = (cb * causal_dwconv(cc * cx, conv_w[l])) @ w_conv_out[l]

        qh = rotary(q.reshape(B, S, RET_HEADS, RET_DK).transpose(0, 2, 1, 3))
        kh = rotary(k.reshape(B, S, RET_HEADS, RET_DK).transpose(0, 2, 1, 3)) * (RET_DK ** -0.5)
        vh = v.reshape(B, S, RET_HEADS, RET_DV).transpose(0, 2, 1, 3)
        ret = retention_chunkwise(qh, kh, vh)
        rf = ret.astype(jnp.float32)
        rf = rf * lax.rsqrt(jnp.mean(rf * rf, axis=-1, keepdims=True) + EPS)
        ret = rf.astype(x.dtype).transpose(0, 2, 1, 3).reshape(B, S, RET_V)
        y_b = (jax.nn.silu(rg) * ret) @ w_ret_out[l]

        merged = jax.nn.sigmoid(gate_a) * y_a + jax.nn.sigmoid(gate_b) * y_b
        x = x + g1[:, None, :] * (merged @ w_o[l])

        h2 = modulate(rmsnorm(x, norm2_g[l]), sh2, sc2).reshape(B * S, D)
        y = hier_moe(h2, w_router_group[l], b_router_group[l], w_router_expert[l], b_router_expert[l],
                     w_gate[l], w_up[l], w_down[l]).reshape(B, S, D)
        x = x + g2[:, None, :] * y
    return rmsnorm(x, norm_f_g)
```

```python
import os
from contextlib import ExitStack
import numpy as np
import concourse.bass as bass
import concourse.mybir as mybir
from concourse.bass_utils import run_bass_kernel_spmd

F32 = mybir.dt.float32
BF16 = mybir.dt.bfloat16
I32 = mybir.dt.int32
ALU = mybir.AluOpType
AF = mybir.ActivationFunctionType
AX = mybir.AxisListType

D = 2048
NCORES = 8
TOK = 1024
NT = 8
NCH = 16
EPS = 1e-6
NEXP = 32
ENGS = ("pe", "act", "dve", "pool", "sp")


class Buf:
    __slots__ = ("name", "writer", "readers", "psum")

    def __init__(self, name):
        self.name = name
        self.writer = None
        self.readers = []
        self.psum = False


class Op:
    __slots__ = ("eng", "fn", "deps", "signal", "token", "is_dma")

    def __init__(self, eng, fn, is_dma):
        self.eng = eng
        self.fn = fn
        self.deps = []
        self.signal = False
        self.token = None
        self.is_dma = is_dma


class Prog:
    def __init__(self, nc):
        self.nc = nc
        self.streams = {e: [] for e in ENGS}
        self.dma_sems = {}
        self.last_dma = {}
        self.wait_all = set()

    def buf(self, name):
        return Buf(name)

    def bufs(self, name, n):
        return [Buf(f"{name}{i}") for i in range(n)]

    def _add(self, op, reads, writes):
        deps = []
        for b in reads:
            if b.writer is not None:
                deps.append(b.writer)
            if b.psum:
                deps.extend(r for r in b.readers if r.eng != op.eng)
        for b in writes:
            if b.writer is not None:
                deps.append(b.writer)
            deps.extend(b.readers)
        seen = set()
        for d in deps:
            if d is op or id(d) in seen:
                continue
            seen.add(id(d))
            if d.eng == "pe" and op.eng == "pe" and not d.is_dma and not op.is_dma:
                continue
            if d.is_dma:
                sname = d.token[0]
                cur = self.dma_sems[sname][1]
                if op.is_dma and op.token[0] == sname:
                    cur -= 16
                op.deps.append((sname, cur))
            else:
                op.deps.append(d)
                d.signal = True
        for b in reads:
            b.readers.append(op)
        for b in writes:
            b.writer = op
            b.readers = []
        self.streams[op.eng].append(op)
        return op

    def op(self, eng, fn, reads=(), writes=()):
        return self._add(Op(eng, fn, False), reads, writes)

    def dma(self, eng, fn, sem, reads=(), writes=()):
        op = Op(eng, fn, True)
        ent = self.dma_sems.setdefault(sem, [None, 0])
        ent[1] += 16
        op.token = (sem, ent[1])
        self.last_dma[sem] = op
        return self._add(op, reads, writes)

    def barrier(self):
        lasts = []
        for e in ENGS:
            for op in reversed(self.streams[e]):
                if not op.is_dma and op.fn is not None:
                    lasts.append(op)
                    break
        lasts.extend(self.last_dma.values())
        for e in ENGS:
            op = Op(e, None, False)
            for d in lasts:
                if d.is_dma:
                    op.deps.append((d.token[0], self.dma_sems[d.token[0]][1]))
                    continue
                if d.eng == e:
                    continue
                op.deps.append(d)
                d.signal = True
            self.streams[e].append(op)

    def _tok(self, d):
        if isinstance(d, tuple):
            s_, v = d
            if s_ in self.wait_all:
                v = self.dma_sems[s_][1]
            return s_, v
        return d.token

    def simulate(self):
        cnt = {e: 0 for e in ENGS}
        for e in ENGS:
            c = 0
            for op in self.streams[e]:
                if op.is_dma or op.fn is None:
                    continue
                if op.signal:
                    c += 1
                    op.token = (e, c)
        sem = {}
        pos = {e: 0 for e in ENGS}
        progress = True
        while progress:
            progress = False
            for e in ENGS:
                st = self.streams[e]
                while pos[e] < len(st):
                    op = st[pos[e]]
                    ok = True
                    for d in op.deps:
                        s_, v = self._tok(d)
                        if sem.get(s_, 0) < v:
                            ok = False
                            break
                    if not ok:
                        break
                    if op.fn is not None:
                        if op.is_dma:
                            sem[op.token[0]] = sem.get(op.token[0], 0) + 16
                        elif op.signal:
                            sem[e] = sem.get(e, 0) + 1
                    pos[e] += 1
                    progress = True
        stuck = {e: (pos[e], len(self.streams[e])) for e in ENGS if pos[e] < len(self.streams[e])}
        return stuck

    def emit(self, final_wait_ops=()):
        nc = self.nc
        stuck = self.simulate()
        if stuck:
            raise RuntimeError(f"semaphore protocol deadlock: {stuck}")
        with ExitStack() as es:
            eng_sem = {e: es.enter_context(nc.semaphore(f"s_{e}")) for e in ENGS}
            for name, ent in self.dma_sems.items():
                ent[0] = es.enter_context(nc.semaphore(f"d_{name}"))
            for e in ENGS:
                cnt = 0
                for op in self.streams[e]:
                    if op.is_dma or op.fn is None:
                        continue
                    if op.signal:
                        cnt += 1
                        op.token = (e, cnt)
            block = es.enter_context(nc.Block())

            def handle(s):
                return eng_sem[s] if s in eng_sem else self.dma_sems[s][0]

            def make(e):
                def body(eng):
                    known = {}
                    for op in self.streams[e]:
                        need = {}
                        for d in op.deps:
                            s, v = self._tok(d)
                            if v > need.get(s, 0):
                                need[s] = v
                        for s, v in need.items():
                            if known.get(s, 0) >= v:
                                continue
                            known[s] = v
                            eng.wait_ge(handle(s), v)
                        if op.fn is None:
                            continue
                        ins = op.fn(eng)
                        if op.is_dma:
                            ins.then_inc(self.dma_sems[op.token[0]][0], 16)
                        elif op.signal:
                            ins.then_inc(eng_sem[e], 1)
                    if e == "sp":
                        for op in final_wait_ops:
                            s = op.token[0]
                            eng.wait_ge(handle(s), self.dma_sems[s][1])
                return body

            block.tensor(make("pe"))
            block.scalar(make("act"))
            block.vector(make("dve"))
            block.gpsimd(make("pool"))
            block.sync(make("sp"))


def _mm(P, items, reads, writes):
    def fn(e):
        ins = None
        for (o, l, r, st, sp) in items:
            ins = e.matmul(out=o, lhsT=l, rhs=r, start=st, stop=sp)
        return ins
    return P.op("pe", fn, reads, writes)


class _Stop(Exception):
    pass


def build_program(debug=None):
    debug = debug or ()
    try:
        return _build_program(debug)
    except _Stop as ex:
        return ex.args[0]


def _build_program(debug):
    nc = bass.Bass("TRN2", target_bir_lowering=False)

    def din(name, shape, dt=F32):
        return nc.dram_tensor(name, list(shape), dt, kind="ExternalInput").ap()

    x_own = din("x_own", [TOK, D]); x_prev = din("x_prev", [TOK, D])
    cT_d = din("cT", [128, NCH]); flag_d = din("flag", [128, 1])
    w_ada = din("w_ada", [D, 6 * D]); b_adaT_d = din("b_adaT", [128, 96]); b_adag_d = din("b_adag", [1, 2 * D])
    n1T_d = din("n1T", [128, NCH]); n2T_d = din("n2T", [128, NCH]); nf_d = din("nf_bc", [128, D])
    w_in = din("w_in", [D, 13312]); convw_d = din("conv_wT", [128, 8, 3])
    w_co = din("w_conv_out", [1024, D]); w_ro = din("w_ret_out", [D, D]); w_o = din("w_o", [D, D])
    w_r = din("w_r", [D, 36]); b_r_d = din("b_r", [128, 36])
    if "nomoe" not in debug:
        wg_r = din("wg_r", [NEXP * 1024, D]); wu_r = din("wu_r", [NEXP * 1024, D]); wd_r = din("wd_r", [NEXP * 1024, D])
    ltri_d = din("ltri", [128, 128]); jrow_d = din("jrow", [128, 48]); p8_d = din("p8", [128, 1]); j8_d = din("j8", [128, 8])
    xbuf = nc.dram_tensor("xbuf", [48 * 128, D], BF16, kind="Internal").ap()
    ybuf = nc.dram_tensor("ybuf", [48 * 128, D], F32, kind="Internal").ap()
    x1buf = nc.dram_tensor("x1buf", [TOK, D], F32, kind="Internal").ap()
    ident_d = din("ident", [128, 128])
    cs_own_d = din("cs_own", [128, NT, 128]); sn_own_d = din("sn_own", [128, NT, 128])
    cs_prev_d = din("cs_prev", [128, NT, 128]); sn_prev_d = din("sn_prev", [128, NT, 128])
    maskT_d = din("maskT", [128, 8, 128]); qdec_d = din("qdec", [128, 8, 128]); kdec_d = din("kdec", [128, 8])
    y_out = nc.dram_tensor("y", [TOK, D], F32, kind="ExternalOutput").ap()
    dbg_outs = {}

    log_g = np.log1p(-(2.0 ** (-5.0 - np.arange(8, dtype=np.float32)))).astype(np.float32)
    cdec = [float(np.exp(np.float32(log_g[h] * 128.0))) for h in range(8)]

    P = Prog(nc)
    fin = []

    def dump(name, ap2d, bufs, dt=F32):
        if name not in debug:
            return
        d = nc.dram_tensor("dbg_" + name, list(ap2d.shape), dt, kind="ExternalOutput").ap()
        fin.append(P.dma("sp", lambda e, d=d: e.dma_start(out=d[:, :], in_=ap2d), "st_" + name, reads=list(bufs)))

    _regcache = {}

    def breg(e, v):
        if v not in _regcache:
            _regcache[v] = e.to_reg(v)
        return _regcache[v]

    def stop(name):
        if name in debug:
            P.emit(final_wait_ops=fin)
            raise _Stop(nc)

    with ExitStack() as G:
        _cnt = [0]

        def S(name, shape, dt=F32, es=G):
            _cnt[0] += 1
            return es.enter_context(nc.sbuf_tensor(f"{name}_{_cnt[0]}", list(shape), dt))

        psb = [G.enter_context(nc.psum_tensor(f"psb{i}", [128, 512], F32)) for i in range(8)]
        pb = P.bufs("pb", 8)
        for b_ in pb:
            b_.psum = True

        ident = S("ident", [128, 128]); b_ident = P.buf("ident")
        ones_bf = S("ones_bf", [128, 128], BF16); b_ones = P.buf("ones")
        ones_row = S("ones_row", [1, 128]); b_onesr = P.buf("onesr")
        epst = S("epst", [128, 1]); b_eps = P.buf("eps")
        cTf = S("cTf", [128, NCH]); cact = S("cact", [128, NCH], BF16); b_cT = P.buf("cT"); b_cact = P.buf("cact")
        flag = S("flag", [128, 1]); b_flag = P.buf("flag")
        b_adaT = S("b_adaT", [128, 96]); b_badaT = P.buf("badaT")
        n1T = S("n1T", [128, NCH]); n2T = S("n2T", [128, NCH]); b_n1 = P.buf("n1"); b_n2 = P.buf("n2")
        mod = S("mod", [128, 96]); b_mod = P.buf("mod")
        A1 = S("A1", [128, NCH]); A2 = S("A2", [128, NCH]); b_A1 = P.buf("A1"); b_A2 = P.buf("A2")
        gbc = S("gbc", [128, 2 * D]); b_gbc = P.bufs("gbc", 8)
        convw = S("convw", [128, 8, 3]); b_convw = P.buf("convw")
        kdec = S("kdec", [128, 8]); kdecp = S("kdecp", [128, 8]); b_kdec = P.buf("kdec"); b_kdecp = P.buf("kdecp")
        b_rt = S("b_rt", [128, 36]); b_br = P.buf("br")
        wr = S("wr", [128, NCH, 36], BF16); b_wr = P.buf("wr")
        htail = S("htail", [128, NCH, 2], BF16); b_htail = P.buf("htail")
        sst = S("sst", [128, 4]); b_ss = P.bufs("ss", 2); b_t1 = P.bufs("t1", 2)
        rstd = S("rstd", [128, 2]); b_rstd = P.bufs("rstd", 2)

        def ld(dst, src, b, sem="ld0"):
            P.wait_all.add(sem)
            P.dma("sp", lambda e: e.dma_start(out=dst, in_=src), sem, writes=[b])

        ld(ident[:, :], ident_d[:, :], b_ident)
        ld(cTf[:, :], cT_d[:, :], b_cT)
        ld(flag[:, :], flag_d[:, :], b_flag)
        ld(b_adaT[:, :], b_adaT_d[:, :], b_badaT)
        ld(n1T[:, :], n1T_d[:, :], b_n1)
        ld(n2T[:, :], n2T_d[:, :], b_n2)
        ld(convw[:, :, :], convw_d[:, :, :], b_convw)
        ld(kdec[:, :], kdec_d[:, :], b_kdec)
        ld(b_rt[:, :], b_r_d[:, :], b_br)
        P.dma("pool", lambda e: e.dma_start(out=wr[:, :, :], in_=w_r.rearrange("(kt p) n -> p kt n", p=128)), "wr", writes=[b_wr])
        P.op("dve", lambda e: e.memset(ones_bf[:, :], 1.0), [], [b_ones])
        P.op("dve", lambda e: e.memset(ones_row[:, :], 1.0), [], [b_onesr])
        P.op("dve", lambda e: e.memset(epst[:, :], EPS), [], [b_eps])
        P.op("act", lambda e: e.activation(out=cact[:, :], in_=cTf[:, :], func=AF.Silu), [b_cT], [b_cact])
        P.op("dve", lambda e: e.tensor_scalar(out=kdecp[:, :], in0=kdec[:, :], scalar1=flag[:, 0:1], scalar2=None, op0=ALU.mult),
             [b_kdec, b_flag], [b_kdecp])

        with ExitStack() as E0:
            def _phase_E0():
                slots = [S(f"ada{i}", [128, NCH, 512], BF16, E0) for i in range(3)]
                b_slots = P.bufs("ada", 3)
                grow = S("grow", [1, 2 * D], es=E0); b_grow = P.bufs("grow", 8)
                badag = S("badag", [1, 2 * D], es=E0); b_badag = P.buf("badag")
                ld(badag[:, :], b_adag_d[:, :], b_badag)
                gi = 0
                for t in range(24):
                    s = t % 3
                    seg = t // 4
                    P.dma("pool", lambda e, t=t, s=s: e.dma_start(
                        out=slots[s][:, :, :], in_=w_ada[:, 512 * t:512 * (t + 1)].rearrange("(kt p) n -> p kt n", p=128)),
                        f"ada{s}", writes=[b_slots[s]])
                    if seg in (2, 5):
                        items = [(psb[1][0:1, :], cact[:, kt:kt + 1], slots[s][:, kt, :], kt == 0, kt == NCH - 1) for kt in range(NCH)]
                        _mm(P, items, [b_cact, b_slots[s]], [pb[1]])
                        P.op("dve", lambda e, gi=gi: e.tensor_tensor(out=grow[0:1, gi * 512:(gi + 1) * 512], in0=psb[1][0:1, :],
                                                                     in1=badag[0:1, gi * 512:(gi + 1) * 512], op=ALU.add),
                             [pb[1], b_badag], [b_grow[gi]])
                        gi += 1
                    else:
                        items = []
                        for blk in range(4):
                            j = 4 * t + blk
                            for kt in range(NCH):
                                items.append((psb[0][:, j:j + 1], slots[s][:, kt, blk * 128:(blk + 1) * 128], cact[:, kt:kt + 1],
                                              kt == 0, kt == NCH - 1))
                        _mm(P, items, [b_cact, b_slots[s]], [pb[0]])
                P.op("dve", lambda e: e.tensor_tensor(out=mod[:, 0:32], in0=psb[0][:, 0:32], in1=b_adaT[:, 0:32], op=ALU.add),
                     [pb[0], b_badaT], [b_mod])
                P.op("dve", lambda e: e.tensor_tensor(out=mod[:, 48:80], in0=psb[0][:, 48:80], in1=b_adaT[:, 48:80], op=ALU.add),
                     [pb[0], b_badaT, b_mod], [b_mod])
                P.op("dve", lambda e: e.scalar_tensor_tensor(out=A1[:, :], in0=mod[:, 16:32], scalar=1.0, in1=n1T[:, :], op0=ALU.add, op1=ALU.mult),
                     [b_mod, b_n1], [b_A1])
                P.op("dve", lambda e: e.scalar_tensor_tensor(out=A2[:, :], in0=mod[:, 64:80], scalar=1.0, in1=n2T[:, :], op0=ALU.add, op1=ALU.mult),
                     [b_mod, b_n2], [b_A2])
                for gi in range(8):
                    bk = 2 + gi % 2
                    _mm(P, [(psb[bk][:, :], ones_row[0:1, :], grow[0:1, gi * 512:(gi + 1) * 512], True, True)], [b_onesr, b_grow[gi]], [pb[bk]])
                    P.op("act", lambda e, gi=gi, bk=bk: e.copy(out=gbc[:, gi * 512:(gi + 1) * 512], in_=psb[bk][:, :]), [pb[bk]], [b_gbc[gi]])
                P.barrier()
            _phase_E0()
        B1 = mod[:, 0:16]
        B2 = mod[:, 48:64]
        if "mod" in debug:
            d = nc.dram_tensor("dbg_mod", [128, 96], F32, kind="ExternalOutput").ap()
            fin.append(P.dma("sp", lambda e, d=d: e.dma_start(out=d[:, :], in_=mod[:, :]), "st_mod", reads=[b_mod]))
            d = nc.dram_tensor("dbg_gbc", [128, 2 * D], F32, kind="ExternalOutput").ap()
            fin.append(P.dma("sp", lambda e, d=d: e.dma_start(out=d[:, :], in_=gbc[:, :]), "st_gbc", reads=b_gbc))
        if "stop0" in debug:
            P.emit(final_wait_ops=fin)
            return nc

        def make_hT(src_ap, b_src, Aap, Bap, b_AB, hT, b_hT_t, t, xs_slots, b_xs, junk, b_junk, k):
            s = k % 2
            P.op("act", lambda e: e.activation(out=junk[:, :], in_=src_ap, func=AF.Square, accum_out=sst[:, s:s + 1]),
                 b_src, [b_junk, b_ss[s]])
            P.op("dve", lambda e: e.tensor_scalar(out=sst[:, 2 + s:3 + s], in0=sst[:, s:s + 1], scalar1=1.0 / D, scalar2=EPS,
                                                  op0=ALU.mult, op1=ALU.add), [b_ss[s]], [b_t1[s]])
            P.op("act", lambda e: e.activation(out=sst[:, 2 + s:3 + s], in_=sst[:, 2 + s:3 + s], func=AF.Sqrt), [b_t1[s]], [b_t1[s]])
            P.op("dve", lambda e: e.reciprocal(out=rstd[:, s:s + 1], in_=sst[:, 2 + s:3 + s]), [b_t1[s]], [b_rstd[s]])
            xs = xs_slots[s]
            P.op("act", lambda e: e.activation(out=xs[:, :], in_=src_ap, func=AF.Copy, scale=rstd[:, s:s + 1]),
                 b_src + [b_rstd[s]], [b_xs[s]])
            for g in range(4):
                bk = g % 2

                def tr(e, g=g, bk=bk):
                    ins = None
                    for j in range(4):
                        c = 4 * g + j
                        ins = e.transpose(out=psb[bk][:, j * 128:(j + 1) * 128], in_=xs[:, c * 128:(c + 1) * 128], identity=ident[:, :])
                    return ins
                P.op("pe", tr, [b_xs[s], b_ident], [pb[bk]])
                if g % 2 == 0:
                    def ev(e, g=g, bk=bk):
                        ins = None
                        for j in range(4):
                            c = 4 * g + j
                            ins = e.activation(out=hT[:, c, t * 128:(t + 1) * 128], in_=psb[bk][:, j * 128:(j + 1) * 128],
                                               func=AF.Identity, scale=Aap[:, c:c + 1], bias=Bap[:, c:c + 1])
                        return ins
                    P.op("act", ev, [pb[bk], b_AB, b_mod], [b_hT_t[g]])
                else:
                    def ev(e, g=g, bk=bk):
                        ins = None
                        for j in range(4):
                            c = 4 * g + j
                            ins = e.tensor_scalar(out=hT[:, c, t * 128:(t + 1) * 128], in0=psb[bk][:, j * 128:(j + 1) * 128],
                                                  scalar1=Aap[:, c:c + 1], scalar2=Bap[:, c:c + 1], op0=ALU.mult, op1=ALU.add)
                        return ins
                    P.op("dve", ev, [pb[bk], b_AB, b_mod], [b_hT_t[g]])

        def rotary(src_ps, cs, sn, t, m1, m2, b_m, dst, b_dst, b_src, b_tab):
            P.op("dve", lambda e: e.tensor_tensor(out=m1[:, :], in0=src_ps, in1=cs[:, t, :], op=ALU.mult), [b_src] + b_tab, [b_m[0]])
            P.op("dve", lambda e: e.tensor_tensor(out=m2[:, :], in0=src_ps, in1=sn[:, t, :], op=ALU.mult), [b_src] + b_tab, [b_m[1]])
            P.op("dve", lambda e: e.tensor_tensor(out=dst[:, 0:64], in0=m1[:, 0:64], in1=m2[:, 64:128], op=ALU.subtract),
                 [b_m[0], b_m[1]], [b_dst[0]])
            P.op("dve", lambda e: e.tensor_tensor(out=dst[:, 64:128], in0=m2[:, 0:64], in1=m1[:, 64:128], op=ALU.add),
                 [b_m[0], b_m[1]], [b_dst[1]])

        hT = S("hT", [128, NCH, TOK], BF16)
        b_hT = [P.bufs(f"hT{t}_", 4) for t in range(NT)]
        mergedT = S("mergedT", [128, NCH, TOK], BF16)
        b_mg = [P.bufs(f"mg{d}_", 2) for d in range(NCH)]

        with ExitStack() as E12:
            Sst = S("Sst", [128, 8, 256], es=E12); b_S = P.bufs("S", 8)
            P.op("dve", lambda e: e.memset(Sst[:, :, :], 0.0), [], b_S)

            with ExitStack() as E1:
                def _phase_E1():
                    xt = [S(f"xt{i}", [128, D], es=E1) for i in range(2)]; b_xt = P.bufs("xt", 2)
                    xs_slots = [S(f"xs{i}", [128, D], es=E1) for i in range(2)]; b_xs = P.bufs("xs", 2)
                    junk = S("junk", [128, D], BF16, E1); b_junk = P.buf("junk")
                    csp = S("csp", [128, NT, 128], es=E1); snp = S("snp", [128, NT, 128], es=E1); b_tabp = P.bufs("tabp", 2)
                    ld(csp[:, :, :], cs_prev_d[:, :, :], b_tabp[0], "ld1")
                    ld(snp[:, :, :], sn_prev_d[:, :, :], b_tabp[1], "ld1")
                    kvp = [S(f"kvp{i}", [128, NCH, 384], BF16, E1) for i in range(2)]
                    b_kvp = [P.bufs(f"kvp{i}_", 2) for i in range(2)]
                    m1_ = [S(f"m1{i}", [128, 128], es=E1) for i in range(2)]; m2_ = [S(f"m2{i}", [128, 128], es=E1) for i in range(2)]
                    b_m_ = [P.bufs(f"m{i}_", 2) for i in range(2)]
                    krot_ = [S(f"krot{i}", [128, 128], es=E1) for i in range(2)]; b_krot_ = [P.bufs(f"krot{i}_", 2) for i in range(2)]
                    kd_ = [S(f"kd{i}", [128, 128], BF16, E1) for i in range(2)]; b_kd_ = P.bufs("kd", 2)
                    vb_ = [S(f"vb{i}", [128, 256], BF16, E1) for i in range(2)]; b_vb_ = P.bufs("vb", 2)
                    for t in range(NT):
                        s = t % 2
                        P.dma("sp", lambda e, t=t, s=s: e.dma_start(out=xt[s][:, :], in_=x_prev[t * 128:(t + 1) * 128, :]), f"xt{s}", writes=[b_xt[s]])
                        make_hT(xt[s][:, :], [b_xt[s]], A1, B1, b_A1, hT, b_hT[t], t, xs_slots, b_xs, junk, b_junk, t)
                    dump("hTp", hT[:, :, :].rearrange("p c t -> p (c t)"), [b for t in range(NT) for b in b_hT[t]], BF16)
                    stop("stop1a")
                    P.op("act", lambda e: e.copy(out=htail[:, :, :], in_=hT[:, :, TOK - 2:TOK]), b_hT[NT - 1], [b_htail])
                    stop("stop1b")
                    def p1A(h, t):
                        s = h % 2
                        if t == 0:
                            P.dma("pool", lambda e: e.dma_start(
                                out=kvp[s][:, :, 0:128], in_=w_in[:, 4096 + 128 * h:4096 + 128 * (h + 1)].rearrange("(kt p) n -> p kt n", p=128)),
                                f"kvp{s}", writes=[b_kvp[s][0]])
                            P.dma("pool", lambda e: e.dma_start(
                                out=kvp[s][:, :, 128:384], in_=w_in[:, 5120 + 256 * h:5120 + 256 * (h + 1)].rearrange("(kt p) n -> p kt n", p=128)),
                                f"kvp{s}", writes=[b_kvp[s][1]])
                        bk = 2 + t % 2
                        ip = t % 2
                        m1 = m1_[ip]; m2 = m2_[ip]; b_m = b_m_[ip]; krot = krot_[ip]; b_krot = b_krot_[ip]
                        kd = kd_[ip]; b_kd = b_kd_[ip]; vb = vb_[ip]; b_vb = b_vb_[ip]
                        items = [(psb[bk][:, 0:384], hT[:, c, t * 128:(t + 1) * 128], kvp[s][:, c, :], c == 0, c == NCH - 1) for c in range(NCH)]
                        _mm(P, items, b_hT[t] + b_kvp[s], [pb[bk]])
                        rotary(psb[bk][:, 0:128], csp, snp, t, m1, m2, b_m, krot, b_krot, pb[bk], b_tabp)
                        P.op("act", lambda e: e.copy(out=vb[:, :], in_=psb[bk][:, 128:384]), [pb[bk]], [b_vb])
                        P.op("act", lambda e: e.activation(out=kd[:, :], in_=krot[:, :], func=AF.Copy, scale=kdecp[:, h:h + 1]),
                             b_krot + [b_kdecp], [b_kd])

                    def p1B(h, t):
                        ip = t % 2
                        kd = kd_[ip]; b_kd = b_kd_[ip]; vb = vb_[ip]; b_vb = b_vb_[ip]
                        bk2 = 4 + t % 2
                        _mm(P, [(psb[bk2][:, 0:256], kd[:, :], vb[:, :], True, True)], [b_kd, b_vb], [pb[bk2]])
                        P.op("dve", lambda e: e.scalar_tensor_tensor(
                            out=Sst[:, h, :], in0=Sst[:, h, :], scalar=cdec[h], in1=psb[bk2][:, 0:256], op0=ALU.mult, op1=ALU.add),
                            [pb[bk2], b_S[h]], [b_S[h]])

                    seq1 = [(h, t) for h in range(8) for t in range(NT)]
                    for i in range(len(seq1) + 1):
                        if i < len(seq1):
                            p1A(*seq1[i])
                        if i >= 1:
                            p1B(*seq1[i - 1])
                    P.barrier()
                _phase_E1()

            if "stop1" in debug:
                dump("S", Sst[:, :, :].rearrange("p h e -> p (h e)"), b_S)
                stop("stop1")
            if "S" in debug:
                d = nc.dram_tensor("dbg_S", [128, 8 * 256], F32, kind="ExternalOutput").ap()
                fin.append(P.dma("sp", lambda e, d=d: e.dma_start(out=d[:, :], in_=Sst[:, :, :].rearrange("p h e -> p (h e)")), "st_S", reads=b_S))

            with ExitStack() as E2a:
                def _phase_E2a():
                    xt = [S(f"xt{i}", [128, D], es=E2a) for i in range(2)]; b_xt = P.bufs("xt", 2)
                    xs_slots = [S(f"xs{i}", [128, D], es=E2a) for i in range(2)]; b_xs = P.bufs("xs", 2)
                    junk = S("junk", [128, D], BF16, E2a); b_junk = P.buf("junk")
                    for t in range(NT):
                        s = t % 2
                        P.dma("sp", lambda e, t=t, s=s: e.dma_start(out=xt[s][:, :], in_=x_own[t * 128:(t + 1) * 128, :]), f"xt{s}", writes=[b_xt[s]])
                        make_hT(xt[s][:, :], [b_xt[s]], A1, B1, b_A1, hT, b_hT[t], t, xs_slots, b_xs, junk, b_junk, t)
                    P.barrier()
                _phase_E2a()
            all_hT = [b for t in range(NT) for b in b_hT[t]]

            if "hT" in debug:
                d = nc.dram_tensor("dbg_hT", [128, NCH * TOK], BF16, kind="ExternalOutput").ap()
                fin.append(P.dma("sp", lambda e, d=d: e.dma_start(out=d[:, :], in_=hT[:, :, :].rearrange("p c t -> p (c t)")), "st_hT", reads=all_hT))

            stop("stop2a")
            zbT = S("zbT", [128, NCH, TOK], BF16, E12); b_zb = [P.bufs(f"zb{c}_", NT) for c in range(NCH)]

            with ExitStack() as E2b:
                def _phase_E2b():
                    cso = S("cso", [128, NT, 128], es=E2b); sno = S("sno", [128, NT, 128], es=E2b); b_tabo = P.bufs("tabo", 2)
                    maskT = S("maskT", [128, 8, 128], es=E2b); qdec = S("qdec", [128, 8, 128], es=E2b); b_msk = P.buf("msk"); b_qdec = P.buf("qdecb")
                    ld(cso[:, :, :], cs_own_d[:, :, :], b_tabo[0], "ld2")
                    ld(sno[:, :, :], sn_own_d[:, :, :], b_tabo[1], "ld2")
                    ld(maskT[:, :, :], maskT_d[:, :, :], b_msk, "ld2")
                    ld(qdec[:, :, :], qdec_d[:, :, :], b_qdec, "ld2")
                    qkvp = [S(f"qkvp{i}", [128, NCH, 512], BF16, E2b) for i in range(2)]
                    b_qkvp = [P.bufs(f"qkvp{i}_", 3) for i in range(2)]
                    rgp = S("rgp", [128, NCH, 256], BF16, E2b); b_rgp = P.buf("rgp")
                    srg_ = [S(f"srg{i}", [128, 2, TOK], BF16, E2b) for i in range(2)]; b_srg_ = [P.bufs(f"srg{i}_", 4) for i in range(2)]
                    def two(name, shape, dt=F32):
                        return [S(f"{name}{i}", shape, dt, E2b) for i in range(2)]
                    mq1_ = two("mq1", [128, 128]); mq2_ = two("mq2", [128, 128]); b_mq_ = [P.bufs(f"mq{i}_", 2) for i in range(2)]
                    mk1_ = two("mk1", [128, 128]); mk2_ = two("mk2", [128, 128]); b_mk_ = [P.bufs(f"mk{i}_", 2) for i in range(2)]
                    qrot_ = two("qrot", [128, 128]); b_qrot_ = [P.bufs(f"qrot{i}_", 2) for i in range(2)]
                    krot_ = two("krot", [128, 128]); b_krot_ = [P.bufs(f"krot{i}_", 2) for i in range(2)]
                    kd_ = two("kd", [128, 128], BF16); b_kd_ = P.bufs("kd", 2)
                    vb_ = two("vb", [128, 256], BF16); b_vb_ = P.bufs("vb", 2)
                    qT_ = two("qT", [128, 128], BF16); qTd_ = two("qTd", [128, 128], BF16); kT_ = two("kT", [128, 128], BF16)
                    b_qT_ = P.bufs("qT", 2); b_qTd_ = P.bufs("qTd", 2); b_kT_ = P.bufs("kT", 2)
                    AT_ = two("AT", [128, 128], BF16); b_AT_ = P.bufs("AT", 2)
                    Sb = S("Sb", [128, 256], BF16, E2b); b_Sb = P.buf("Sb")
                    sq_ = two("sq", [128, 256], BF16); b_sq_ = P.bufs("sq", 2)
                    rbc_ = two("rbc", [128, 128]); b_rbc_ = P.bufs("rbc", 2)
                    ztmp_ = two("ztmp", [128, 2, 128]); b_ztmp_ = P.bufs("ztmp", 2)

                    def chunk(h, t, s, ip, stage):
                        srg = srg_[h % 2]; b_srg = b_srg_[h % 2]
                        pT_ = psb[ip]; r_T = pb[ip]
                        bsc = 6 if ip == 0 else 4
                        pSC = psb[bsc]; r_sc = pb[bsc]
                        brt = 7 if ip == 0 else 5
                        pRET = psb[brt]; r_ret = pb[brt]
                        mq1 = mq1_[ip]; mq2 = mq2_[ip]; b_mq = b_mq_[ip]; mk1 = mk1_[ip]; mk2 = mk2_[ip]; b_mk = b_mk_[ip]
                        qrot = qrot_[ip]; b_qrot = b_qrot_[ip]; krot = krot_[ip]; b_krot = b_krot_[ip]
                        kd = kd_[ip]; b_kd = b_kd_[ip]; vb = vb_[ip]; b_vb = b_vb_[ip]
                        qT = qT_[ip]; qTd = qTd_[ip]; kT = kT_[ip]; b_qT = b_qT_[ip]; b_qTd = b_qTd_[ip]; b_kT = b_kT_[ip]
                        AT = AT_[ip]; b_AT = b_AT_[ip]; sq = sq_[ip]; b_sq = b_sq_[ip]; rbc = rbc_[ip]; b_rbc = b_rbc_[ip]
                        ztmp = ztmp_[ip]; b_ztmp = b_ztmp_[ip]
                        bk = 2 + t % 2
                        if stage == "B":
                            return chunkB(locals())
                        items = [(psb[bk][:, :], hT[:, c, t * 128:(t + 1) * 128], qkvp[s][:, c, :], c == 0, c == NCH - 1) for c in range(NCH)]
                        _mm(P, items, b_hT[t] + b_qkvp[s], [pb[bk]])
                        rotary(psb[bk][:, 0:128], cso, sno, t, mq1, mq2, b_mq, qrot, b_qrot, pb[bk], b_tabo)
                        rotary(psb[bk][:, 128:256], cso, sno, t, mk1, mk2, b_mk, krot, b_krot, pb[bk], b_tabo)
                        P.op("act", lambda e: e.copy(out=vb[:, :], in_=psb[bk][:, 256:512]), [pb[bk]], [b_vb])
                        P.op("act", lambda e: e.activation(out=kd[:, :], in_=krot[:, :], func=AF.Copy, scale=kdec[:, h:h + 1]),
                             b_krot + [b_kdec], [b_kd])

                        def trq(e):
                            e.transpose(out=pT_[:, 0:128], in_=qrot[:, :], identity=ident[:, :])
                            return e.transpose(out=pT_[:, 128:256], in_=krot[:, :], identity=ident[:, :])
                        P.op("pe", trq, b_qrot + b_krot + [b_ident], [r_T])
                        P.op("act", lambda e: e.copy(out=qT[:, :], in_=pT_[:, 0:128]), [r_T], [b_qT])
                        P.op("act", lambda e: e.copy(out=kT[:, :], in_=pT_[:, 128:256]), [r_T], [b_kT])
                        P.op("dve", lambda e: e.tensor_tensor(out=qTd[:, :], in0=pT_[:, 0:128], in1=qdec[:, h, :], op=ALU.mult),
                             [r_T, b_qdec], [b_qTd])
                        _mm(P, [(pSC[:, 0:128], kT[:, :], qT[:, :], True, True)], [b_kT, b_qT], [r_sc])
                        P.op("dve", lambda e: e.tensor_tensor(out=AT[:, :], in0=pSC[:, 0:128], in1=maskT[:, h, :], op=ALU.mult),
                             [r_sc, b_msk], [b_AT])

                    def chunkB(L):
                        h = L["h"]; t = L["t"]; srg = L["srg"]; b_srg = L["b_srg"]
                        pSC = L["pSC"]; r_sc = L["r_sc"]; pRET = L["pRET"]; r_ret = L["r_ret"]
                        kd = L["kd"]; b_kd = L["b_kd"]; vb = L["vb"]; b_vb = L["b_vb"]; qTd = L["qTd"]; b_qTd = L["b_qTd"]
                        AT = L["AT"]; b_AT = L["b_AT"]; sq = L["sq"]; b_sq = L["b_sq"]; rbc = L["rbc"]; b_rbc = L["b_rbc"]
                        ztmp = L["ztmp"]; b_ztmp = L["b_ztmp"]
                        if t == 0:
                            P.op("act", lambda e: e.copy(out=Sb[:, :], in_=Sst[:, h, :]), [b_S[h]], [b_Sb])
                        items = []
                        for eb in range(2):
                            items.append((pRET[:, eb * 128:(eb + 1) * 128], vb[:, eb * 128:(eb + 1) * 128], AT[:, :], True, False))
                            items.append((pRET[:, eb * 128:(eb + 1) * 128], Sb[:, eb * 128:(eb + 1) * 128], qTd[:, :], False, True))
                        _mm(P, items, [b_vb, b_AT, b_Sb, b_qTd], [r_ret])
                        P.op("act", lambda e: e.activation(out=sq[:, :], in_=pRET[:, 0:256], func=AF.Square), [r_ret], [b_sq])
                        _mm(P, [(pSC[:, 128:256], ones_bf[:, :], sq[:, 0:128], True, False),
                                (pSC[:, 128:256], ones_bf[:, :], sq[:, 128:256], False, True)], [b_ones, b_sq], [r_sc])
                        P.op("dve", lambda e: e.tensor_scalar(out=rbc[:, :], in0=pSC[:, 128:256], scalar1=1.0 / 256, scalar2=EPS,
                                                              op0=ALU.mult, op1=ALU.add), [r_sc], [b_rbc])
                        P.op("act", lambda e: e.activation(out=rbc[:, :], in_=rbc[:, :], func=AF.Sqrt), [b_rbc], [b_rbc])
                        P.op("dve", lambda e: e.reciprocal(out=rbc[:, :], in_=rbc[:, :]), [b_rbc], [b_rbc])
                        P.op("dve", lambda e: e.tensor_tensor(out=ztmp[:, :, :], in0=pRET[:, 0:256].rearrange("p (a i) -> p a i", a=2),
                                                              in1=rbc[:, :].unsqueeze(1).to_broadcast([128, 2, 128]), op=ALU.mult),
                             [r_ret, b_rbc], [b_ztmp])
                        P.op("dve", lambda e: e.tensor_tensor(out=zbT[:, 2 * h:2 * h + 2, t * 128:(t + 1) * 128], in0=ztmp[:, :, :],
                                                              in1=srg[:, :, t * 128:(t + 1) * 128], op=ALU.mult),
                             [b_ztmp] + b_srg, [b_zb[2 * h][t], b_zb[2 * h + 1][t]])
                        if t < NT - 1:
                            _mm(P, [(pRET[:, 256:512], kd[:, :], vb[:, :], True, True)], [b_kd, b_vb], [r_ret])
                            P.op("dve", lambda e: e.scalar_tensor_tensor(
                                out=Sst[:, h, :], in0=Sst[:, h, :], scalar=cdec[h], in1=pRET[:, 256:512], op0=ALU.mult, op1=ALU.add),
                                [r_ret, b_S[h]], [b_S[h]])
                            P.op("act", lambda e: e.copy(out=Sb[:, :], in_=Sst[:, h, :]), [b_S[h]], [b_Sb])

                    def prologue(h):
                        s = h % 2
                        srg = srg_[h % 2]; b_srg = b_srg_[h % 2]
                        for i, (c0, n, off) in enumerate([(3072 + 128 * h, 128, 0), (4096 + 128 * h, 128, 128), (5120 + 256 * h, 256, 256)]):
                            P.dma("pool", lambda e, s=s, c0=c0, n=n, off=off: e.dma_start(
                                out=qkvp[s][:, :, off:off + n], in_=w_in[:, c0:c0 + n].rearrange("(kt p) n -> p kt n", p=128)),
                                f"qkvp{s}", writes=[b_qkvp[s][i]])
                        P.dma("pool", lambda e, h=h: e.dma_start(
                            out=rgp[:, :, :], in_=w_in[:, 7168 + 256 * h:7168 + 256 * (h + 1)].rearrange("(kt p) n -> p kt n", p=128)),
                            "rgp", writes=[b_rgp])
                        for eb in range(2):
                            for th in range(2):
                                bk = 4 + th
                                items = [(psb[bk][:, :], rgp[:, c, eb * 128:(eb + 1) * 128], hT[:, c, th * 512:(th + 1) * 512], c == 0, c == NCH - 1)
                                         for c in range(NCH)]
                                _mm(P, items, all_hT + [b_rgp], [pb[bk]])
                                P.op("act", lambda e, eb=eb, th=th, bk=bk: e.activation(out=srg[:, eb, th * 512:(th + 1) * 512], in_=psb[bk][:, :], func=AF.Silu),
                                     [pb[bk]], [b_srg[eb * 2 + th]])

                    seq2 = [(h, t) for h in range(8) for t in range(NT)]
                    for i in range(len(seq2) + 1):
                        if i < len(seq2):
                            h, t = seq2[i]
                            if t == 0:
                                prologue(h)
                            chunk(h, t, h % 2, t % 2, "A")
                        if i >= 1:
                            h, t = seq2[i - 1]
                            chunk(h, t, h % 2, t % 2, "B")
                    P.barrier()
                _phase_E2b()
            all_zb = [b for c in range(NCH) for b in b_zb[c]]
            if "zbT" in debug:
                d = nc.dram_tensor("dbg_zbT", [128, NCH * TOK], BF16, kind="ExternalOutput").ap()
                fin.append(P.dma("sp", lambda e, d=d: e.dma_start(out=d[:, :], in_=zbT[:, :, :].rearrange("p c t -> p (c t)")), "st_zb", reads=all_zb))

            stop("stop2b")
            zaT = S("zaT", [128, 8, TOK], BF16, E12); b_za = P.bufs("za", 8)

            with ExitStack() as E2c:
                def _phase_E2c():
                    cvp = [S(f"cvp{i}", [128, NCH, 384], BF16, E2c) for i in range(2)]
                    b_cvp = [P.bufs(f"cvp{i}_", 3) for i in range(2)]
                    ccs = S("ccs", [128, TOK], es=E2c); b_ccs = P.bufs("ccs", 2)
                    tt = S("tt", [128, TOK + 2], es=E2c); b_tt = P.bufs("tt", 3)
                    hcc = S("hcc", [128, 2], es=E2c); b_hcc = P.buf("hcc")
                    acc = S("acc", [128, TOK], es=E2c); b_acc = P.buf("acc")
                    r_h = pb[6]
                    for cb in range(8):
                        s = cb % 2
                        for i in range(3):
                            P.dma("pool", lambda e, s=s, i=i, cb=cb: e.dma_start(
                                out=cvp[s][:, :, i * 128:(i + 1) * 128],
                                in_=w_in[:, 1024 * i + 128 * cb:1024 * i + 128 * (cb + 1)].rearrange("(kt p) n -> p kt n", p=128)),
                                f"cvp{s}", writes=[b_cvp[s][i]])
                        items = [(psb[6][:, 0:2], cvp[s][:, c, 128:256], htail[:, c, :], c == 0, c == NCH - 1) for c in range(NCH)]
                        items += [(psb[6][:, 2:4], cvp[s][:, c, 256:384], htail[:, c, :], c == 0, c == NCH - 1) for c in range(NCH)]
                        _mm(P, items, [b_htail] + b_cvp[s], [r_h])
                        P.op("act", lambda e: e.activation(out=hcc[:, :], in_=psb[6][:, 0:2], func=AF.Copy, scale=flag[:, 0:1]), [r_h, b_flag], [b_hcc])
                        P.op("dve", lambda e: e.tensor_tensor(out=tt[:, 0:2], in0=hcc[:, :], in1=psb[6][:, 2:4], op=ALU.mult), [r_h, b_hcc], [b_tt[2]])
                        for th in range(2):
                            bk = 2 + th
                            items = [(psb[bk][:, :], cvp[s][:, c, 128:256], hT[:, c, th * 512:(th + 1) * 512], c == 0, c == NCH - 1) for c in range(NCH)]
                            _mm(P, items, all_hT + b_cvp[s], [pb[bk]])
                            P.op("act", lambda e, th=th, bk=bk: e.copy(out=ccs[:, th * 512:(th + 1) * 512], in_=psb[bk][:, :]), [pb[bk]], [b_ccs[th]])
                        for th in range(2):
                            bk = 4 + th
                            items = [(psb[bk][:, :], cvp[s][:, c, 256:384], hT[:, c, th * 512:(th + 1) * 512], c == 0, c == NCH - 1) for c in range(NCH)]
                            _mm(P, items, all_hT + b_cvp[s], [pb[bk]])
                            P.op("dve", lambda e, th=th, bk=bk: e.tensor_tensor(out=tt[:, 2 + th * 512:2 + (th + 1) * 512], in0=ccs[:, th * 512:(th + 1) * 512],
                                                                                in1=psb[bk][:, :], op=ALU.mult), [pb[bk], b_ccs[th]], [b_tt[th]])
                        P.op("dve", lambda e, cb=cb: e.tensor_scalar(out=acc[:, :], in0=tt[:, 0:TOK], scalar1=convw[:, cb, 0:1], scalar2=None, op0=ALU.mult),
                             b_tt + [b_convw], [b_acc])
                        P.op("dve", lambda e, cb=cb: e.scalar_tensor_tensor(out=acc[:, :], in0=tt[:, 1:TOK + 1], scalar=convw[:, cb, 1:2], in1=acc[:, :],
                                                                            op0=ALU.mult, op1=ALU.add), b_tt + [b_convw, b_acc], [b_acc])
                        P.op("dve", lambda e, cb=cb: e.scalar_tensor_tensor(out=acc[:, :], in0=tt[:, 2:TOK + 2], scalar=convw[:, cb, 2:3], in1=acc[:, :],
                                                                            op0=ALU.mult, op1=ALU.add), b_tt + [b_convw, b_acc], [b_acc])
                        for th in range(2):
                            bk = 2 + th
                            items = [(psb[bk][:, :], cvp[s][:, c, 0:128], hT[:, c, th * 512:(th + 1) * 512], c == 0, c == NCH - 1) for c in range(NCH)]
                            _mm(P, items, all_hT + b_cvp[s], [pb[bk]])
                            P.op("dve", lambda e, th=th, bk=bk, cb=cb: e.tensor_tensor(out=zaT[:, cb, th * 512:(th + 1) * 512], in0=acc[:, th * 512:(th + 1) * 512],
                                                                                       in1=psb[bk][:, :], op=ALU.mult), [pb[bk], b_acc], [b_za[cb]])
                    P.barrier()
                _phase_E2c()
            if "zaT" in debug:
                d = nc.dram_tensor("dbg_zaT", [128, 8 * TOK], BF16, kind="ExternalOutput").ap()
                fin.append(P.dma("sp", lambda e, d=d: e.dma_start(out=d[:, :], in_=zaT[:, :, :].rearrange("p c t -> p (c t)")), "st_za", reads=b_za))

            stop("stop2c")
            with ExitStack() as E2d:
                def _phase_E2d():
                    opp = [S(f"opp{i}", [128, 56, 128], BF16, E2d) for i in range(2)]
                    b_opp = [P.bufs(f"opp{i}_", 4) for i in range(2)]
                    sga = [S(f"sga{i}", [128, 512], es=E2d) for i in range(2)]; b_sga = P.bufs("sga", 2)
                    sgb = [S(f"sgb{i}", [128, 512], es=E2d) for i in range(2)]; b_sgb = P.bufs("sgb", 2)
                    ma = [S(f"ma{i}", [128, 512], es=E2d) for i in range(2)]; b_ma = P.bufs("ma", 2)
                    mb = [S(f"mb{i}", [128, 512], es=E2d) for i in range(2)]; b_mb = P.bufs("mb", 2)
                    banks = [(2, 3, 4, 5), (0, 1, 6, 7)]
                    for db in range(NCH):
                        s = db % 2
                        srcs = [(w_co[:, db * 128:(db + 1) * 128], 0, 8), (w_ro[:, db * 128:(db + 1) * 128], 8, 16),
                                (w_in[:, 9216 + db * 128:9216 + (db + 1) * 128], 24, 16), (w_in[:, 11264 + db * 128:11264 + (db + 1) * 128], 40, 16)]
                        for i, (src, off, n) in enumerate(srcs):
                            P.dma("pool", lambda e, s=s, src=src, off=off, n=n: e.dma_start(
                                out=opp[s][:, off:off + n, :], in_=src.rearrange("(kt p) n -> p kt n", p=128)), f"opp{s}", writes=[b_opp[s][i]])
                        for th in range(2):
                            ba, bb, bga, bgb = banks[th]
                            tsl = slice(th * 512, (th + 1) * 512)
                            _mm(P, [(psb[ba][:, :], opp[s][:, c, :], zaT[:, c, tsl], c == 0, c == 7) for c in range(8)], b_za + [b_opp[s][0]], [pb[ba]])
                            _mm(P, [(psb[bb][:, :], opp[s][:, 8 + c, :], zbT[:, c, tsl], c == 0, c == 15) for c in range(16)], all_zb + [b_opp[s][1]], [pb[bb]])
                            _mm(P, [(psb[bga][:, :], opp[s][:, 24 + c, :], hT[:, c, tsl], c == 0, c == 15) for c in range(16)], all_hT + [b_opp[s][2]], [pb[bga]])
                            _mm(P, [(psb[bgb][:, :], opp[s][:, 40 + c, :], hT[:, c, tsl], c == 0, c == 15) for c in range(16)], all_hT + [b_opp[s][3]], [pb[bgb]])
                            P.op("act", lambda e, th=th, bga=bga: e.activation(out=sga[th][:, :], in_=psb[bga][:, :], func=AF.Sigmoid), [pb[bga]], [b_sga[th]])
                            P.op("act", lambda e, th=th, bgb=bgb: e.activation(out=sgb[th][:, :], in_=psb[bgb][:, :], func=AF.Sigmoid), [pb[bgb]], [b_sgb[th]])
                            P.op("dve", lambda e, th=th, ba=ba: e.tensor_tensor(out=ma[th][:, :], in0=sga[th][:, :], in1=psb[ba][:, :], op=ALU.mult),
                                 [pb[ba], b_sga[th]], [b_ma[th]])
                            P.op("dve", lambda e, th=th, bb=bb: e.tensor_tensor(out=mb[th][:, :], in0=sgb[th][:, :], in1=psb[bb][:, :], op=ALU.mult),
                                 [pb[bb], b_sgb[th]], [b_mb[th]])
                            P.op("dve", lambda e, th=th, db=db, tsl=tsl: e.tensor_tensor(out=mergedT[:, db, tsl], in0=ma[th][:, :], in1=mb[th][:, :], op=ALU.add),
                                 [b_ma[th], b_mb[th]], [b_mg[db][th]])
                    P.barrier()
                _phase_E2d()
        all_mg = [b for d_ in range(NCH) for b in b_mg[d_]]
        if "mergedT" in debug:
            d = nc.dram_tensor("dbg_mergedT", [128, NCH * TOK], BF16, kind="ExternalOutput").ap()
            fin.append(P.dma("sp", lambda e, d=d: e.dma_start(out=d[:, :], in_=mergedT[:, :, :].rearrange("p c t -> p (c t)")), "st_mg", reads=all_mg))

        stop("stop2d")
        desti = [S(f"desti{k}", [128, NT], I32) for k in range(2)]; b_desti = P.buf("desti")
        cc = S("cc", [128, NT, 2]); b_cc = P.bufs("cc", NT)
        idxw = S("idxw", [128, 48 * 8], I32); b_idxw = P.buf("idxw")
        ident_bf = S("ident_bf", [128, 128], BF16); b_identb = P.buf("identb")
        P.op("act", lambda e: e.copy(out=ident_bf[:, :], in_=ident[:, :]), [b_ident], [b_identb])
        b_x1buf = P.bufs("x1buf", NT)
        with ExitStack() as EX1:
            x1 = S("x1", [128, NT, D], es=EX1); b_x1 = [P.bufs(f"x1_{t}_", 4) for t in range(NT)]
            P.wait_all.add("x1ld")
            for t in range(NT):
                P.dma("sp", lambda e, t=t: e.dma_start(out=x1[:, t, :], in_=x_own[t * 128:(t + 1) * 128, :]), "x1ld", writes=b_x1[t])
            with ExitStack() as E3:
                def _phase_E3():
                    wop = [S(f"wop{i}", [128, NCH, 256], BF16, E3) for i in range(2)]; b_wop = P.bufs("wop", 2)
                    tmp = [S(f"tmp{i}", [128, 256], es=E3) for i in range(2)]; b_tmp = P.bufs("tmp", 2)
                    xs_slots = [S(f"xs{i}", [128, D], es=E3) for i in range(2)]; b_xs = P.bufs("xs", 2)
                    junk = S("junk", [128, D], BF16, E3); b_junk = P.buf("junk")
                    ltri_f = S("ltri_f", [128, 128], es=E3); ltri = S("ltri", [128, 128], BF16, E3); b_ltf = P.buf("ltf"); b_ltri = P.buf("ltri")
                    jrow = S("jrow", [128, 48], es=E3); p8 = S("p8", [128, 1], es=E3); j8 = S("j8", [128, 8], es=E3)
                    b_jrow = P.buf("jrow"); b_p8 = P.buf("p8"); b_j8 = P.buf("j8")
                    ld(ltri_f[:, :], ltri_d[:, :], b_ltf, "ld3"); ld(jrow[:, :], jrow_d[:, :], b_jrow, "ld3")
                    ld(p8[:, :], p8_d[:, :], b_p8, "ld3"); ld(j8[:, :], j8_d[:, :], b_j8, "ld3")
                    P.op("act", lambda e: e.copy(out=ltri[:, :], in_=ltri_f[:, :]), [b_ltf], [b_ltri])
                    k = 0
                    for nb in range(8):
                        s = nb % 2
                        P.dma("pool", lambda e, s=s, nb=nb: e.dma_start(
                            out=wop[s][:, :, :], in_=w_o[:, nb * 256:(nb + 1) * 256].rearrange("(kt p) n -> p kt n", p=128)), f"wop{s}", writes=[b_wop[s]])
                        for t in range(NT):
                            bk = 2 + k % 2
                            q = nb // 2
                            _mm(P, [(psb[bk][:, 0:256], mergedT[:, c, t * 128:(t + 1) * 128], wop[s][:, c, :], c == 0, c == NCH - 1) for c in range(NCH)],
                                all_mg + [b_wop[s]], [pb[bk]])
                            P.op("dve", lambda e, bk=bk, nb=nb, k=k: e.tensor_tensor(out=tmp[k % 2][:, :], in0=psb[bk][:, 0:256], in1=gbc[:, nb * 256:(nb + 1) * 256], op=ALU.mult),
                                 [pb[bk], b_gbc[nb // 2]], [b_tmp[k % 2]])
                            P.op("dve", lambda e, t=t, nb=nb, k=k: e.tensor_tensor(out=x1[:, t, nb * 256:(nb + 1) * 256], in0=x1[:, t, nb * 256:(nb + 1) * 256],
                                                                                   in1=tmp[k % 2][:, :], op=ALU.add), [b_tmp[k % 2], b_x1[t][q]], [b_x1[t][q]])
                            k += 1
                    if "x1" in debug:
                        d = nc.dram_tensor("dbg_x1", [TOK, D], F32, kind="ExternalOutput").ap()
                        for t in range(NT):
                            fin.append(P.dma("sp", lambda e, t=t, d=d: e.dma_start(out=d[t * 128:(t + 1) * 128, :], in_=x1[:, t, :]), f"st_x1{t}", reads=b_x1[t]))
                    for t in range(NT):
                        P.dma("sp", lambda e, t=t: e.dma_start(out=x1buf[t * 128:(t + 1) * 128, :], in_=x1[:, t, :]), "x1st", reads=b_x1[t], writes=[b_x1buf[t]])
                    lg = S("lg", [128, 36], es=E3); b_lg = P.buf("lg")
                    rs = S("rs", [128, 16], es=E3); b_rs = P.bufs("rs", 16)
                    ohg = S("ohg", [128, 4], es=E3); b_ohg = P.buf("ohg")
                    egj = S("egj", [128, 4], es=E3); b_egj = P.buf("egj")
                    selm = S("selm", [128, 4, 8], es=E3); b_selm = P.buf("selm")
                    sel = S("sel", [128, 8], es=E3); sel2 = S("sel2", [128, 8], es=E3); b_sel = P.buf("sel"); b_sel2 = P.buf("sel2")
                    oh1 = S("oh1", [128, 8], es=E3); oh2 = S("oh2", [128, 8], es=E3); b_oh1 = P.buf("oh1"); b_oh2 = P.buf("oh2")
                    Mk = S("Mk", [128, NT, 2, NEXP], es=E3); b_Mk = [P.bufs(f"Mk{t}_", 2) for t in range(NT)]
                    M12 = S("M12", [128, NT, NEXP], BF16, E3); b_M12 = P.bufs("M12", NT)
                    for t in range(NT):
                        make_hT(x1[:, t, :], b_x1[t], A2, B2, b_A2, hT, b_hT[t], t, xs_slots, b_xs, junk, b_junk, t)
                        _mm(P, [(psb[4][:, 0:36], hT[:, c, t * 128:(t + 1) * 128], wr[:, c, :], c == 0, c == NCH - 1) for c in range(NCH)],
                            b_hT[t] + [b_wr], [pb[4]])
                        P.op("dve", lambda e: e.tensor_tensor(out=lg[:, :], in0=psb[4][:, 0:36], in1=b_rt[:, :], op=ALU.add), [pb[4], b_br], [b_lg])
                        P.op("dve", lambda e: e.reduce_max(out=rs[:, 0:1], in_=lg[:, 0:4], axis=AX.X), [b_lg], [b_rs[0]])
                        P.op("dve", lambda e: e.tensor_scalar(out=ohg[:, :], in0=lg[:, 0:4], scalar1=rs[:, 0:1], scalar2=None, op0=ALU.is_equal), [b_lg, b_rs[0]], [b_ohg])
                        P.op("dve", lambda e: e.tensor_scalar(out=rs[:, 1:2], in0=rs[:, 0:1], scalar1=-1.0, scalar2=None, op0=ALU.mult), [b_rs[0]], [b_rs[1]])
                        P.op("act", lambda e: e.activation(out=egj[:, :], in_=lg[:, 0:4], func=AF.Exp, bias=rs[:, 1:2], scale=1.0, accum_out=rs[:, 2:3]),
                             [b_lg, b_rs[1]], [b_egj, b_rs[2]])
                        P.op("dve", lambda e: e.reciprocal(out=rs[:, 3:4], in_=rs[:, 2:3]), [b_rs[2]], [b_rs[3]])
                        P.op("dve", lambda e: e.tensor_tensor(out=selm[:, :, :], in0=lg[:, 4:36].rearrange("p (g j) -> p g j", g=4),
                                                              in1=ohg[:, :].unsqueeze(2).to_broadcast([128, 4, 8]), op=ALU.mult), [b_lg, b_ohg], [b_selm])
                        P.op("dve", lambda e: e.tensor_reduce(out=sel[:, :], in_=selm[:, :, :].rearrange("p g j -> p j g"), axis=AX.X, op=ALU.add), [b_selm], [b_sel])
                        P.op("dve", lambda e: e.reduce_max(out=rs[:, 4:5], in_=sel[:, :], axis=AX.X), [b_sel], [b_rs[4]])
                        P.op("dve", lambda e: e.tensor_scalar(out=oh1[:, :], in0=sel[:, :], scalar1=rs[:, 4:5], scalar2=None, op0=ALU.is_equal), [b_sel, b_rs[4]], [b_oh1])
                        P.op("dve", lambda e: e.scalar_tensor_tensor(out=sel2[:, :], in0=oh1[:, :], scalar=-1e30, in1=sel[:, :], op0=ALU.mult, op1=ALU.add),
                             [b_oh1, b_sel], [b_sel2])
                        P.op("dve", lambda e: e.reduce_max(out=rs[:, 5:6], in_=sel2[:, :], axis=AX.X), [b_sel2], [b_rs[5]])
                        P.op("dve", lambda e: e.tensor_scalar(out=oh2[:, :], in0=sel2[:, :], scalar1=rs[:, 5:6], scalar2=None, op0=ALU.is_equal), [b_sel2, b_rs[5]], [b_oh2])
                        P.op("dve", lambda e: e.tensor_tensor(out=rs[:, 6:7], in0=rs[:, 5:6], in1=rs[:, 4:5], op=ALU.subtract), [b_rs[4], b_rs[5]], [b_rs[6]])
                        P.op("act", lambda e: e.activation(out=rs[:, 7:8], in_=rs[:, 6:7], func=AF.Exp), [b_rs[6]], [b_rs[7]])
                        P.op("dve", lambda e: e.tensor_scalar(out=rs[:, 8:9], in0=rs[:, 7:8], scalar1=1.0, scalar2=None, op0=ALU.add), [b_rs[7]], [b_rs[8]])
                        P.op("dve", lambda e: e.reciprocal(out=rs[:, 9:10], in_=rs[:, 8:9]), [b_rs[8]], [b_rs[9]])
                        P.op("dve", lambda e: e.tensor_tensor(out=rs[:, 10:11], in0=rs[:, 9:10], in1=rs[:, 3:4], op=ALU.mult), [b_rs[9], b_rs[3]], [b_rs[10]])
                        P.op("dve", lambda e: e.tensor_tensor(out=rs[:, 11:12], in0=rs[:, 10:11], in1=rs[:, 7:8], op=ALU.mult), [b_rs[10], b_rs[7]], [b_rs[11]])
                        P.op("act", lambda e, t=t: e.copy(out=cc[:, t, :], in_=rs[:, 10:12]), [b_rs[10], b_rs[11]], [b_cc[t]])
                        P.op("dve", lambda e, t=t: e.tensor_tensor(out=Mk[:, t, 0, :].rearrange("p (g j) -> p g j", g=4),
                                                                   in0=ohg[:, :].unsqueeze(2).to_broadcast([128, 4, 8]),
                                                                   in1=oh1[:, :].unsqueeze(1).to_broadcast([128, 4, 8]), op=ALU.mult), [b_ohg, b_oh1], [b_Mk[t][0]])
                        P.op("dve", lambda e, t=t: e.tensor_tensor(out=Mk[:, t, 1, :].rearrange("p (g j) -> p g j", g=4),
                                                                   in0=ohg[:, :].unsqueeze(2).to_broadcast([128, 4, 8]),
                                                                   in1=oh2[:, :].unsqueeze(1).to_broadcast([128, 4, 8]), op=ALU.mult), [b_ohg, b_oh2], [b_Mk[t][1]])
                        P.op("dve", lambda e, t=t: e.tensor_tensor(out=M12[:, t, :], in0=Mk[:, t, 0, :], in1=Mk[:, t, 1, :], op=ALU.add), b_Mk[t], [b_M12[t]])
                    rank = S("rank", [128, NT, NEXP], es=E3); b_rank = P.buf("rank")
                    cnt = S("cnt", [128, NEXP], es=E3); b_cnt = P.buf("cnt")
                    nblk = S("nblk", [128, NEXP], es=E3); b_nblk = P.buf("nblk")
                    sa = S("sa", [128, NEXP], es=E3); sb_ = S("sb", [128, NEXP], es=E3); b_sa = P.buf("sa"); b_sb = P.buf("sb")
                    pst = S("pst", [128, NEXP], es=E3); b_pst = P.buf("pst")
                    base = S("base", [128, NT, NEXP], es=E3); b_base = P.buf("base")
                    prod = S("prod", [128, NT, NEXP], es=E3); b_prod = P.buf("prod")
                    destf = [S(f"destf{k}", [128, NT], es=E3) for k in range(2)]; b_destf = P.bufs("destf", 2)
                    cmp_ = S("cmp", [128, 48, NEXP], es=E3); b_cmp = P.buf("cmp")
                    blke = S("blke", [128, 48], es=E3); b_blke = P.buf("blke")
                    valid = S("valid", [128, 48], es=E3); b_valid = P.buf("valid")
                    tmpi = S("tmpi", [128, 48], es=E3); b_tmpi = P.buf("tmpi")
                    idxf = S("idxf", [128, 48, 8], es=E3); b_idxf = P.buf("idxf")
                    items = []
                    for t in range(NT):
                        for t2 in range(t):
                            items.append((psb[2][:, t * 32:(t + 1) * 32], ones_bf[:, :], M12[:, t2, :], t2 == 0, False))
                        items.append((psb[2][:, t * 32:(t + 1) * 32], ltri[:, :], M12[:, t, :], t == 0, True))
                    _mm(P, items, b_M12 + [b_ones, b_ltri], [pb[2]])
                    P.op("dve", lambda e: e.tensor_copy(out=rank[:, :, :].rearrange("p t e -> p (t e)"), in_=psb[2][:, 0:256]), [pb[2]], [b_rank])
                    _mm(P, [(psb[3][:, 0:32], ones_bf[:, :], M12[:, t, :], t == 0, t == NT - 1) for t in range(NT)], b_M12 + [b_ones], [pb[3]])
                    P.op("dve", lambda e: e.tensor_copy(out=cnt[:, :], in_=psb[3][:, 0:32]), [pb[3]], [b_cnt])
                    P.op("dve", lambda e: e.tensor_scalar(out=nblk[:, :], in0=cnt[:, :], scalar1=0.0, scalar2=None, op0=ALU.is_gt), [b_cnt], [b_nblk])
                    for kk in range(1, 8):
                        P.op("dve", lambda e, kk=kk: e.scalar_tensor_tensor(out=nblk[:, :], in0=cnt[:, :], scalar=128.0 * kk, in1=nblk[:, :],
                                                                            op0=ALU.is_gt, op1=ALU.add), [b_cnt, b_nblk], [b_nblk])
                    P.op("dve", lambda e: e.tensor_copy(out=sa[:, :], in_=nblk[:, :]), [b_nblk], [b_sa])
                    cur, nxt, bc_, bn_ = sa, sb_, b_sa, b_sb
                    for sh in (1, 2, 4, 8, 16):
                        P.op("dve", lambda e, cur=cur, nxt=nxt, sh=sh: e.tensor_copy(out=nxt[:, 0:sh], in_=cur[:, 0:sh]), [bc_], [bn_])
                        P.op("dve", lambda e, cur=cur, nxt=nxt, sh=sh: e.tensor_tensor(out=nxt[:, sh:NEXP], in0=cur[:, sh:NEXP], in1=cur[:, 0:NEXP - sh], op=ALU.add),
                             [bc_, bn_], [bn_])
                        cur, nxt, bc_, bn_ = nxt, cur, bn_, bc_
                    incl, b_incl = cur, bc_
                    P.op("dve", lambda e: e.tensor_tensor(out=pst[:, :], in0=incl[:, :], in1=nblk[:, :], op=ALU.subtract), [b_incl, b_nblk], [b_pst])
                    P.op("dve", lambda e: e.tensor_scalar(out=pst[:, :], in0=pst[:, :], scalar1=128.0, scalar2=None, op0=ALU.mult), [b_pst], [b_pst])
                    P.op("dve", lambda e: e.tensor_tensor(out=base[:, :, :], in0=rank[:, :, :], in1=pst[:, :].unsqueeze(1).to_broadcast([128, NT, NEXP]), op=ALU.add),
                         [b_rank, b_pst], [b_base])
                    for kq in range(2):
                        P.op("dve", lambda e, kq=kq: e.tensor_tensor(out=prod[:, :, :], in0=Mk[:, :, kq, :], in1=base[:, :, :], op=ALU.mult),
                             [b_base] + [b_Mk[t][kq] for t in range(NT)], [b_prod])
                        P.op("dve", lambda e, kq=kq: e.tensor_reduce(out=destf[kq][:, :], in_=prod[:, :, :], axis=AX.X, op=ALU.add), [b_prod], [b_destf[kq]])
                        P.op("dve", lambda e, kq=kq: e.tensor_copy(out=desti[kq][:, :], in_=destf[kq][:, :]), [b_destf[kq]], [b_desti])
                    P.op("dve", lambda e: e.tensor_tensor(out=cmp_[:, :, :], in0=incl[:, :].unsqueeze(1).to_broadcast([128, 48, NEXP]),
                                                          in1=jrow[:, :].unsqueeze(2).to_broadcast([128, 48, NEXP]), op=ALU.is_le), [b_incl, b_jrow], [b_cmp])
                    P.op("dve", lambda e: e.tensor_reduce(out=blke[:, :], in_=cmp_[:, :, :], axis=AX.X, op=ALU.add), [b_cmp], [b_blke])
                    P.op("dve", lambda e: e.tensor_scalar(out=blke[:, :], in0=blke[:, :], scalar1=31.0, scalar2=None, op0=ALU.min), [b_blke], [b_blke])
                    P.op("dve", lambda e: e.tensor_scalar(out=valid[:, :], in0=jrow[:, :], scalar1=incl[:, NEXP - 1:NEXP], scalar2=None, op0=ALU.is_lt),
                         [b_jrow, b_incl], [b_valid])
                    P.op("dve", lambda e: e.tensor_scalar(out=valid[:, :], in0=valid[:, :], scalar1=-1.0e6, scalar2=1.0e6, op0=ALU.mult, op1=ALU.add), [b_valid], [b_valid])
                    P.op("dve", lambda e: e.scalar_tensor_tensor(out=tmpi[:, :], in0=blke[:, :], scalar=1024.0, in1=valid[:, :], op0=ALU.mult, op1=ALU.add),
                         [b_blke, b_valid], [b_tmpi])
                    P.op("dve", lambda e: e.tensor_scalar(out=tmpi[:, :], in0=tmpi[:, :], scalar1=p8[:, 0:1], scalar2=None, op0=ALU.add), [b_tmpi, b_p8], [b_tmpi])
                    P.op("dve", lambda e: e.tensor_tensor(out=idxf[:, :, :], in0=tmpi[:, :].unsqueeze(2).to_broadcast([128, 48, 8]),
                                                          in1=j8[:, :].unsqueeze(1).to_broadcast([128, 48, 8]), op=ALU.add), [b_tmpi, b_j8], [b_idxf])
                    P.op("dve", lambda e: e.tensor_copy(out=idxw[:, :], in_=idxf[:, :, :].rearrange("p a b -> p (a b)")), [b_idxf], [b_idxw])
                    if "route" in debug:
                        dump("destf0", destf[0][:, :], [b_destf[0]]); dump("destf1", destf[1][:, :], [b_destf[1]])
                        dump("blke", blke[:, :], [b_blke]); dump("idxf", idxf[:, :, :].rearrange("p a b -> p (a b)"), [b_idxf])
                    P.barrier()
                _phase_E3()
        all_h2 = [b for t in range(NT) for b in b_hT[t]]
        stop("stop3")

        NBLK = 48
        with ExitStack() as E4:
            def _phase_E4():
                zt = S("zt", [128, D], BF16, E4); b_zt = P.buf("zt")
                P.op("dve", lambda e: e.memset(zt[:, :], 0.0), [], [b_zt])
                b_xz = P.bufs("xz", NBLK)
                for j in range(NBLK):
                    P.dma("sp", lambda e, j=j: e.dma_start(out=xbuf[j * 128:(j + 1) * 128, :], in_=zt[:, :]), "xz", reads=[b_zt], writes=[b_xz[j]])
                h2tm = [S(f"h2tm{i}", [128, D], BF16, E4) for i in range(2)]; b_h2tm = P.bufs("h2tm", 2)
                b_xsc = P.bufs("xsc", 2 * NT)
                for t in range(NT):
                    s = t % 2
                    for g in range(4):
                        _mm(P, [(psb[g][:, j * 128:(j + 1) * 128], hT[:, 4 * g + j, t * 128:(t + 1) * 128], ident_bf[:, :], True, True) for j in range(4)],
                            b_hT[t] + [b_identb], [pb[g]])
                        if g % 2 == 0:
                            P.op("act", lambda e, g=g, s=s: e.copy(out=h2tm[s][:, g * 512:(g + 1) * 512], in_=psb[g][:, :]), [pb[g]], [b_h2tm[s]])
                        else:
                            P.op("dve", lambda e, g=g, s=s: e.tensor_copy(out=h2tm[s][:, g * 512:(g + 1) * 512], in_=psb[g][:, :]), [pb[g]], [b_h2tm[s]])
                    for kq in range(2):
                        P.dma("pool", lambda e, t=t, s=s, kq=kq: e.indirect_dma_start(
                            out=xbuf[:, :], out_offset=bass.IndirectOffsetOnAxis(ap=desti[kq][:, t:t + 1], axis=0),
                            in_=h2tm[s][:, :], in_offset=None, bounds_check=breg(e, NBLK * 128 - 1), oob_is_err=False),
                            "xsc", reads=[b_h2tm[s], b_desti] + b_xz, writes=[b_xsc[2 * t + kq]])
                P.barrier()
                wgp = hT
                wup = S("wup", [128, NCH, 1024], BF16, E4)
                b_wg = P.bufs("wg", 8); b_wu = P.bufs("wu", 8); b_wd = P.bufs("wd", 8)
                xb = [S(f"xb{i}", [128, D], BF16, E4) for i in range(2)]; b_xb = P.bufs("xb", 2)
                xTb = [S(f"xTb{i}", [128, NCH, 128], BF16, E4) for i in range(2)]; b_xTb = [P.bufs(f"xTb{i}_", 4) for i in range(2)]
                sg = [S(f"sg{i}", [128, 512], es=E4) for i in range(2)]; b_sg = P.bufs("sg", 2)
                hb = [S(f"hb{i}", [128, 1024], BF16, E4) for i in range(2)]; b_hb = [P.bufs(f"hb{i}_", 2) for i in range(2)]
                hTb = [S(f"hTb{i}", [128, 8, 128], BF16, E4) for i in range(2)]; b_hTb = [P.bufs(f"hTb{i}_", 2) for i in range(2)]
                yb = [S(f"yb{i}", [128, D], es=E4) for i in range(2)]; b_yb = [P.bufs(f"yb{i}_", 4) for i in range(2)]
                b_yst = P.bufs("yst", NBLK)

                def block(j):
                    s = j % 2
                    P.dma("sp", lambda e: e.dma_start(out=xb[s][:, :], in_=xbuf[j * 128:(j + 1) * 128, :]), f"xb{s}", reads=b_xsc + b_xz, writes=[b_xb[s]])
                    for jj in range(8):
                        P.dma("pool", lambda e, jj=jj: e.indirect_dma_start(
                            out=wgp[:, 2 * jj:2 * jj + 2, :].rearrange("p a n -> p (a n)"), out_offset=None, in_=wg_r[:, :],
                            in_offset=bass.IndirectOffsetOnAxis(ap=idxw[:, j * 8 + jj:j * 8 + jj + 1], axis=0), bounds_check=breg(e, NEXP * 1024 - 1), oob_is_err=False),
                            "wg", reads=[b_idxw], writes=[b_wg[jj]])
                    for jj in range(8):
                        P.dma("pool", lambda e, jj=jj: e.indirect_dma_start(
                            out=wup[:, 2 * jj:2 * jj + 2, :].rearrange("p a n -> p (a n)"), out_offset=None, in_=wu_r[:, :],
                            in_offset=bass.IndirectOffsetOnAxis(ap=idxw[:, j * 8 + jj:j * 8 + jj + 1], axis=0), bounds_check=breg(e, NEXP * 1024 - 1), oob_is_err=False),
                            "wu", reads=[b_idxw], writes=[b_wu[jj]])
                    for jj in range(8):
                        P.dma("pool", lambda e, jj=jj: e.indirect_dma_start(
                            out=mergedT[:, 2 * jj:2 * jj + 2, :].rearrange("p a n -> p (a n)"), out_offset=None, in_=wd_r[:, :],
                            in_offset=bass.IndirectOffsetOnAxis(ap=idxw[:, j * 8 + jj:j * 8 + jj + 1], axis=0), bounds_check=breg(e, NEXP * 1024 - 1), oob_is_err=False),
                            "wd", reads=[b_idxw], writes=[b_wd[jj]])
                    for g in range(4):
                        _mm(P, [(psb[g][:, q * 128:(q + 1) * 128], xb[s][:, (4 * g + q) * 128:(4 * g + q + 1) * 128], ident_bf[:, :], True, True) for q in range(4)],
                            [b_xb[s], b_identb], [pb[g]])
                        if g % 2 == 0:
                            P.op("act", lambda e, g=g: e.copy(out=xTb[s][:, 4 * g:4 * g + 4, :].rearrange("p a n -> p (a n)"), in_=psb[g][:, :]), [pb[g]], [b_xTb[s][g]])
                        else:
                            P.op("dve", lambda e, g=g: e.tensor_copy(out=xTb[s][:, 4 * g:4 * g + 4, :].rearrange("p a n -> p (a n)"), in_=psb[g][:, :]), [pb[g]], [b_xTb[s][g]])
                    for fh in range(2):
                        bg = 4 + fh
                        bu = 6 + fh
                        _mm(P, [(psb[bg][:, :], xTb[s][:, c, :], wgp[:, c, fh * 512:(fh + 1) * 512], c == 0, c == NCH - 1) for c in range(NCH)],
                            b_xTb[s] + b_wg, [pb[bg]])
                        _mm(P, [(psb[bu][:, :], xTb[s][:, c, :], wup[:, c, fh * 512:(fh + 1) * 512], c == 0, c == NCH - 1) for c in range(NCH)],
                            b_xTb[s] + b_wu, [pb[bu]])
                        P.op("act", lambda e, fh=fh, bg=bg: e.activation(out=sg[fh][:, :], in_=psb[bg][:, :], func=AF.Silu), [pb[bg]], [b_sg[fh]])
                        P.op("dve", lambda e, fh=fh, bu=bu: e.tensor_tensor(out=hb[s][:, fh * 512:(fh + 1) * 512], in0=sg[fh][:, :], in1=psb[bu][:, :], op=ALU.mult),
                             [pb[bu], b_sg[fh]], [b_hb[s][fh]])
                    for g in range(2):
                        bk = 4 + g
                        _mm(P, [(psb[bk][:, q * 128:(q + 1) * 128], hb[s][:, (4 * g + q) * 128:(4 * g + q + 1) * 128], ident_bf[:, :], True, True) for q in range(4)],
                            b_hb[s] + [b_identb], [pb[bk]])
                        if g == 0:
                            P.op("act", lambda e, g=g, bk=bk: e.copy(out=hTb[s][:, 4 * g:4 * g + 4, :].rearrange("p a n -> p (a n)"), in_=psb[bk][:, :]), [pb[bk]], [b_hTb[s][g]])
                        else:
                            P.op("dve", lambda e, g=g, bk=bk: e.tensor_copy(out=hTb[s][:, 4 * g:4 * g + 4, :].rearrange("p a n -> p (a n)"), in_=psb[bk][:, :]), [pb[bk]], [b_hTb[s][g]])
                    for nb in range(4):
                        bk = (6, 7, 0, 1)[nb]
                        _mm(P, [(psb[bk][:, :], hTb[s][:, fc, :], mergedT[:, 2 * fc + nb // 2, (nb % 2) * 512:(nb % 2) * 512 + 512], fc == 0, fc == 7) for fc in range(8)],
                            b_hTb[s] + b_wd, [pb[bk]])
                        if nb % 2 == 0:
                            P.op("act", lambda e, nb=nb, bk=bk: e.copy(out=yb[s][:, nb * 512:(nb + 1) * 512], in_=psb[bk][:, :]), [pb[bk]], [b_yb[s][nb]])
                        else:
                            P.op("dve", lambda e, nb=nb, bk=bk: e.tensor_copy(out=yb[s][:, nb * 512:(nb + 1) * 512], in_=psb[bk][:, :]), [pb[bk]], [b_yb[s][nb]])
                    P.dma("sp", lambda e: e.dma_start(out=ybuf[j * 128:(j + 1) * 128, :], in_=yb[s][:, :]), f"yst{s}", reads=b_yb[s], writes=[b_yst[j]])

                if "nomoe" not in debug:
                    for j in range(NBLK):
                        block(j)
                P.barrier()
                return b_yst
            b_yst = _phase_E4()

        with ExitStack() as E5:
            def _phase_E5():
                nfb = S("nfb", [128, D], es=E5); b_nfb = P.buf("nfb")
                P.dma("sp", lambda e: e.dma_start(out=nfb[:, :], in_=nf_d[:, :]), "nfb", writes=[b_nfb])
                x1t = [S(f"x1t{i}", [128, D], es=E5) for i in range(2)]; b_x1t = P.bufs("x1t", 2)
                y1 = [S(f"y1_{i}", [128, D], es=E5) for i in range(2)]; b_y1 = P.bufs("y1", 2)
                y2 = [S(f"y2_{i}", [128, D], es=E5) for i in range(2)]; b_y2 = P.bufs("y2", 2)
                acc = [S(f"acc{i}", [128, D], es=E5) for i in range(2)]; b_acc = P.bufs("acc", 2)
                ot = [S(f"ot{i}", [128, D], es=E5) for i in range(2)]; b_ot = P.bufs("ot", 2)
                junk = S("junk", [128, D], BF16, E5); b_junk = P.buf("junk")
                for t in range(NT):
                    s = t % 2
                    P.dma("sp", lambda e, t=t, s=s: e.dma_start(out=x1t[s][:, :], in_=x1buf[t * 128:(t + 1) * 128, :]), f"x1t{s}", reads=[b_x1buf[t]], writes=[b_x1t[s]])
                    if "nomoe" not in debug:
                        P.dma("pool", lambda e, t=t, s=s: e.indirect_dma_start(
                            out=y1[s][:, :], out_offset=None, in_=ybuf[:, :],
                            in_offset=bass.IndirectOffsetOnAxis(ap=desti[0][:, t:t + 1], axis=0)), f"y1_{s}", reads=b_yst + [b_desti], writes=[b_y1[s]])
                        P.dma("pool", lambda e, t=t, s=s: e.indirect_dma_start(
                            out=y2[s][:, :], out_offset=None, in_=ybuf[:, :],
                            in_offset=bass.IndirectOffsetOnAxis(ap=desti[1][:, t:t + 1], axis=0)), f"y2_{s}", reads=b_yst + [b_desti], writes=[b_y2[s]])
                        P.op("act", lambda e, t=t, s=s: e.activation(out=acc[s][:, :], in_=y1[s][:, :], func=AF.Copy, scale=cc[:, t, 0:1]), [b_y1[s], b_cc[t]], [b_acc[s]])
                        P.op("dve", lambda e, t=t, s=s: e.scalar_tensor_tensor(out=acc[s][:, :], in0=y2[s][:, :], scalar=cc[:, t, 1:2], in1=acc[s][:, :],
                                                                               op0=ALU.mult, op1=ALU.add), [b_y2[s], b_cc[t], b_acc[s]], [b_acc[s]])
                        P.op("dve", lambda e, s=s: e.tensor_tensor(out=acc[s][:, :], in0=acc[s][:, :], in1=gbc[:, D:2 * D], op=ALU.mult), [b_acc[s]] + b_gbc[4:8], [b_acc[s]])
                        P.op("dve", lambda e, s=s: e.tensor_tensor(out=x1t[s][:, :], in0=x1t[s][:, :], in1=acc[s][:, :], op=ALU.add), [b_acc[s], b_x1t[s]], [b_x1t[s]])
                    P.op("act", lambda e, s=s: e.activation(out=junk[:, :], in_=x1t[s][:, :], func=AF.Square, accum_out=sst[:, s:s + 1]),
                         [b_x1t[s]], [b_junk, b_ss[s]])
                    P.op("dve", lambda e, s=s: e.tensor_scalar(out=sst[:, 2 + s:3 + s], in0=sst[:, s:s + 1], scalar1=1.0 / D, scalar2=EPS,
                                                               op0=ALU.mult, op1=ALU.add), [b_ss[s]], [b_t1[s]])
                    P.op("act", lambda e, s=s: e.activation(out=sst[:, 2 + s:3 + s], in_=sst[:, 2 + s:3 + s], func=AF.Sqrt), [b_t1[s]], [b_t1[s]])
                    P.op("dve", lambda e, s=s: e.reciprocal(out=rstd[:, s:s + 1], in_=sst[:, 2 + s:3 + s]), [b_t1[s]], [b_rstd[s]])
                    P.op("dve", lambda e, s=s: e.scalar_tensor_tensor(out=ot[s][:, :], in0=x1t[s][:, :], scalar=rstd[:, s:s + 1], in1=nfb[:, :],
                                                                      op0=ALU.mult, op1=ALU.mult), [b_x1t[s], b_rstd[s], b_nfb], [b_ot[s]])
                    fin.append(P.dma("sp", lambda e, t=t, s=s: e.dma_start(out=y_out[t * 128:(t + 1) * 128, :], in_=ot[s][:, :]), f"ot{s}", reads=[b_ot[s]]))
                P.emit(final_wait_ops=fin)
            _phase_E5()
    return nc


def _const_tables(half):
    inv = (np.float32(10000.0) ** (-np.arange(0, 128, 2, dtype=np.float32) / np.float32(128))).astype(np.float32)

    def tabs(pos0):
        pos = (pos0 + np.arange(TOK, dtype=np.float32)).astype(np.float32)
        ang = (pos[:, None] * inv[None, :]).astype(np.float32)
        cos = np.cos(ang).astype(np.float32); sin = np.sin(ang).astype(np.float32)
        cs = np.concatenate([cos, cos], axis=1).reshape(NT, 128, 128).transpose(1, 0, 2)
        sn = np.concatenate([sin, sin], axis=1).reshape(NT, 128, 128).transpose(1, 0, 2)
        return np.ascontiguousarray(cs), np.ascontiguousarray(sn)

    cs_own, sn_own = tabs(np.float32(half * TOK))
    cs_prev, sn_prev = tabs(np.float32(0))
    log_g = np.log1p(-(2.0 ** (-5.0 - np.arange(8, dtype=np.float32)))).astype(np.float32)
    i = np.arange(128, dtype=np.float32)
    scale = np.float32(128.0 ** -0.5)
    diff = i[None, :] - i[:, None]
    maskT = np.where(diff[None] >= 0, np.exp(log_g[:, None, None] * np.maximum(diff[None], 0.0)), 0.0).astype(np.float32) * scale
    maskT = np.ascontiguousarray(maskT.transpose(1, 0, 2))
    qd = np.exp(log_g[:, None] * (i + 1.0)).astype(np.float32)
    qdec = np.ascontiguousarray(np.broadcast_to(qd[None], (128, 8, 128))).astype(np.float32)
    kdec = np.ascontiguousarray((np.exp(log_g[:, None] * (127.0 - i)).astype(np.float32) * scale).T)
    return dict(cs_own=cs_own, sn_own=sn_own, cs_prev=cs_prev, sn_prev=sn_prev, maskT=maskT, qdec=qdec, kdec=kdec)


_PROG_CACHE = {}


def _make_in_maps(inputs):
    f = lambda a: np.ascontiguousarray(np.asarray(a, dtype=np.float32))
    x = f(inputs["x"]); c = f(inputs["c"])
    b_ada = f(inputs["b_ada"])[0]
    shared = dict(
        w_ada=f(inputs["w_ada"])[0],
        b_adaT=np.ascontiguousarray(b_ada.reshape(96, 128).T),
        b_adag=np.ascontiguousarray(np.concatenate([b_ada[2 * D:3 * D], b_ada[5 * D:6 * D]])[None, :]),
        n1T=np.ascontiguousarray(f(inputs["norm1_g"])[0].reshape(NCH, 128).T),
        n2T=np.ascontiguousarray(f(inputs["norm2_g"])[0].reshape(NCH, 128).T),
        nf_bc=np.ascontiguousarray(np.broadcast_to(f(inputs["norm_f_g"])[None, :], (128, D))),
        w_in=f(inputs["w_in"])[0],
        conv_wT=np.ascontiguousarray(f(inputs["conv_w"])[0].reshape(3, 8, 128).transpose(2, 1, 0)),
        w_conv_out=f(inputs["w_conv_out"])[0], w_ret_out=f(inputs["w_ret_out"])[0], w_o=f(inputs["w_o"])[0],
        w_r=np.ascontiguousarray(np.concatenate([f(inputs["w_router_group"])[0], f(inputs["w_router_expert"])[0]], axis=1)),
        b_r=np.ascontiguousarray(np.broadcast_to(np.concatenate([f(inputs["b_router_group"])[0], f(inputs["b_router_expert"])[0]])[None, :], (128, 36))),
        wg_r=np.ascontiguousarray(f(inputs["w_gate"])[0].reshape(NEXP, 8, 2, 128, 1024).transpose(0, 3, 1, 2, 4)).reshape(NEXP * 1024, D),
        wu_r=np.ascontiguousarray(f(inputs["w_up"])[0].reshape(NEXP, 8, 2, 128, 1024).transpose(0, 3, 1, 2, 4)).reshape(NEXP * 1024, D),
        wd_r=np.ascontiguousarray(f(inputs["w_down"])[0].reshape(NEXP, 8, 128, D).transpose(0, 2, 1, 3)).reshape(NEXP * 1024, D),
        ident=np.eye(128, dtype=np.float32),
        ltri=np.triu(np.ones((128, 128), dtype=np.float32), k=1),
        jrow=np.ascontiguousarray(np.broadcast_to(np.arange(48, dtype=np.float32)[None, :], (128, 48))),
        p8=(np.arange(128, dtype=np.float32) * 8.0)[:, None].copy(),
        j8=np.ascontiguousarray(np.broadcast_to(np.arange(8, dtype=np.float32)[None, :], (128, 8))),
    )
    tabs = [_const_tables(0), _const_tables(1)]
    in_maps = []
    for core in range(NCORES):
        b, half = core // 2, core % 2
        m = dict(shared)
        m.update(tabs[half])
        m["x_own"] = np.ascontiguousarray(x[b, half * TOK:(half + 1) * TOK])
        m["x_prev"] = np.ascontiguousarray(x[b, 0:TOK])
        m["cT"] = np.ascontiguousarray(c[b].reshape(NCH, 128).T)
        m["flag"] = np.full((128, 1), float(half), dtype=np.float32)
        in_maps.append(m)
    return in_maps


def kernel(**inputs):
    if "prog" not in _PROG_CACHE:
        _PROG_CACHE["prog"] = build_program()
    nc = _PROG_CACHE["prog"]
    in_maps = _make_in_maps(inputs)
    res = run_bass_kernel_spmd(nc, in_maps, core_ids=list(range(NCORES)))
    out = np.empty((4, 2048, D), dtype=np.float32)
    for core in range(NCORES):
        b, half = core // 2, core % 2
        out[b, half * TOK:(half + 1) * TOK] = res.results[core]["y"]
    return out
```

```python
import os
from contextlib import ExitStack
import numpy as np
import concourse.bass as bass
import concourse.mybir as mybir
from concourse.bass_utils import run_bass_kernel_spmd

F32 = mybir.dt.float32
BF16 = mybir.dt.bfloat16
ALU = mybir.AluOpType
AF = mybir.ActivationFunctionType
AX = mybir.AxisListType

D = 2048
NCORES = 8
TOK = 1024
NT = 8
NCH = 16
EPS = 1e-6
NEXP = 32
ENGS = ("pe", "act", "dve", "pool", "sp")


class Buf:
    __slots__ = ("name", "writer", "readers", "psum")

    def __init__(self, name):
        self.name = name
        self.writer = None
        self.readers = []
        self.psum = False


class Op:
    __slots__ = ("eng", "fn", "deps", "signal", "token", "is_dma")

    def __init__(self, eng, fn, is_dma):
        self.eng = eng
        self.fn = fn
        self.deps = []
        self.signal = False
        self.token = None
        self.is_dma = is_dma


class Prog:
    def __init__(self, nc):
        self.nc = nc
        self.streams = {e: [] for e in ENGS}
        self.dma_sems = {}
        self.last_dma = {}
        self.wait_all = set()

    def buf(self, name):
        return Buf(name)

    def bufs(self, name, n):
        return [Buf(f"{name}{i}") for i in range(n)]

    def _add(self, op, reads, writes):
        deps = []
        for b in reads:
            if b.writer is not None:
                deps.append(b.writer)
            if b.psum:
                deps.extend(r for r in b.readers if r.eng != op.eng)
        for b in writes:
            if b.writer is not None:
                deps.append(b.writer)
            deps.extend(b.readers)
        seen = set()
        for d in deps:
            if d is op or id(d) in seen:
                continue
            seen.add(id(d))
            if d.eng == "pe" and op.eng == "pe" and not d.is_dma and not op.is_dma:
                continue
            if d.is_dma:
                sname = d.token[0]
                cur = self.dma_sems[sname][1]
                if op.is_dma and op.token[0] == sname:
                    cur -= 16
                op.deps.append((sname, cur))
            else:
                op.deps.append(d)
                d.signal = True
        for b in reads:
            b.readers.append(op)
        for b in writes:
            b.writer = op
            b.readers = []
        self.streams[op.eng].append(op)
        return op

    def op(self, eng, fn, reads=(), writes=()):
        return self._add(Op(eng, fn, False), reads, writes)

    def dma(self, eng, fn, sem, reads=(), writes=()):
        op = Op(eng, fn, True)
        ent = self.dma_sems.setdefault(sem, [None, 0])
        ent[1] += 16
        op.token = (sem, ent[1])
        self.last_dma[sem] = op
        return self._add(op, reads, writes)

    def barrier(self):
        lasts = []
        for e in ENGS:
            for op in reversed(self.streams[e]):
                if not op.is_dma and op.fn is not None:
                    lasts.append(op)
                    break
        lasts.extend(self.last_dma.values())
        for e in ENGS:
            op = Op(e, None, False)
            for d in lasts:
                if d.is_dma:
                    op.deps.append((d.token[0], self.dma_sems[d.token[0]][1]))
                    continue
                if d.eng == e:
                    continue
                op.deps.append(d)
                d.signal = True
            self.streams[e].append(op)

    def _tok(self, d):
        if isinstance(d, tuple):
            s_, v = d
            if s_ in self.wait_all:
                v = self.dma_sems[s_][1]
            return s_, v
        return d.token

    def simulate(self):
        cnt = {e: 0 for e in ENGS}
        for e in ENGS:
            c = 0
            for op in self.streams[e]:
                if op.is_dma or op.fn is None:
                    continue
                if op.signal:
                    c += 1
                    op.token = (e, c)
        sem = {}
        pos = {e: 0 for e in ENGS}
        progress = True
        while progress:
            progress = False
            for e in ENGS:
                st = self.streams[e]
                while pos[e] < len(st):
                    op = st[pos[e]]
                    ok = True
                    for d in op.deps:
                        s_, v = self._tok(d)
                        if sem.get(s_, 0) < v:
                            ok = False
                            break
                    if not ok:
                        break
                    if op.fn is not None:
                        if op.is_dma:
                            sem[op.token[0]] = sem.get(op.token[0], 0) + 16
                        elif op.signal:
                            sem[e] = sem.get(e, 0) + 1
                    pos[e] += 1
                    progress = True
        stuck = {e: (pos[e], len(self.streams[e])) for e in ENGS if pos[e] < len(self.streams[e])}
        return stuck

    def emit(self, final_wait_ops=()):
        nc = self.nc
        stuck = self.simulate()
        if stuck:
            raise RuntimeError(f"semaphore protocol deadlock: {stuck}")
        with ExitStack() as es:
            eng_sem = {e: es.enter_context(nc.semaphore(f"s_{e}")) for e in ENGS}
            for name, ent in self.dma_sems.items():
                ent[0] = es.enter_context(nc.semaphore(f"d_{name}"))
            for e in ENGS:
                cnt = 0
                for op in self.streams[e]:
                    if op.is_dma or op.fn is None:
                        continue
                    if op.signal:
                        cnt += 1
                        op.token = (e, cnt)
            block = es.enter_context(nc.Block())

            def handle(s):
                return eng_sem[s] if s in eng_sem else self.dma_sems[s][0]

            def make(e):
                def body(eng):
                    known = {}
                    for op in self.streams[e]:
                        need = {}
                        for d in op.deps:
                            s, v = self._tok(d)
                            if v > need.get(s, 0):
                                need[s] = v
                        for s, v in need.items():
                            if known.get(s, 0) >= v:
                                continue
                            known[s] = v
                            eng.wait_ge(handle(s), v)
                        if op.fn is None:
                            continue
                        ins = op.fn(eng)
                        if op.is_dma:
                            ins.then_inc(self.dma_sems[op.token[0]][0], 16)
                        elif op.signal:
                            ins.then_inc(eng_sem[e], 1)
                    if e == "sp":
                        for op in final_wait_ops:
                            s = op.token[0]
                            eng.wait_ge(handle(s), self.dma_sems[s][1])
                return body

            block.tensor(make("pe"))
            block.scalar(make("act"))
            block.vector(make("dve"))
            block.gpsimd(make("pool"))
            block.sync(make("sp"))


def _mm(P, items, reads, writes):
    def fn(e):
        ins = None
        for (o, l, r, st, sp) in items:
            ins = e.matmul(out=o, lhsT=l, rhs=r, start=st, stop=sp)
        return ins
    return P.op("pe", fn, reads, writes)


class _Stop(Exception):
    pass


def build_program(debug=None):
    debug = debug or ()
    try:
        return _build_program(debug)
    except _Stop as ex:
        return ex.args[0]


def _build_program(debug):
    nc = bass.Bass("TRN2", target_bir_lowering=False)

    def din(name, shape, dt=F32):
        return nc.dram_tensor(name, list(shape), dt, kind="ExternalInput").ap()

    x_own = din("x_own", [TOK, D]); x_prev = din("x_prev", [TOK, D])
    cT_d = din("cT", [128, NCH]); flag_d = din("flag", [128, 1])
    w_ada = din("w_ada", [D, 6 * D]); b_adaT_d = din("b_adaT", [128, 96]); b_adag_d = din("b_adag", [1, 2 * D])
    n1T_d = din("n1T", [128, NCH]); n2T_d = din("n2T", [128, NCH]); nf_d = din("nf_bc", [128, D])
    w_in = din("w_in", [D, 13312]); convw_d = din("conv_wT", [128, 8, 3])
    w_co = din("w_conv_out", [1024, D]); w_ro = din("w_ret_out", [D, D]); w_o = din("w_o", [D, D])
    w_r = din("w_r", [D, 36]); b_r_d = din("b_r", [128, 36])
    if "nomoe" not in debug:
        w_gate = din("w_gate", [NEXP, D, 1024]); w_up = din("w_up", [NEXP, D, 1024]); w_down = din("w_down", [NEXP, 1024, D])
    ident_d = din("ident", [128, 128])
    cs_own_d = din("cs_own", [128, NT, 128]); sn_own_d = din("sn_own", [128, NT, 128])
    cs_prev_d = din("cs_prev", [128, NT, 128]); sn_prev_d = din("sn_prev", [128, NT, 128])
    maskT_d = din("maskT", [128, 8, 128]); qdec_d = din("qdec", [128, 8, 128]); kdec_d = din("kdec", [128, 8])
    y_out = nc.dram_tensor("y", [TOK, D], F32, kind="ExternalOutput").ap()
    dbg_outs = {}

    log_g = np.log1p(-(2.0 ** (-5.0 - np.arange(8, dtype=np.float32)))).astype(np.float32)
    cdec = [float(np.exp(np.float32(log_g[h] * 128.0))) for h in range(8)]

    P = Prog(nc)
    fin = []

    def dump(name, ap2d, bufs, dt=F32):
        if name not in debug:
            return
        d = nc.dram_tensor("dbg_" + name, list(ap2d.shape), dt, kind="ExternalOutput").ap()
        fin.append(P.dma("sp", lambda e, d=d: e.dma_start(out=d[:, :], in_=ap2d), "st_" + name, reads=list(bufs)))

    def stop(name):
        if name in debug:
            P.emit(final_wait_ops=fin)
            raise _Stop(nc)

    with ExitStack() as G:
        _cnt = [0]

        def S(name, shape, dt=F32, es=G):
            _cnt[0] += 1
            return es.enter_context(nc.sbuf_tensor(f"{name}_{_cnt[0]}", list(shape), dt))

        psb = [G.enter_context(nc.psum_tensor(f"psb{i}", [128, 512], F32)) for i in range(8)]
        pb = P.bufs("pb", 8)
        for b_ in pb:
            b_.psum = True

        ident = S("ident", [128, 128]); b_ident = P.buf("ident")
        ones_bf = S("ones_bf", [128, 128], BF16); b_ones = P.buf("ones")
        ones_row = S("ones_row", [1, 128]); b_onesr = P.buf("onesr")
        epst = S("epst", [128, 1]); b_eps = P.buf("eps")
        cTf = S("cTf", [128, NCH]); cact = S("cact", [128, NCH], BF16); b_cT = P.buf("cT"); b_cact = P.buf("cact")
        flag = S("flag", [128, 1]); b_flag = P.buf("flag")
        b_adaT = S("b_adaT", [128, 96]); b_badaT = P.buf("badaT")
        n1T = S("n1T", [128, NCH]); n2T = S("n2T", [128, NCH]); b_n1 = P.buf("n1"); b_n2 = P.buf("n2")
        mod = S("mod", [128, 96]); b_mod = P.buf("mod")
        A1 = S("A1", [128, NCH]); A2 = S("A2", [128, NCH]); b_A1 = P.buf("A1"); b_A2 = P.buf("A2")
        gbc = S("gbc", [128, 2 * D]); b_gbc = P.bufs("gbc", 8)
        convw = S("convw", [128, 8, 3]); b_convw = P.buf("convw")
        kdec = S("kdec", [128, 8]); kdecp = S("kdecp", [128, 8]); b_kdec = P.buf("kdec"); b_kdecp = P.buf("kdecp")
        b_rt = S("b_rt", [128, 36]); b_br = P.buf("br")
        wr = S("wr", [128, NCH, 36], BF16); b_wr = P.buf("wr")
        htail = S("htail", [128, NCH, 2], BF16); b_htail = P.buf("htail")
        sst = S("sst", [128, 4]); b_ss = P.bufs("ss", 2); b_t1 = P.bufs("t1", 2)
        rstd = S("rstd", [128, 2]); b_rstd = P.bufs("rstd", 2)

        def ld(dst, src, b, sem="ld0"):
            P.wait_all.add(sem)
            P.dma("sp", lambda e: e.dma_start(out=dst, in_=src), sem, writes=[b])

        ld(ident[:, :], ident_d[:, :], b_ident)
        ld(cTf[:, :], cT_d[:, :], b_cT)
        ld(flag[:, :], flag_d[:, :], b_flag)
        ld(b_adaT[:, :], b_adaT_d[:, :], b_badaT)
        ld(n1T[:, :], n1T_d[:, :], b_n1)
        ld(n2T[:, :], n2T_d[:, :], b_n2)
        ld(convw[:, :, :], convw_d[:, :, :], b_convw)
        ld(kdec[:, :], kdec_d[:, :], b_kdec)
        ld(b_rt[:, :], b_r_d[:, :], b_br)
        P.dma("pool", lambda e: e.dma_start(out=wr[:, :, :], in_=w_r.rearrange("(kt p) n -> p kt n", p=128)), "wr", writes=[b_wr])
        P.op("dve", lambda e: e.memset(ones_bf[:, :], 1.0), [], [b_ones])
        P.op("dve", lambda e: e.memset(ones_row[:, :], 1.0), [], [b_onesr])
        P.op("dve", lambda e: e.memset(epst[:, :], EPS), [], [b_eps])
        P.op("act", lambda e: e.activation(out=cact[:, :], in_=cTf[:, :], func=AF.Silu), [b_cT], [b_cact])
        P.op("dve", lambda e: e.tensor_scalar(out=kdecp[:, :], in0=kdec[:, :], scalar1=flag[:, 0:1], scalar2=None, op0=ALU.mult),
             [b_kdec, b_flag], [b_kdecp])

        with ExitStack() as E0:
            def _phase_E0():
                slots = [S(f"ada{i}", [128, NCH, 512], BF16, E0) for i in range(3)]
                b_slots = P.bufs("ada", 3)
                grow = S("grow", [1, 2 * D], es=E0); b_grow = P.bufs("grow", 8)
                badag = S("badag", [1, 2 * D], es=E0); b_badag = P.buf("badag")
                ld(badag[:, :], b_adag_d[:, :], b_badag)
                gi = 0
                for t in range(24):
                    s = t % 3
                    seg = t // 4
                    P.dma("pool", lambda e, t=t, s=s: e.dma_start(
                        out=slots[s][:, :, :], in_=w_ada[:, 512 * t:512 * (t + 1)].rearrange("(kt p) n -> p kt n", p=128)),
                        f"ada{s}", writes=[b_slots[s]])
                    if seg in (2, 5):
                        items = [(psb[1][0:1, :], cact[:, kt:kt + 1], slots[s][:, kt, :], kt == 0, kt == NCH - 1) for kt in range(NCH)]
                        _mm(P, items, [b_cact, b_slots[s]], [pb[1]])
                        P.op("dve", lambda e, gi=gi: e.tensor_tensor(out=grow[0:1, gi * 512:(gi + 1) * 512], in0=psb[1][0:1, :],
                                                                     in1=badag[0:1, gi * 512:(gi + 1) * 512], op=ALU.add),
                             [pb[1], b_badag], [b_grow[gi]])
                        gi += 1
                    else:
                        items = []
                        for blk in range(4):
                            j = 4 * t + blk
                            for kt in range(NCH):
                                items.append((psb[0][:, j:j + 1], slots[s][:, kt, blk * 128:(blk + 1) * 128], cact[:, kt:kt + 1],
                                              kt == 0, kt == NCH - 1))
                        _mm(P, items, [b_cact, b_slots[s]], [pb[0]])
                P.op("dve", lambda e: e.tensor_tensor(out=mod[:, 0:32], in0=psb[0][:, 0:32], in1=b_adaT[:, 0:32], op=ALU.add),
                     [pb[0], b_badaT], [b_mod])
                P.op("dve", lambda e: e.tensor_tensor(out=mod[:, 48:80], in0=psb[0][:, 48:80], in1=b_adaT[:, 48:80], op=ALU.add),
                     [pb[0], b_badaT, b_mod], [b_mod])
                P.op("dve", lambda e: e.scalar_tensor_tensor(out=A1[:, :], in0=mod[:, 16:32], scalar=1.0, in1=n1T[:, :], op0=ALU.add, op1=ALU.mult),
                     [b_mod, b_n1], [b_A1])
                P.op("dve", lambda e: e.scalar_tensor_tensor(out=A2[:, :], in0=mod[:, 64:80], scalar=1.0, in1=n2T[:, :], op0=ALU.add, op1=ALU.mult),
                     [b_mod, b_n2], [b_A2])
                for gi in range(8):
                    bk = 2 + gi % 2
                    _mm(P, [(psb[bk][:, :], ones_row[0:1, :], grow[0:1, gi * 512:(gi + 1) * 512], True, True)], [b_onesr, b_grow[gi]], [pb[bk]])
                    P.op("act", lambda e, gi=gi, bk=bk: e.copy(out=gbc[:, gi * 512:(gi + 1) * 512], in_=psb[bk][:, :]), [pb[bk]], [b_gbc[gi]])
                P.barrier()
            _phase_E0()
        B1 = mod[:, 0:16]
        B2 = mod[:, 48:64]
        if "mod" in debug:
            d = nc.dram_tensor("dbg_mod", [128, 96], F32, kind="ExternalOutput").ap()
            fin.append(P.dma("sp", lambda e, d=d: e.dma_start(out=d[:, :], in_=mod[:, :]), "st_mod", reads=[b_mod]))
            d = nc.dram_tensor("dbg_gbc", [128, 2 * D], F32, kind="ExternalOutput").ap()
            fin.append(P.dma("sp", lambda e, d=d: e.dma_start(out=d[:, :], in_=gbc[:, :]), "st_gbc", reads=b_gbc))
        if "stop0" in debug:
            P.emit(final_wait_ops=fin)
            return nc

        def make_hT(src_ap, b_src, Aap, Bap, b_AB, hT, b_hT_t, t, xs_slots, b_xs, junk, b_junk, k):
            s = k % 2
            P.op("act", lambda e: e.activation(out=junk[:, :], in_=src_ap, func=AF.Square, accum_out=sst[:, s:s + 1]),
                 b_src, [b_junk, b_ss[s]])
            P.op("dve", lambda e: e.tensor_scalar(out=sst[:, 2 + s:3 + s], in0=sst[:, s:s + 1], scalar1=1.0 / D, scalar2=EPS,
                                                  op0=ALU.mult, op1=ALU.add), [b_ss[s]], [b_t1[s]])
            P.op("act", lambda e: e.activation(out=sst[:, 2 + s:3 + s], in_=sst[:, 2 + s:3 + s], func=AF.Sqrt), [b_t1[s]], [b_t1[s]])
            P.op("dve", lambda e: e.reciprocal(out=rstd[:, s:s + 1], in_=sst[:, 2 + s:3 + s]), [b_t1[s]], [b_rstd[s]])
            xs = xs_slots[s]
            P.op("act", lambda e: e.activation(out=xs[:, :], in_=src_ap, func=AF.Copy, scale=rstd[:, s:s + 1]),
                 b_src + [b_rstd[s]], [b_xs[s]])
            for g in range(4):
                bk = g % 2

                def tr(e, g=g, bk=bk):
                    ins = None
                    for j in range(4):
                        c = 4 * g + j
                        ins = e.transpose(out=psb[bk][:, j * 128:(j + 1) * 128], in_=xs[:, c * 128:(c + 1) * 128], identity=ident[:, :])
                    return ins
                P.op("pe", tr, [b_xs[s], b_ident], [pb[bk]])
                if g % 2 == 0:
                    def ev(e, g=g, bk=bk):
                        ins = None
                        for j in range(4):
                            c = 4 * g + j
                            ins = e.activation(out=hT[:, c, t * 128:(t + 1) * 128], in_=psb[bk][:, j * 128:(j + 1) * 128],
                                               func=AF.Identity, scale=Aap[:, c:c + 1], bias=Bap[:, c:c + 1])
                        return ins
                    P.op("act", ev, [pb[bk], b_AB, b_mod], [b_hT_t[g]])
                else:
                    def ev(e, g=g, bk=bk):
                        ins = None
                        for j in range(4):
                            c = 4 * g + j
                            ins = e.tensor_scalar(out=hT[:, c, t * 128:(t + 1) * 128], in0=psb[bk][:, j * 128:(j + 1) * 128],
                                                  scalar1=Aap[:, c:c + 1], scalar2=Bap[:, c:c + 1], op0=ALU.mult, op1=ALU.add)
                        return ins
                    P.op("dve", ev, [pb[bk], b_AB, b_mod], [b_hT_t[g]])

        def rotary(src_ps, cs, sn, t, m1, m2, b_m, dst, b_dst, b_src, b_tab, stg=None, b_stg=None):
            eng = "dve"
            src = src_ps
            rd = [b_src]
            if stg is not None:
                P.op("act", lambda e: e.copy(out=stg[:, :], in_=src_ps), [b_src], [b_stg])
                eng = "pool"; src = stg[:, :]; rd = [b_stg]
            P.op(eng, lambda e: e.tensor_tensor(out=m1[:, :], in0=src, in1=cs[:, t, :], op=ALU.mult), rd + b_tab, [b_m[0]])
            P.op(eng, lambda e: e.tensor_tensor(out=m2[:, :], in0=src, in1=sn[:, t, :], op=ALU.mult), rd + b_tab, [b_m[1]])
            P.op(eng, lambda e: e.tensor_tensor(out=dst[:, 0:64], in0=m1[:, 0:64], in1=m2[:, 64:128], op=ALU.subtract),
                 [b_m[0], b_m[1]], [b_dst[0]])
            P.op(eng, lambda e: e.tensor_tensor(out=dst[:, 64:128], in0=m2[:, 0:64], in1=m1[:, 64:128], op=ALU.add),
                 [b_m[0], b_m[1]], [b_dst[1]])

        hT = S("hT", [128, NCH, TOK], BF16)
        b_hT = [P.bufs(f"hT{t}_", 4) for t in range(NT)]
        mergedT = S("mergedT", [128, NCH, TOK], BF16)
        b_mg = [P.bufs(f"mg{d}_", 2) for d in range(NCH)]

        with ExitStack() as E12:
            Sst = S("Sst", [128, 8, 256], es=E12); b_S = P.bufs("S", 8)
            P.op("dve", lambda e: e.memset(Sst[:, :, :], 0.0), [], b_S)

            with ExitStack() as E1:
                def _phase_E1():
                    xt = [S(f"xt{i}", [128, D], es=E1) for i in range(2)]; b_xt = P.bufs("xt", 2)
                    xs_slots = [S(f"xs{i}", [128, D], es=E1) for i in range(2)]; b_xs = P.bufs("xs", 2)
                    junk = S("junk", [128, D], BF16, E1); b_junk = P.buf("junk")
                    csp = S("csp", [128, NT, 128], es=E1); snp = S("snp", [128, NT, 128], es=E1); b_tabp = P.bufs("tabp", 2)
                    ld(csp[:, :, :], cs_prev_d[:, :, :], b_tabp[0], "ld1")
                    ld(snp[:, :, :], sn_prev_d[:, :, :], b_tabp[1], "ld1")
                    kvp = [S(f"kvp{i}", [128, NCH, 384], BF16, E1) for i in range(2)]
                    b_kvp = [P.bufs(f"kvp{i}_", 2) for i in range(2)]
                    m1_ = [S(f"m1{i}", [128, 128], es=E1) for i in range(2)]; m2_ = [S(f"m2{i}", [128, 128], es=E1) for i in range(2)]
                    b_m_ = [P.bufs(f"m{i}_", 2) for i in range(2)]
                    krot_ = [S(f"krot{i}", [128, 128], es=E1) for i in range(2)]; b_krot_ = [P.bufs(f"krot{i}_", 2) for i in range(2)]
                    kd_ = [S(f"kd{i}", [128, 128], BF16, E1) for i in range(2)]; b_kd_ = P.bufs("kd", 2)
                    vb_ = [S(f"vb{i}", [128, 256], BF16, E1) for i in range(2)]; b_vb_ = P.bufs("vb", 2)
                    stg_ = [S(f"stg{i}", [128, 128], es=E1) for i in range(2)]; b_stg_ = P.bufs("stg", 2)
                    for t in range(NT):
                        s = t % 2
                        P.dma("sp", lambda e, t=t, s=s: e.dma_start(out=xt[s][:, :], in_=x_prev[t * 128:(t + 1) * 128, :]), f"xt{s}", writes=[b_xt[s]])
                        make_hT(xt[s][:, :], [b_xt[s]], A1, B1, b_A1, hT, b_hT[t], t, xs_slots, b_xs, junk, b_junk, t)
                    dump("hTp", hT[:, :, :].rearrange("p c t -> p (c t)"), [b for t in range(NT) for b in b_hT[t]], BF16)
                    stop("stop1a")
                    P.op("act", lambda e: e.copy(out=htail[:, :, :], in_=hT[:, :, TOK - 2:TOK]), b_hT[NT - 1], [b_htail])
                    stop("stop1b")
                    def p1A(h, t):
                        s = h % 2
                        if t == 0:
                            P.dma("pool", lambda e: e.dma_start(
                                out=kvp[s][:, :, 0:128], in_=w_in[:, 4096 + 128 * h:4096 + 128 * (h + 1)].rearrange("(kt p) n -> p kt n", p=128)),
                                f"kvp{s}", writes=[b_kvp[s][0]])
                            P.dma("pool", lambda e: e.dma_start(
                                out=kvp[s][:, :, 128:384], in_=w_in[:, 5120 + 256 * h:5120 + 256 * (h + 1)].rearrange("(kt p) n -> p kt n", p=128)),
                                f"kvp{s}", writes=[b_kvp[s][1]])
                        bk = 2 + t % 2
                        ip = t % 2
                        m1 = m1_[ip]; m2 = m2_[ip]; b_m = b_m_[ip]; krot = krot_[ip]; b_krot = b_krot_[ip]
                        kd = kd_[ip]; b_kd = b_kd_[ip]; vb = vb_[ip]; b_vb = b_vb_[ip]
                        items = [(psb[bk][:, 0:384], hT[:, c, t * 128:(t + 1) * 128], kvp[s][:, c, :], c == 0, c == NCH - 1) for c in range(NCH)]
                        _mm(P, items, b_hT[t] + b_kvp[s], [pb[bk]])
                        rotary(psb[bk][:, 0:128], csp, snp, t, m1, m2, b_m, krot, b_krot, pb[bk], b_tabp, stg_[ip], b_stg_[ip])
                        P.op("act", lambda e: e.copy(out=vb[:, :], in_=psb[bk][:, 128:384]), [pb[bk]], [b_vb])
                        P.op("act", lambda e: e.activation(out=kd[:, :], in_=krot[:, :], func=AF.Copy, scale=kdecp[:, h:h + 1]),
                             b_krot + [b_kdecp], [b_kd])

                    def p1B(h, t):
                        ip = t % 2
                        kd = kd_[ip]; b_kd = b_kd_[ip]; vb = vb_[ip]; b_vb = b_vb_[ip]
                        bk2 = 4 + t % 2
                        _mm(P, [(psb[bk2][:, 0:256], kd[:, :], vb[:, :], True, True)], [b_kd, b_vb], [pb[bk2]])
                        P.op("dve", lambda e: e.scalar_tensor_tensor(
                            out=Sst[:, h, :], in0=Sst[:, h, :], scalar=cdec[h], in1=psb[bk2][:, 0:256], op0=ALU.mult, op1=ALU.add),
                            [pb[bk2], b_S[h]], [b_S[h]])

                    seq1 = [(h, t) for h in range(8) for t in range(NT)]
                    for i in range(len(seq1) + 1):
                        if i < len(seq1):
                            p1A(*seq1[i])
                        if i >= 1:
                            p1B(*seq1[i - 1])
                    P.barrier()
                _phase_E1()

            if "stop1" in debug:
                dump("S", Sst[:, :, :].rearrange("p h e -> p (h e)"), b_S)
                stop("stop1")
            if "S" in debug:
                d = nc.dram_tensor("dbg_S", [128, 8 * 256], F32, kind="ExternalOutput").ap()
                fin.append(P.dma("sp", lambda e, d=d: e.dma_start(out=d[:, :], in_=Sst[:, :, :].rearrange("p h e -> p (h e)")), "st_S", reads=b_S))

            with ExitStack() as E2a:
                def _phase_E2a():
                    xt = [S(f"xt{i}", [128, D], es=E2a) for i in range(2)]; b_xt = P.bufs("xt", 2)
                    xs_slots = [S(f"xs{i}", [128, D], es=E2a) for i in range(2)]; b_xs = P.bufs("xs", 2)
                    junk = S("junk", [128, D], BF16, E2a); b_junk = P.buf("junk")
                    for t in range(NT):
                        s = t % 2
                        P.dma("sp", lambda e, t=t, s=s: e.dma_start(out=xt[s][:, :], in_=x_own[t * 128:(t + 1) * 128, :]), f"xt{s}", writes=[b_xt[s]])
                        make_hT(xt[s][:, :], [b_xt[s]], A1, B1, b_A1, hT, b_hT[t], t, xs_slots, b_xs, junk, b_junk, t)
                    P.barrier()
                _phase_E2a()
            all_hT = [b for t in range(NT) for b in b_hT[t]]

            if "hT" in debug:
                d = nc.dram_tensor("dbg_hT", [128, NCH * TOK], BF16, kind="ExternalOutput").ap()
                fin.append(P.dma("sp", lambda e, d=d: e.dma_start(out=d[:, :], in_=hT[:, :, :].rearrange("p c t -> p (c t)")), "st_hT", reads=all_hT))

            stop("stop2a")
            zbT = S("zbT", [128, NCH, TOK], BF16, E12); b_zb = [P.bufs(f"zb{c}_", NT) for c in range(NCH)]

            with ExitStack() as E2b:
                def _phase_E2b():
                    cso = S("cso", [128, NT, 128], es=E2b); sno = S("sno", [128, NT, 128], es=E2b); b_tabo = P.bufs("tabo", 2)
                    maskT = S("maskT", [128, 8, 128], es=E2b); qdec = S("qdec", [128, 8, 128], es=E2b); b_msk = P.buf("msk"); b_qdec = P.buf("qdecb")
                    ld(cso[:, :, :], cs_own_d[:, :, :], b_tabo[0], "ld2")
                    ld(sno[:, :, :], sn_own_d[:, :, :], b_tabo[1], "ld2")
                    ld(maskT[:, :, :], maskT_d[:, :, :], b_msk, "ld2")
                    ld(qdec[:, :, :], qdec_d[:, :, :], b_qdec, "ld2")
                    qkvp = [S(f"qkvp{i}", [128, NCH, 512], BF16, E2b) for i in range(2)]
                    b_qkvp = [P.bufs(f"qkvp{i}_", 3) for i in range(2)]
                    rgp = S("rgp", [128, NCH, 256], BF16, E2b); b_rgp = P.buf("rgp")
                    srg_ = [S(f"srg{i}", [128, 2, TOK], BF16, E2b) for i in range(2)]; b_srg_ = [P.bufs(f"srg{i}_", 4) for i in range(2)]
                    def two(name, shape, dt=F32):
                        return [S(f"{name}{i}", shape, dt, E2b) for i in range(2)]
                    mq1_ = two("mq1", [128, 128]); mq2_ = two("mq2", [128, 128]); b_mq_ = [P.bufs(f"mq{i}_", 2) for i in range(2)]
                    mk1_ = two("mk1", [128, 128]); mk2_ = two("mk2", [128, 128]); b_mk_ = [P.bufs(f"mk{i}_", 2) for i in range(2)]
                    qrot_ = two("qrot", [128, 128]); b_qrot_ = [P.bufs(f"qrot{i}_", 2) for i in range(2)]
                    krot_ = two("krot", [128, 128]); b_krot_ = [P.bufs(f"krot{i}_", 2) for i in range(2)]
                    kd_ = two("kd", [128, 128], BF16); b_kd_ = P.bufs("kd", 2)
                    stq_ = two("stq", [128, 128]); stk_ = two("stk", [128, 128]); b_stq_ = P.bufs("stq", 2); b_stk_ = P.bufs("stk", 2)
                    vb_ = two("vb", [128, 256], BF16); b_vb_ = P.bufs("vb", 2)
                    qT_ = two("qT", [128, 128], BF16); qTd_ = two("qTd", [128, 128], BF16); kT_ = two("kT", [128, 128], BF16)
                    b_qT_ = P.bufs("qT", 2); b_qTd_ = P.bufs("qTd", 2); b_kT_ = P.bufs("kT", 2)
                    AT_ = two("AT", [128, 128], BF16); b_AT_ = P.bufs("AT", 2)
                    Sb = S("Sb", [128, 256], BF16, E2b); b_Sb = P.buf("Sb")
                    sq_ = two("sq", [128, 256], BF16); b_sq_ = P.bufs("sq", 2)
                    rbc_ = two("rbc", [128, 128]); b_rbc_ = P.bufs("rbc", 2)
                    ztmp_ = two("ztmp", [128, 2, 128]); b_ztmp_ = P.bufs("ztmp", 2)

                    def chunk(h, t, s, ip, stage):
                        srg = srg_[h % 2]; b_srg = b_srg_[h % 2]
                        pT_ = psb[ip]; r_T = pb[ip]
                        bsc = 6 if ip == 0 else 4
                        pSC = psb[bsc]; r_sc = pb[bsc]
                        brt = 7 if ip == 0 else 5
                        pRET = psb[brt]; r_ret = pb[brt]
                        mq1 = mq1_[ip]; mq2 = mq2_[ip]; b_mq = b_mq_[ip]; mk1 = mk1_[ip]; mk2 = mk2_[ip]; b_mk = b_mk_[ip]
                        qrot = qrot_[ip]; b_qrot = b_qrot_[ip]; krot = krot_[ip]; b_krot = b_krot_[ip]
                        kd = kd_[ip]; b_kd = b_kd_[ip]; vb = vb_[ip]; b_vb = b_vb_[ip]
                        qT = qT_[ip]; qTd = qTd_[ip]; kT = kT_[ip]; b_qT = b_qT_[ip]; b_qTd = b_qTd_[ip]; b_kT = b_kT_[ip]
                        AT = AT_[ip]; b_AT = b_AT_[ip]; sq = sq_[ip]; b_sq = b_sq_[ip]; rbc = rbc_[ip]; b_rbc = b_rbc_[ip]
                        ztmp = ztmp_[ip]; b_ztmp = b_ztmp_[ip]
                        bk = 2 + t % 2
                        if stage == "B":
                            return chunkB(locals())
                        items = [(psb[bk][:, :], hT[:, c, t * 128:(t + 1) * 128], qkvp[s][:, c, :], c == 0, c == NCH - 1) for c in range(NCH)]
                        _mm(P, items, b_hT[t] + b_qkvp[s], [pb[bk]])
                        rotary(psb[bk][:, 0:128], cso, sno, t, mq1, mq2, b_mq, qrot, b_qrot, pb[bk], b_tabo, stq_[ip], b_stq_[ip])
                        rotary(psb[bk][:, 128:256], cso, sno, t, mk1, mk2, b_mk, krot, b_krot, pb[bk], b_tabo, stk_[ip], b_stk_[ip])
                        P.op("act", lambda e: e.copy(out=vb[:, :], in_=psb[bk][:, 256:512]), [pb[bk]], [b_vb])
                        P.op("act", lambda e: e.activation(out=kd[:, :], in_=krot[:, :], func=AF.Copy, scale=kdec[:, h:h + 1]),
                             b_krot + [b_kdec], [b_kd])

                        def trq(e):
                            e.transpose(out=pT_[:, 0:128], in_=qrot[:, :], identity=ident[:, :])
                            return e.transpose(out=pT_[:, 128:256], in_=krot[:, :], identity=ident[:, :])
                        P.op("pe", trq, b_qrot + b_krot + [b_ident], [r_T])
                        P.op("act", lambda e: e.copy(out=qT[:, :], in_=pT_[:, 0:128]), [r_T], [b_qT])
                        P.op("act", lambda e: e.copy(out=kT[:, :], in_=pT_[:, 128:256]), [r_T], [b_kT])
                        P.op("dve", lambda e: e.tensor_tensor(out=qTd[:, :], in0=pT_[:, 0:128], in1=qdec[:, h, :], op=ALU.mult),
                             [r_T, b_qdec], [b_qTd])
                        _mm(P, [(pSC[:, 0:128], kT[:, :], qT[:, :], True, True)], [b_kT, b_qT], [r_sc])
                        P.op("dve", lambda e: e.tensor_tensor(out=AT[:, :], in0=pSC[:, 0:128], in1=maskT[:, h, :], op=ALU.mult),
                             [r_sc, b_msk], [b_AT])

                    def chunkB(L):
                        h = L["h"]; t = L["t"]; srg = L["srg"]; b_srg = L["b_srg"]
                        pSC = L["pSC"]; r_sc = L["r_sc"]; pRET = L["pRET"]; r_ret = L["r_ret"]
                        kd = L["kd"]; b_kd = L["b_kd"]; vb = L["vb"]; b_vb = L["b_vb"]; qTd = L["qTd"]; b_qTd = L["b_qTd"]
                        AT = L["AT"]; b_AT = L["b_AT"]; sq = L["sq"]; b_sq = L["b_sq"]; rbc = L["rbc"]; b_rbc = L["b_rbc"]
                        ztmp = L["ztmp"]; b_ztmp = L["b_ztmp"]
                        if t == 0:
                            P.op("act", lambda e: e.copy(out=Sb[:, :], in_=Sst[:, h, :]), [b_S[h]], [b_Sb])
                        items = []
                        for eb in range(2):
                            items.append((pRET[:, eb * 128:(eb + 1) * 128], vb[:, eb * 128:(eb + 1) * 128], AT[:, :], True, False))
                            items.append((pRET[:, eb * 128:(eb + 1) * 128], Sb[:, eb * 128:(eb + 1) * 128], qTd[:, :], False, True))
                        _mm(P, items, [b_vb, b_AT, b_Sb, b_qTd], [r_ret])
                        P.op("act", lambda e: e.activation(out=sq[:, :], in_=pRET[:, 0:256], func=AF.Square), [r_ret], [b_sq])
                        _mm(P, [(pSC[:, 128:256], ones_bf[:, :], sq[:, 0:128], True, False),
                                (pSC[:, 128:256], ones_bf[:, :], sq[:, 128:256], False, True)], [b_ones, b_sq], [r_sc])
                        P.op("dve", lambda e: e.tensor_scalar(out=rbc[:, :], in0=pSC[:, 128:256], scalar1=1.0 / 256, scalar2=EPS,
                                                              op0=ALU.mult, op1=ALU.add), [r_sc], [b_rbc])
                        P.op("act", lambda e: e.activation(out=rbc[:, :], in_=rbc[:, :], func=AF.Sqrt), [b_rbc], [b_rbc])
                        P.op("dve", lambda e: e.reciprocal(out=rbc[:, :], in_=rbc[:, :]), [b_rbc], [b_rbc])
                        P.op("dve", lambda e: e.tensor_tensor(out=ztmp[:, :, :], in0=pRET[:, 0:256].rearrange("p (a i) -> p a i", a=2),
                                                              in1=rbc[:, :].unsqueeze(1).to_broadcast([128, 2, 128]), op=ALU.mult),
                             [r_ret, b_rbc], [b_ztmp])
                        P.op("dve", lambda e: e.tensor_tensor(out=zbT[:, 2 * h:2 * h + 2, t * 128:(t + 1) * 128], in0=ztmp[:, :, :],
                                                              in1=srg[:, :, t * 128:(t + 1) * 128], op=ALU.mult),
                             [b_ztmp] + b_srg, [b_zb[2 * h][t], b_zb[2 * h + 1][t]])
                        if t < NT - 1:
                            _mm(P, [(pRET[:, 256:512], kd[:, :], vb[:, :], True, True)], [b_kd, b_vb], [r_ret])
                            P.op("dve", lambda e: e.scalar_tensor_tensor(
                                out=Sst[:, h, :], in0=Sst[:, h, :], scalar=cdec[h], in1=pRET[:, 256:512], op0=ALU.mult, op1=ALU.add),
                                [r_ret, b_S[h]], [b_S[h]])
                            P.op("act", lambda e: e.copy(out=Sb[:, :], in_=Sst[:, h, :]), [b_S[h]], [b_Sb])

                    def prologue(h):
                        s = h % 2
                        srg = srg_[h % 2]; b_srg = b_srg_[h % 2]
                        for i, (c0, n, off) in enumerate([(3072 + 128 * h, 128, 0), (4096 + 128 * h, 128, 128), (5120 + 256 * h, 256, 256)]):
                            P.dma("pool", lambda e, s=s, c0=c0, n=n, off=off: e.dma_start(
                                out=qkvp[s][:, :, off:off + n], in_=w_in[:, c0:c0 + n].rearrange("(kt p) n -> p kt n", p=128)),
                                f"qkvp{s}", writes=[b_qkvp[s][i]])
                        P.dma("pool", lambda e, h=h: e.dma_start(
                            out=rgp[:, :, :], in_=w_in[:, 7168 + 256 * h:7168 + 256 * (h + 1)].rearrange("(kt p) n -> p kt n", p=128)),
                            "rgp", writes=[b_rgp])
                        for eb in range(2):
                            for th in range(2):
                                bk = 4 + th
                                items = [(psb[bk][:, :], rgp[:, c, eb * 128:(eb + 1) * 128], hT[:, c, th * 512:(th + 1) * 512], c == 0, c == NCH - 1)
                                         for c in range(NCH)]
                                _mm(P, items, all_hT + [b_rgp], [pb[bk]])
                                P.op("act", lambda e, eb=eb, th=th, bk=bk: e.activation(out=srg[:, eb, th * 512:(th + 1) * 512], in_=psb[bk][:, :], func=AF.Silu),
                                     [pb[bk]], [b_srg[eb * 2 + th]])

                    seq2 = [(h, t) for h in range(8) for t in range(NT)]
                    for i in range(len(seq2) + 1):
                        if i < len(seq2):
                            h, t = seq2[i]
                            if t == 0:
                                prologue(h)
                            chunk(h, t, h % 2, t % 2, "A")
                        if i >= 1:
                            h, t = seq2[i - 1]
                            chunk(h, t, h % 2, t % 2, "B")
                    P.barrier()
                _phase_E2b()
            all_zb = [b for c in range(NCH) for b in b_zb[c]]
            if "zbT" in debug:
                d = nc.dram_tensor("dbg_zbT", [128, NCH * TOK], BF16, kind="ExternalOutput").ap()
                fin.append(P.dma("sp", lambda e, d=d: e.dma_start(out=d[:, :], in_=zbT[:, :, :].rearrange("p c t -> p (c t)")), "st_zb", reads=all_zb))

            stop("stop2b")
            zaT = S("zaT", [128, 8, TOK], BF16, E12); b_za = P.bufs("za", 8)

            with ExitStack() as E2c:
                def _phase_E2c():
                    cvp = [S(f"cvp{i}", [128, NCH, 384], BF16, E2c) for i in range(2)]
                    b_cvp = [P.bufs(f"cvp{i}_", 3) for i in range(2)]
                    ccs = S("ccs", [128, TOK], es=E2c); b_ccs = P.bufs("ccs", 2)
                    tt = S("tt", [128, TOK + 2], es=E2c); b_tt = P.bufs("tt", 3)
                    hcc = S("hcc", [128, 2], es=E2c); b_hcc = P.buf("hcc")
                    acc = S("acc", [128, TOK], es=E2c); b_acc = P.buf("acc")
                    r_h = pb[6]
                    for cb in range(8):
                        s = cb % 2
                        for i in range(3):
                            P.dma("pool", lambda e, s=s, i=i, cb=cb: e.dma_start(
                                out=cvp[s][:, :, i * 128:(i + 1) * 128],
                                in_=w_in[:, 1024 * i + 128 * cb:1024 * i + 128 * (cb + 1)].rearrange("(kt p) n -> p kt n", p=128)),
                                f"cvp{s}", writes=[b_cvp[s][i]])
                        items = [(psb[6][:, 0:2], cvp[s][:, c, 128:256], htail[:, c, :], c == 0, c == NCH - 1) for c in range(NCH)]
                        items += [(psb[6][:, 2:4], cvp[s][:, c, 256:384], htail[:, c, :], c == 0, c == NCH - 1) for c in range(NCH)]
                        _mm(P, items, [b_htail] + b_cvp[s], [r_h])
                        P.op("act", lambda e: e.activation(out=hcc[:, :], in_=psb[6][:, 0:2], func=AF.Copy, scale=flag[:, 0:1]), [r_h, b_flag], [b_hcc])
                        P.op("dve", lambda e: e.tensor_tensor(out=tt[:, 0:2], in0=hcc[:, :], in1=psb[6][:, 2:4], op=ALU.mult), [r_h, b_hcc], [b_tt[2]])
                        for th in range(2):
                            bk = 2 + th
                            items = [(psb[bk][:, :], cvp[s][:, c, 128:256], hT[:, c, th * 512:(th + 1) * 512], c == 0, c == NCH - 1) for c in range(NCH)]
                            _mm(P, items, all_hT + b_cvp[s], [pb[bk]])
                            P.op("act", lambda e, th=th, bk=bk: e.copy(out=ccs[:, th * 512:(th + 1) * 512], in_=psb[bk][:, :]), [pb[bk]], [b_ccs[th]])
                        for th in range(2):
                            bk = 4 + th
                            items = [(psb[bk][:, :], cvp[s][:, c, 256:384], hT[:, c, th * 512:(th + 1) * 512], c == 0, c == NCH - 1) for c in range(NCH)]
                            _mm(P, items, all_hT + b_cvp[s], [pb[bk]])
                            P.op("dve", lambda e, th=th, bk=bk: e.tensor_tensor(out=tt[:, 2 + th * 512:2 + (th + 1) * 512], in0=ccs[:, th * 512:(th + 1) * 512],
                                                                                in1=psb[bk][:, :], op=ALU.mult), [pb[bk], b_ccs[th]], [b_tt[th]])
                        P.op("dve", lambda e, cb=cb: e.tensor_scalar(out=acc[:, :], in0=tt[:, 0:TOK], scalar1=convw[:, cb, 0:1], scalar2=None, op0=ALU.mult),
                             b_tt + [b_convw], [b_acc])
                        P.op("dve", lambda e, cb=cb: e.scalar_tensor_tensor(out=acc[:, :], in0=tt[:, 1:TOK + 1], scalar=convw[:, cb, 1:2], in1=acc[:, :],
                                                                            op0=ALU.mult, op1=ALU.add), b_tt + [b_convw, b_acc], [b_acc])
                        P.op("dve", lambda e, cb=cb: e.scalar_tensor_tensor(out=acc[:, :], in0=tt[:, 2:TOK + 2], scalar=convw[:, cb, 2:3], in1=acc[:, :],
                                                                            op0=ALU.mult, op1=ALU.add), b_tt + [b_convw, b_acc], [b_acc])
                        for th in range(2):
                            bk = 2 + th
                            items = [(psb[bk][:, :], cvp[s][:, c, 0:128], hT[:, c, th * 512:(th + 1) * 512], c == 0, c == NCH - 1) for c in range(NCH)]
                            _mm(P, items, all_hT + b_cvp[s], [pb[bk]])
                            P.op("dve", lambda e, th=th, bk=bk, cb=cb: e.tensor_tensor(out=zaT[:, cb, th * 512:(th + 1) * 512], in0=acc[:, th * 512:(th + 1) * 512],
                                                                                       in1=psb[bk][:, :], op=ALU.mult), [pb[bk], b_acc], [b_za[cb]])
                    P.barrier()
                _phase_E2c()
            if "zaT" in debug:
                d = nc.dram_tensor("dbg_zaT", [128, 8 * TOK], BF16, kind="ExternalOutput").ap()
                fin.append(P.dma("sp", lambda e, d=d: e.dma_start(out=d[:, :], in_=zaT[:, :, :].rearrange("p c t -> p (c t)")), "st_za", reads=b_za))

            stop("stop2c")
            with ExitStack() as E2d:
                def _phase_E2d():
                    opp = [S(f"opp{i}", [128, 56, 128], BF16, E2d) for i in range(2)]
                    b_opp = [P.bufs(f"opp{i}_", 4) for i in range(2)]
                    sga = [S(f"sga{i}", [128, 512], es=E2d) for i in range(2)]; b_sga = P.bufs("sga", 2)
                    sgb = [S(f"sgb{i}", [128, 512], es=E2d) for i in range(2)]; b_sgb = P.bufs("sgb", 2)
                    ma = [S(f"ma{i}", [128, 512], es=E2d) for i in range(2)]; b_ma = P.bufs("ma", 2)
                    mb = [S(f"mb{i}", [128, 512], es=E2d) for i in range(2)]; b_mb = P.bufs("mb", 2)
                    banks = [(2, 3, 4, 5), (0, 1, 6, 7)]
                    for db in range(NCH):
                        s = db % 2
                        srcs = [(w_co[:, db * 128:(db + 1) * 128], 0, 8), (w_ro[:, db * 128:(db + 1) * 128], 8, 16),
                                (w_in[:, 9216 + db * 128:9216 + (db + 1) * 128], 24, 16), (w_in[:, 11264 + db * 128:11264 + (db + 1) * 128], 40, 16)]
                        for i, (src, off, n) in enumerate(srcs):
                            P.dma("pool", lambda e, s=s, src=src, off=off, n=n: e.dma_start(
                                out=opp[s][:, off:off + n, :], in_=src.rearrange("(kt p) n -> p kt n", p=128)), f"opp{s}", writes=[b_opp[s][i]])
                        for th in range(2):
                            ba, bb, bga, bgb = banks[th]
                            tsl = slice(th * 512, (th + 1) * 512)
                            _mm(P, [(psb[ba][:, :], opp[s][:, c, :], zaT[:, c, tsl], c == 0, c == 7) for c in range(8)], b_za + [b_opp[s][0]], [pb[ba]])
                            _mm(P, [(psb[bb][:, :], opp[s][:, 8 + c, :], zbT[:, c, tsl], c == 0, c == 15) for c in range(16)], all_zb + [b_opp[s][1]], [pb[bb]])
                            _mm(P, [(psb[bga][:, :], opp[s][:, 24 + c, :], hT[:, c, tsl], c == 0, c == 15) for c in range(16)], all_hT + [b_opp[s][2]], [pb[bga]])
                            _mm(P, [(psb[bgb][:, :], opp[s][:, 40 + c, :], hT[:, c, tsl], c == 0, c == 15) for c in range(16)], all_hT + [b_opp[s][3]], [pb[bgb]])
                            P.op("act", lambda e, th=th, bga=bga: e.activation(out=sga[th][:, :], in_=psb[bga][:, :], func=AF.Sigmoid), [pb[bga]], [b_sga[th]])
                            P.op("act", lambda e, th=th, bgb=bgb: e.activation(out=sgb[th][:, :], in_=psb[bgb][:, :], func=AF.Sigmoid), [pb[bgb]], [b_sgb[th]])
                            P.op("dve", lambda e, th=th, ba=ba: e.tensor_tensor(out=ma[th][:, :], in0=sga[th][:, :], in1=psb[ba][:, :], op=ALU.mult),
                                 [pb[ba], b_sga[th]], [b_ma[th]])
                            P.op("dve", lambda e, th=th, bb=bb: e.tensor_tensor(out=mb[th][:, :], in0=sgb[th][:, :], in1=psb[bb][:, :], op=ALU.mult),
                                 [pb[bb], b_sgb[th]], [b_mb[th]])
                            P.op("dve", lambda e, th=th, db=db, tsl=tsl: e.tensor_tensor(out=mergedT[:, db, tsl], in0=ma[th][:, :], in1=mb[th][:, :], op=ALU.add),
                                 [b_ma[th], b_mb[th]], [b_mg[db][th]])
                    P.barrier()
                _phase_E2d()
        all_mg = [b for d_ in range(NCH) for b in b_mg[d_]]
        if "mergedT" in debug:
            d = nc.dram_tensor("dbg_mergedT", [128, NCH * TOK], BF16, kind="ExternalOutput").ap()
            fin.append(P.dma("sp", lambda e, d=d: e.dma_start(out=d[:, :], in_=mergedT[:, :, :].rearrange("p c t -> p (c t)")), "st_mg", reads=all_mg))

        stop("stop2d")
        x1 = S("x1", [128, NT, D]); b_x1 = [P.bufs(f"x1_{t}_", 4) for t in range(NT)]
        Cw = S("Cw", [128, NT, NEXP]); b_C = P.bufs("C", NT)
        P.wait_all.add("x1ld")
        for t in range(NT):
            P.dma("sp", lambda e, t=t: e.dma_start(out=x1[:, t, :], in_=x_own[t * 128:(t + 1) * 128, :]), "x1ld", writes=b_x1[t])
        with ExitStack() as E3:
            def _phase_E3():
                wop = [S(f"wop{i}", [128, NCH, 256], BF16, E3) for i in range(2)]; b_wop = P.bufs("wop", 2)
                tmp = [S(f"tmp{i}", [128, 256], es=E3) for i in range(2)]; b_tmp = P.bufs("tmp", 2)
                xs_slots = [S(f"xs{i}", [128, D], es=E3) for i in range(2)]; b_xs = P.bufs("xs", 2)
                junk = S("junk", [128, D], BF16, E3); b_junk = P.buf("junk")
                k = 0
                for nb in range(8):
                    s = nb % 2
                    P.dma("pool", lambda e, s=s, nb=nb: e.dma_start(
                        out=wop[s][:, :, :], in_=w_o[:, nb * 256:(nb + 1) * 256].rearrange("(kt p) n -> p kt n", p=128)), f"wop{s}", writes=[b_wop[s]])
                    for t in range(NT):
                        bk = 2 + k % 2
                        q = nb // 2
                        _mm(P, [(psb[bk][:, 0:256], mergedT[:, c, t * 128:(t + 1) * 128], wop[s][:, c, :], c == 0, c == NCH - 1) for c in range(NCH)],
                            all_mg + [b_wop[s]], [pb[bk]])
                        P.op("dve", lambda e, bk=bk, nb=nb, k=k: e.tensor_tensor(out=tmp[k % 2][:, :], in0=psb[bk][:, 0:256], in1=gbc[:, nb * 256:(nb + 1) * 256], op=ALU.mult),
                             [pb[bk], b_gbc[nb // 2]], [b_tmp[k % 2]])
                        P.op("dve", lambda e, t=t, nb=nb, k=k: e.tensor_tensor(out=x1[:, t, nb * 256:(nb + 1) * 256], in0=x1[:, t, nb * 256:(nb + 1) * 256],
                                                                               in1=tmp[k % 2][:, :], op=ALU.add), [b_tmp[k % 2], b_x1[t][q]], [b_x1[t][q]])
                        k += 1
                if "x1" in debug:
                    d = nc.dram_tensor("dbg_x1", [TOK, D], F32, kind="ExternalOutput").ap()
                    for t in range(NT):
                        fin.append(P.dma("sp", lambda e, t=t, d=d: e.dma_start(out=d[t * 128:(t + 1) * 128, :], in_=x1[:, t, :]), f"st_x1{t}", reads=b_x1[t]))
                lg = S("lg", [128, 36], es=E3); b_lg = P.buf("lg")
                rs = S("rs", [128, 16], es=E3); b_rs = P.bufs("rs", 16)
                ohg = S("ohg", [128, 4], es=E3); b_ohg = P.buf("ohg")
                egj = S("egj", [128, 4], es=E3); b_egj = P.buf("egj")
                selm = S("selm", [128, 4, 8], es=E3); b_selm = P.buf("selm")
                sel = S("sel", [128, 8], es=E3); sel2 = S("sel2", [128, 8], es=E3); b_sel = P.buf("sel"); b_sel2 = P.buf("sel2")
                oh1 = S("oh1", [128, 8], es=E3); oh2 = S("oh2", [128, 8], es=E3); b_oh1 = P.buf("oh1"); b_oh2 = P.buf("oh2")
                w8 = S("w8", [128, 8], es=E3); w8b = S("w8b", [128, 8], es=E3); b_w8 = P.buf("w8"); b_w8b = P.buf("w8b")
                for t in range(NT):
                    make_hT(x1[:, t, :], b_x1[t], A2, B2, b_A2, hT, b_hT[t], t, xs_slots, b_xs, junk, b_junk, t)
                    _mm(P, [(psb[4][:, 0:36], hT[:, c, t * 128:(t + 1) * 128], wr[:, c, :], c == 0, c == NCH - 1) for c in range(NCH)],
                        b_hT[t] + [b_wr], [pb[4]])
                    P.op("dve", lambda e: e.tensor_tensor(out=lg[:, :], in0=psb[4][:, 0:36], in1=b_rt[:, :], op=ALU.add), [pb[4], b_br], [b_lg])
                    P.op("dve", lambda e: e.reduce_max(out=rs[:, 0:1], in_=lg[:, 0:4], axis=AX.X), [b_lg], [b_rs[0]])
                    P.op("dve", lambda e: e.tensor_scalar(out=ohg[:, :], in0=lg[:, 0:4], scalar1=rs[:, 0:1], scalar2=None, op0=ALU.is_equal), [b_lg, b_rs[0]], [b_ohg])
                    P.op("dve", lambda e: e.tensor_scalar(out=rs[:, 1:2], in0=rs[:, 0:1], scalar1=-1.0, scalar2=None, op0=ALU.mult), [b_rs[0]], [b_rs[1]])
                    P.op("act", lambda e: e.activation(out=egj[:, :], in_=lg[:, 0:4], func=AF.Exp, bias=rs[:, 1:2], scale=1.0, accum_out=rs[:, 2:3]),
                         [b_lg, b_rs[1]], [b_egj, b_rs[2]])
                    P.op("dve", lambda e: e.reciprocal(out=rs[:, 3:4], in_=rs[:, 2:3]), [b_rs[2]], [b_rs[3]])
                    P.op("dve", lambda e: e.tensor_tensor(out=selm[:, :, :], in0=lg[:, 4:36].rearrange("p (g j) -> p g j", g=4),
                                                          in1=ohg[:, :].unsqueeze(2).to_broadcast([128, 4, 8]), op=ALU.mult), [b_lg, b_ohg], [b_selm])
                    P.op("dve", lambda e: e.tensor_reduce(out=sel[:, :], in_=selm[:, :, :].rearrange("p g j -> p j g"), axis=AX.X, op=ALU.add), [b_selm], [b_sel])
                    P.op("dve", lambda e: e.reduce_max(out=rs[:, 4:5], in_=sel[:, :], axis=AX.X), [b_sel], [b_rs[4]])
                    P.op("dve", lambda e: e.tensor_scalar(out=oh1[:, :], in0=sel[:, :], scalar1=rs[:, 4:5], scalar2=None, op0=ALU.is_equal), [b_sel, b_rs[4]], [b_oh1])
                    P.op("dve", lambda e: e.scalar_tensor_tensor(out=sel2[:, :], in0=oh1[:, :], scalar=-1e30, in1=sel[:, :], op0=ALU.mult, op1=ALU.add),
                         [b_oh1, b_sel], [b_sel2])
                    P.op("dve", lambda e: e.reduce_max(out=rs[:, 5:6], in_=sel2[:, :], axis=AX.X), [b_sel2], [b_rs[5]])
                    P.op("dve", lambda e: e.tensor_scalar(out=oh2[:, :], in0=sel2[:, :], scalar1=rs[:, 5:6], scalar2=None, op0=ALU.is_equal), [b_sel2, b_rs[5]], [b_oh2])
                    P.op("dve", lambda e: e.tensor_tensor(out=rs[:, 6:7], in0=rs[:, 5:6], in1=rs[:, 4:5], op=ALU.subtract), [b_rs[4], b_rs[5]], [b_rs[6]])
                    P.op("act", lambda e: e.activation(out=rs[:, 7:8], in_=rs[:, 6:7], func=AF.Exp), [b_rs[6]], [b_rs[7]])
                    P.op("dve", lambda e: e.tensor_scalar(out=rs[:, 8:9], in0=rs[:, 7:8], scalar1=1.0, scalar2=None, op0=ALU.add), [b_rs[7]], [b_rs[8]])
                    P.op("dve", lambda e: e.reciprocal(out=rs[:, 9:10], in_=rs[:, 8:9]), [b_rs[8]], [b_rs[9]])
                    P.op("dve", lambda e: e.tensor_tensor(out=rs[:, 10:11], in0=rs[:, 9:10], in1=rs[:, 3:4], op=ALU.mult), [b_rs[9], b_rs[3]], [b_rs[10]])
                    P.op("dve", lambda e: e.tensor_tensor(out=rs[:, 11:12], in0=rs[:, 10:11], in1=rs[:, 7:8], op=ALU.mult), [b_rs[10], b_rs[7]], [b_rs[11]])
                    P.op("dve", lambda e: e.tensor_scalar(out=w8[:, :], in0=oh1[:, :], scalar1=rs[:, 10:11], scalar2=None, op0=ALU.mult), [b_oh1, b_rs[10]], [b_w8])
                    P.op("dve", lambda e: e.scalar_tensor_tensor(out=w8b[:, :], in0=oh2[:, :], scalar=rs[:, 11:12], in1=w8[:, :], op0=ALU.mult, op1=ALU.add),
                         [b_oh2, b_rs[11], b_w8], [b_w8b])
                    P.op("dve", lambda e, t=t: e.tensor_tensor(out=Cw[:, t, :].rearrange("p (g j) -> p g j", g=4),
                                                               in0=ohg[:, :].unsqueeze(2).to_broadcast([128, 4, 8]),
                                                               in1=w8b[:, :].unsqueeze(1).to_broadcast([128, 4, 8]), op=ALU.mult), [b_ohg, b_w8b], [b_C[t]])
                P.barrier()
            _phase_E3()
        if "h2T" in debug:
            d = nc.dram_tensor("dbg_h2T", [128, NCH * TOK], BF16, kind="ExternalOutput").ap()
            fin.append(P.dma("sp", lambda e, d=d: e.dma_start(out=d[:, :], in_=hT[:, :, :].rearrange("p c t -> p (c t)")), "st_h2", reads=[b for t in range(NT) for b in b_hT[t]]))
        if "C" in debug:
            d = nc.dram_tensor("dbg_C", [128, NT * NEXP], F32, kind="ExternalOutput").ap()
            fin.append(P.dma("sp", lambda e, d=d: e.dma_start(out=d[:, :], in_=Cw[:, :, :].rearrange("p t e -> p (t e)")), "st_C", reads=b_C))
        all_h2 = [b for t in range(NT) for b in b_hT[t]]
        stop("stop3")

        if "nomoe" not in debug:
            with ExitStack() as E4:
                def _phase_E4():
                    aT = mergedT
                    b_aT = [P.bufs(f"aT{i}_", 16) for i in range(2)]
                    wg = [S(f"wg{i}", [128, NCH, 128], BF16, E4) for i in range(2)]; b_wg = P.bufs("wg", 2)
                    wu = [S(f"wu{i}", [128, NCH, 128], BF16, E4) for i in range(2)]; b_wu = P.bufs("wu", 2)
                    wd = [S(f"wd{i}", [128, 8, 1024], BF16, E4) for i in range(2)]; b_wd = P.bufs("wd", 2)
                    sg = [S(f"sg{i}", [128, 512], es=E4) for i in range(2)]; b_sg = P.bufs("sg", 2)
                    tm = [S(f"tm{i}", [128, 512], es=E4) for i in range(2)]; b_tm = P.bufs("tm", 2)
                    kq = 0
                    kd_ = 0
                    for ex in range(NEXP):
                        sl = ex % 2
                        for fc in range(8):
                            s = kq % 2
                            P.dma("pool", lambda e, s=s, ex=ex, fc=fc: e.dma_start(
                                out=wg[s][:, :, :], in_=w_gate[ex, :, fc * 128:(fc + 1) * 128].rearrange("(kt p) n -> p kt n", p=128)), f"wg{s}", writes=[b_wg[s]])
                            P.dma("pool", lambda e, s=s, ex=ex, fc=fc: e.dma_start(
                                out=wu[s][:, :, :], in_=w_up[ex, :, fc * 128:(fc + 1) * 128].rearrange("(kt p) n -> p kt n", p=128)), f"wu{s}", writes=[b_wu[s]])
                            for th in range(2):
                                tsl = slice(th * 512, (th + 1) * 512)
                                bg = 2 + th
                                bu = 4 + th
                                _mm(P, [(psb[bg][:, :], wg[s][:, c, :], hT[:, c, tsl], c == 0, c == NCH - 1) for c in range(NCH)], all_h2 + [b_wg[s]], [pb[bg]])
                                _mm(P, [(psb[bu][:, :], wu[s][:, c, :], hT[:, c, tsl], c == 0, c == NCH - 1) for c in range(NCH)], all_h2 + [b_wu[s]], [pb[bu]])
                                P.op("act", lambda e, th=th, bg=bg: e.activation(out=sg[th][:, :], in_=psb[bg][:, :], func=AF.Silu), [pb[bg]], [b_sg[th]])
                                P.op("dve", lambda e, th=th, bu=bu, sl=sl, fc=fc, tsl=tsl: e.tensor_tensor(
                                    out=aT[:, sl * 8 + fc, tsl], in0=sg[th][:, :], in1=psb[bu][:, :], op=ALU.mult), [pb[bu], b_sg[th]], [b_aT[sl][fc * 2 + th]])
                            kq += 1
                        for hf in range(2):
                            P.dma("pool", lambda e, hf=hf, ex=ex: e.dma_start(
                                out=wd[hf][:, :, :], in_=w_down[ex, :, hf * 1024:(hf + 1) * 1024].rearrange("(fc p) n -> p fc n", p=128)), f"wd{hf}", writes=[b_wd[hf]])
                        for hf in range(2):
                            for nb2 in range(2):
                                nb = hf * 2 + nb2
                                for t in range(NT):
                                    bk = (6, 7, 0, 1)[kd_ % 4]
                                    _mm(P, [(psb[bk][:, :], aT[:, sl * 8 + fc, t * 128:(t + 1) * 128], wd[hf][:, fc, nb2 * 512:(nb2 + 1) * 512], fc == 0, fc == 7)
                                            for fc in range(8)], b_aT[sl] + [b_wd[hf]], [pb[bk]])
                                    ts_ = kd_ % 2
                                    P.op("dve", lambda e, bk=bk, t=t, ex=ex, nb=nb, ts_=ts_: e.scalar_tensor_tensor(
                                        out=tm[ts_][:, :], in0=psb[bk][:, :], scalar=Cw[:, t, ex:ex + 1], in1=gbc[:, D + nb * 512:D + (nb + 1) * 512],
                                        op0=ALU.mult, op1=ALU.mult), [pb[bk], b_C[t], b_gbc[4 + nb]], [b_tm[ts_]])
                                    P.op("dve", lambda e, t=t, nb=nb, ts_=ts_: e.tensor_tensor(
                                        out=x1[:, t, nb * 512:(nb + 1) * 512], in0=x1[:, t, nb * 512:(nb + 1) * 512], in1=tm[ts_][:, :], op=ALU.add),
                                        [b_tm[ts_], b_x1[t][nb]], [b_x1[t][nb]])
                                    kd_ += 1
                    P.barrier()
                _phase_E4()

        with ExitStack() as E5:
            def _phase_E5():
                nfb = S("nfb", [128, D], es=E5); b_nfb = P.buf("nfb")
                P.dma("sp", lambda e: e.dma_start(out=nfb[:, :], in_=nf_d[:, :]), "nfb", writes=[b_nfb])
                ot = [S(f"ot{i}", [128, D], es=E5) for i in range(2)]; b_ot = P.bufs("ot", 2)
                junk = S("junk", [128, D], BF16, E5); b_junk = P.buf("junk")
                for t in range(NT):
                    s = t % 2
                    P.op("act", lambda e, t=t, s=s: e.activation(out=junk[:, :], in_=x1[:, t, :], func=AF.Square, accum_out=sst[:, s:s + 1]),
                         b_x1[t], [b_junk, b_ss[s]])
                    P.op("dve", lambda e, s=s: e.tensor_scalar(out=sst[:, 2 + s:3 + s], in0=sst[:, s:s + 1], scalar1=1.0 / D, scalar2=EPS,
                                                               op0=ALU.mult, op1=ALU.add), [b_ss[s]], [b_t1[s]])
                    P.op("act", lambda e, s=s: e.activation(out=sst[:, 2 + s:3 + s], in_=sst[:, 2 + s:3 + s], func=AF.Sqrt), [b_t1[s]], [b_t1[s]])
                    P.op("dve", lambda e, s=s: e.reciprocal(out=rstd[:, s:s + 1], in_=sst[:, 2 + s:3 + s]), [b_t1[s]], [b_rstd[s]])
                    P.op("dve", lambda e, t=t, s=s: e.scalar_tensor_tensor(out=ot[s][:, :], in0=x1[:, t, :], scalar=rstd[:, s:s + 1], in1=nfb[:, :],
                                                                           op0=ALU.mult, op1=ALU.mult), b_x1[t] + [b_rstd[s], b_nfb], [b_ot[s]])
                    fin.append(P.dma("sp", lambda e, t=t, s=s: e.dma_start(out=y_out[t * 128:(t + 1) * 128, :], in_=ot[s][:, :]), f"ot{s}", reads=[b_ot[s]]))
                P.emit(final_wait_ops=fin)
            _phase_E5()
    return nc


def _const_tables(half):
    inv = (np.float32(10000.0) ** (-np.arange(0, 128, 2, dtype=np.float32) / np.float32(128))).astype(np.float32)

    def tabs(pos0):
        pos = (pos0 + np.arange(TOK, dtype=np.float32)).astype(np.float32)
        ang = (pos[:, None] * inv[None, :]).astype(np.float32)
        cos = np.cos(ang).astype(np.float32); sin = np.sin(ang).astype(np.float32)
        cs = np.concatenate([cos, cos], axis=1).reshape(NT, 128, 128).transpose(1, 0, 2)
        sn = np.concatenate([sin, sin], axis=1).reshape(NT, 128, 128).transpose(1, 0, 2)
        return np.ascontiguousarray(cs), np.ascontiguousarray(sn)

    cs_own, sn_own = tabs(np.float32(half * TOK))
    cs_prev, sn_prev = tabs(np.float32(0))
    log_g = np.log1p(-(2.0 ** (-5.0 - np.arange(8, dtype=np.float32)))).astype(np.float32)
    i = np.arange(128, dtype=np.float32)
    scale = np.float32(128.0 ** -0.5)
    diff = i[None, :] - i[:, None]
    maskT = np.where(diff[None] >= 0, np.exp(log_g[:, None, None] * np.maximum(diff[None], 0.0)), 0.0).astype(np.float32) * scale
    maskT = np.ascontiguousarray(maskT.transpose(1, 0, 2))
    qd = np.exp(log_g[:, None] * (i + 1.0)).astype(np.float32)
    qdec = np.ascontiguousarray(np.broadcast_to(qd[None], (128, 8, 128))).astype(np.float32)
    kdec = np.ascontiguousarray((np.exp(log_g[:, None] * (127.0 - i)).astype(np.float32) * scale).T)
    return dict(cs_own=cs_own, sn_own=sn_own, cs_prev=cs_prev, sn_prev=sn_prev, maskT=maskT, qdec=qdec, kdec=kdec)


_PROG_CACHE = {}


def _make_in_maps(inputs):
    f = lambda a: np.ascontiguousarray(np.asarray(a, dtype=np.float32))
    x = f(inputs["x"]); c = f(inputs["c"])
    b_ada = f(inputs["b_ada"])[0]
    shared = dict(
        w_ada=f(inputs["w_ada"])[0],
        b_adaT=np.ascontiguousarray(b_ada.reshape(96, 128).T),
        b_adag=np.ascontiguousarray(np.concatenate([b_ada[2 * D:3 * D], b_ada[5 * D:6 * D]])[None, :]),
        n1T=np.ascontiguousarray(f(inputs["norm1_g"])[0].reshape(NCH, 128).T),
        n2T=np.ascontiguousarray(f(inputs["norm2_g"])[0].reshape(NCH, 128).T),
        nf_bc=np.ascontiguousarray(np.broadcast_to(f(inputs["norm_f_g"])[None, :], (128, D))),
        w_in=f(inputs["w_in"])[0],
        conv_wT=np.ascontiguousarray(f(inputs["conv_w"])[0].reshape(3, 8, 128).transpose(2, 1, 0)),
        w_conv_out=f(inputs["w_conv_out"])[0], w_ret_out=f(inputs["w_ret_out"])[0], w_o=f(inputs["w_o"])[0],
        w_r=np.ascontiguousarray(np.concatenate([f(inputs["w_router_group"])[0], f(inputs["w_router_expert"])[0]], axis=1)),
        b_r=np.ascontiguousarray(np.broadcast_to(np.concatenate([f(inputs["b_router_group"])[0], f(inputs["b_router_expert"])[0]])[None, :], (128, 36))),
        w_gate=f(inputs["w_gate"])[0], w_up=f(inputs["w_up"])[0], w_down=f(inputs["w_down"])[0],
        ident=np.eye(128, dtype=np.float32),
    )
    tabs = [_const_tables(0), _const_tables(1)]
    in_maps = []
    for core in range(NCORES):
        b, half = core // 2, core % 2
        m = dict(shared)
        m.update(tabs[half])
        m["x_own"] = np.ascontiguousarray(x[b, half * TOK:(half + 1) * TOK])
        m["x_prev"] = np.ascontiguousarray(x[b, 0:TOK])
        m["cT"] = np.ascontiguousarray(c[b].reshape(NCH, 128).T)
        m["flag"] = np.full((128, 1), float(half), dtype=np.float32)
        in_maps.append(m)
    return in_maps


def kernel(**inputs):
    if "prog" not in _PROG_CACHE:
        _PROG_CACHE["prog"] = build_program()
    nc = _PROG_CACHE["prog"]
    in_maps = _make_in_maps(inputs)
    res = run_bass_kernel_spmd(nc, in_maps, core_ids=list(range(NCORES)))
    out = np.empty((4, 2048, D), dtype=np.float32)
    for core in range(NCORES):
        b, half = core // 2, core % 2
        out[b, half * TOK:(half + 1) * TOK] = res.results[core]["y"]
    return out
```

```python
import os
from contextlib import ExitStack
import numpy as np
import concourse.bass as bass
import concourse.mybir as mybir
from concourse.bass_utils import run_bass_kernel_spmd

F32 = mybir.dt.float32
BF16 = mybir.dt.bfloat16
I32 = mybir.dt.int32
ALU = mybir.AluOpType
AF = mybir.ActivationFunctionType
AX = mybir.AxisListType

D = 2048
NCORES = 8
TOK = 1024
NT = 8
NCH = 16
EPS = 1e-6
NEXP = 32
ENGS = ("pe", "act", "dve", "pool", "sp")


class Buf:
    __slots__ = ("name", "writer", "readers", "psum")

    def __init__(self, name):
        self.name = name
        self.writer = None
        self.readers = []
        self.psum = False


class Op:
    __slots__ = ("eng", "fn", "deps", "signal", "token", "is_dma")

    def __init__(self, eng, fn, is_dma):
        self.eng = eng
        self.fn = fn
        self.deps = []
        self.signal = False
        self.token = None
        self.is_dma = is_dma


class Prog:
    def __init__(self, nc):
        self.nc = nc
        self.streams = {e: [] for e in ENGS}
        self.dma_sems = {}
        self.last_dma = {}
        self.wait_all = set()

    def buf(self, name):
        return Buf(name)

    def bufs(self, name, n):
        return [Buf(f"{name}{i}") for i in range(n)]

    def _add(self, op, reads, writes):
        deps = []
        for b in reads:
            if b.writer is not None:
                deps.append(b.writer)
            if b.psum:
                deps.extend(r for r in b.readers if r.eng != op.eng)
        for b in writes:
            if b.writer is not None:
                deps.append(b.writer)
            deps.extend(b.readers)
        seen = set()
        for d in deps:
            if d is op or id(d) in seen:
                continue
            seen.add(id(d))
            if d.eng == "pe" and op.eng == "pe" and not d.is_dma and not op.is_dma:
                continue
            if d.is_dma:
                sname = d.token[0]
                cur = self.dma_sems[sname][1]
                if op.is_dma and op.token[0] == sname:
                    cur -= 16
                op.deps.append((sname, cur))
            else:
                op.deps.append(d)
                d.signal = True
        for b in reads:
            b.readers.append(op)
        for b in writes:
            b.writer = op
            b.readers = []
        self.streams[op.eng].append(op)
        return op

    def op(self, eng, fn, reads=(), writes=()):
        return self._add(Op(eng, fn, False), reads, writes)

    def dma(self, eng, fn, sem, reads=(), writes=()):
        op = Op(eng, fn, True)
        ent = self.dma_sems.setdefault(sem, [None, 0])
        ent[1] += 16
        op.token = (sem, ent[1])
        self.last_dma[sem] = op
        return self._add(op, reads, writes)

    def barrier(self):
        lasts = []
        for e in ENGS:
            for op in reversed(self.streams[e]):
                if not op.is_dma and op.fn is not None:
                    lasts.append(op)
                    break
        lasts.extend(self.last_dma.values())
        for e in ENGS:
            op = Op(e, None, False)
            for d in lasts:
                if d.is_dma:
                    op.deps.append((d.token[0], self.dma_sems[d.token[0]][1]))
                    continue
                if d.eng == e:
                    continue
                op.deps.append(d)
                d.signal = True
            self.streams[e].append(op)

    def _tok(self, d):
        if isinstance(d, tuple):
            s_, v = d
            if s_ in self.wait_all:
                v = self.dma_sems[s_][1]
            return s_, v
        return d.token

    def simulate(self):
        cnt = {e: 0 for e in ENGS}
        for e in ENGS:
            c = 0
            for op in self.streams[e]:
                if op.is_dma or op.fn is None:
                    continue
                if op.signal:
                    c += 1
                    op.token = (e, c)
        sem = {}
        pos = {e: 0 for e in ENGS}
        progress = True
        while progress:
            progress = False
            for e in ENGS:
                st = self.streams[e]
                while pos[e] < len(st):
                    op = st[pos[e]]
                    ok = True
                    for d in op.deps:
                        s_, v = self._tok(d)
                        if sem.get(s_, 0) < v:
                            ok = False
                            break
                    if not ok:
                        break
                    if op.fn is not None:
                        if op.is_dma:
                            sem[op.token[0]] = sem.get(op.token[0], 0) + 16
                        elif op.signal:
                            sem[e] = sem.get(e, 0) + 1
                    pos[e] += 1
                    progress = True
        stuck = {e: (pos[e], len(self.streams[e])) for e in ENGS if pos[e] < len(self.streams[e])}
        return stuck

    def emit(self, final_wait_ops=()):
        nc = self.nc
        stuck = self.simulate()
        if stuck:
            raise RuntimeError(f"semaphore protocol deadlock: {stuck}")
        with ExitStack() as es:
            eng_sem = {e: es.enter_context(nc.semaphore(f"s_{e}")) for e in ENGS}
            for name, ent in self.dma_sems.items():
                ent[0] = es.enter_context(nc.semaphore(f"d_{name}"))
            for e in ENGS:
                cnt = 0
                for op in self.streams[e]:
                    if op.is_dma or op.fn is None:
                        continue
                    if op.signal:
                        cnt += 1
                        op.token = (e, cnt)
            block = es.enter_context(nc.Block())

            def handle(s):
                return eng_sem[s] if s in eng_sem else self.dma_sems[s][0]

            def make(e):
                def body(eng):
                    known = {}
                    for op in self.streams[e]:
                        need = {}
                        for d in op.deps:
                            s, v = self._tok(d)
                            if v > need.get(s, 0):
                                need[s] = v
                        for s, v in need.items():
                            if known.get(s, 0) >= v:
                                continue
                            known[s] = v
                            eng.wait_ge(handle(s), v)
                        if op.fn is None:
                            continue
                        ins = op.fn(eng)
                        if op.is_dma:
                            ins.then_inc(self.dma_sems[op.token[0]][0], 16)
                        elif op.signal:
                            ins.then_inc(eng_sem[e], 1)
                    if e == "sp":
                        for op in final_wait_ops:
                            s = op.token[0]
                            eng.wait_ge(handle(s), self.dma_sems[s][1])
                return body

            block.tensor(make("pe"))
            block.scalar(make("act"))
            block.vector(make("dve"))
            block.gpsimd(make("pool"))
            block.sync(make("sp"))


def _mm(P, items, reads, writes):
    def fn(e):
        ins = None
        for (o, l, r, st, sp) in items:
            ins = e.matmul(out=o, lhsT=l, rhs=r, start=st, stop=sp)
        return ins
    return P.op("pe", fn, reads, writes)


class _Stop(Exception):
    pass


def build_program(debug=None):
    debug = debug or ()
    try:
        return _build_program(debug)
    except _Stop as ex:
        return ex.args[0]


def _build_program(debug):
    nc = bass.Bass("TRN2", target_bir_lowering=False)

    def din(name, shape, dt=F32):
        return nc.dram_tensor(name, list(shape), dt, kind="ExternalInput").ap()

    x_own = din("x_own", [TOK, D]); x_prev = din("x_prev", [TOK, D])
    cT_d = din("cT", [128, NCH]); flag_d = din("flag", [128, 1])
    w_ada = din("w_ada", [D, 6 * D]); b_adaT_d = din("b_adaT", [128, 96]); b_adag_d = din("b_adag", [1, 2 * D])
    n1T_d = din("n1T", [128, NCH]); n2T_d = din("n2T", [128, NCH]); nf_d = din("nf_bc", [128, D])
    w_in = din("w_in", [D, 13312]); convw_d = din("conv_wT", [128, 8, 3])
    w_co = din("w_conv_out", [1024, D]); w_ro = din("w_ret_out", [D, D]); w_o = din("w_o", [D, D])
    w_r = din("w_r", [D, 36]); b_r_d = din("b_r", [128, 36])
    if "nomoe" not in debug:
        wg_r = din("wg_r", [NEXP * 1024, D]); wu_r = din("wu_r", [NEXP * 1024, D]); wd_r = din("wd_r", [NEXP * 1024, D])
    erow_d = din("erow128", [128, NEXP])
    ltri_d = din("ltri", [128, 128]); jrow_d = din("jrow", [128, 48]); p8_d = din("p8", [128, 1]); j8_d = din("j8", [128, 8])
    xbuf = nc.dram_tensor("xbuf", [48 * 128, D], BF16, kind="Internal").ap()
    ybuf = nc.dram_tensor("ybuf", [48 * 128, D], F32, kind="Internal").ap()
    x1buf = nc.dram_tensor("x1buf", [TOK, D], F32, kind="Internal").ap()
    ident_d = din("ident", [128, 128])
    cs_own_d = din("cs_own", [128, NT, 128]); sn_own_d = din("sn_own", [128, NT, 128])
    cs_prev_d = din("cs_prev", [128, NT, 128]); sn_prev_d = din("sn_prev", [128, NT, 128])
    maskT_d = din("maskT", [128, 8, 128]); qdec_d = din("qdec", [128, 8, 128]); kdec_d = din("kdec", [128, 8])
    y_out = nc.dram_tensor("y", [TOK, D], F32, kind="ExternalOutput").ap()
    dbg_outs = {}

    log_g = np.log1p(-(2.0 ** (-5.0 - np.arange(8, dtype=np.float32)))).astype(np.float32)
    cdec = [float(np.exp(np.float32(log_g[h] * 128.0))) for h in range(8)]

    P = Prog(nc)
    fin = []

    def dump(name, ap2d, bufs, dt=F32):
        if name not in debug:
            return
        d = nc.dram_tensor("dbg_" + name, list(ap2d.shape), dt, kind="ExternalOutput").ap()
        fin.append(P.dma("sp", lambda e, d=d: e.dma_start(out=d[:, :], in_=ap2d), "st_" + name, reads=list(bufs)))

    _regcache = {}

    def breg(e, v):
        if v not in _regcache:
            _regcache[v] = e.to_reg(v)
        return _regcache[v]

    def stop(name):
        if name in debug:
            P.emit(final_wait_ops=fin)
            raise _Stop(nc)

    with ExitStack() as G:
        _cnt = [0]

        def S(name, shape, dt=F32, es=G):
            _cnt[0] += 1
            return es.enter_context(nc.sbuf_tensor(f"{name}_{_cnt[0]}", list(shape), dt))

        psb = [G.enter_context(nc.psum_tensor(f"psb{i}", [128, 512], F32)) for i in range(8)]
        pb = P.bufs("pb", 8)
        for b_ in pb:
            b_.psum = True

        ident = S("ident", [128, 128]); b_ident = P.buf("ident")
        ones_bf = S("ones_bf", [128, 128], BF16); b_ones = P.buf("ones")
        ones_row = S("ones_row", [1, 128]); b_onesr = P.buf("onesr")
        epst = S("epst", [128, 1]); b_eps = P.buf("eps")
        cTf = S("cTf", [128, NCH]); cact = S("cact", [128, NCH], BF16); b_cT = P.buf("cT"); b_cact = P.buf("cact")
        flag = S("flag", [128, 1]); b_flag = P.buf("flag")
        b_adaT = S("b_adaT", [128, 96]); b_badaT = P.buf("badaT")
        n1T = S("n1T", [128, NCH]); n2T = S("n2T", [128, NCH]); b_n1 = P.buf("n1"); b_n2 = P.buf("n2")
        mod = S("mod", [128, 96]); b_mod = P.buf("mod")
        A1 = S("A1", [128, NCH]); A2 = S("A2", [128, NCH]); b_A1 = P.buf("A1"); b_A2 = P.buf("A2")
        gbc = S("gbc", [128, 2 * D]); b_gbc = P.bufs("gbc", 8)
        convw = S("convw", [128, 8, 3]); b_convw = P.buf("convw")
        kdec = S("kdec", [128, 8]); kdecp = S("kdecp", [128, 8]); b_kdec = P.buf("kdec"); b_kdecp = P.buf("kdecp")
        b_rt = S("b_rt", [128, 36]); b_br = P.buf("br")
        wr = S("wr", [128, NCH, 36], BF16); b_wr = P.buf("wr")
        htail = S("htail", [128, NCH, 2], BF16); b_htail = P.buf("htail")
        sst = S("sst", [128, 4]); b_ss = P.bufs("ss", 2); b_t1 = P.bufs("t1", 2)
        rstd = S("rstd", [128, 2]); b_rstd = P.bufs("rstd", 2)

        def ld(dst, src, b, sem="ld0"):
            P.wait_all.add(sem)
            P.dma("sp", lambda e: e.dma_start(out=dst, in_=src), sem, writes=[b])

        ld(ident[:, :], ident_d[:, :], b_ident)
        ld(cTf[:, :], cT_d[:, :], b_cT)
        ld(flag[:, :], flag_d[:, :], b_flag)
        ld(b_adaT[:, :], b_adaT_d[:, :], b_badaT)
        ld(n1T[:, :], n1T_d[:, :], b_n1)
        ld(n2T[:, :], n2T_d[:, :], b_n2)
        ld(convw[:, :, :], convw_d[:, :, :], b_convw)
        ld(kdec[:, :], kdec_d[:, :], b_kdec)
        ld(b_rt[:, :], b_r_d[:, :], b_br)
        P.dma("pool", lambda e: e.dma_start(out=wr[:, :, :], in_=w_r.rearrange("(kt p) n -> p kt n", p=128)), "wr", writes=[b_wr])
        P.op("dve", lambda e: e.memset(ones_bf[:, :], 1.0), [], [b_ones])
        P.op("dve", lambda e: e.memset(ones_row[:, :], 1.0), [], [b_onesr])
        P.op("dve", lambda e: e.memset(epst[:, :], EPS), [], [b_eps])
        P.op("act", lambda e: e.activation(out=cact[:, :], in_=cTf[:, :], func=AF.Silu), [b_cT], [b_cact])
        P.op("dve", lambda e: e.tensor_scalar(out=kdecp[:, :], in0=kdec[:, :], scalar1=flag[:, 0:1], scalar2=None, op0=ALU.mult),
             [b_kdec, b_flag], [b_kdecp])

        with ExitStack() as E0:
            def _phase_E0():
                slots = [S(f"ada{i}", [128, NCH, 512], BF16, E0) for i in range(3)]
                b_slots = P.bufs("ada", 3)
                grow = S("grow", [1, 2 * D], es=E0); b_grow = P.bufs("grow", 8)
                badag = S("badag", [1, 2 * D], es=E0); b_badag = P.buf("badag")
                ld(badag[:, :], b_adag_d[:, :], b_badag)
                gi = 0
                for t in range(24):
                    s = t % 3
                    seg = t // 4
                    P.dma("pool", lambda e, t=t, s=s: e.dma_start(
                        out=slots[s][:, :, :], in_=w_ada[:, 512 * t:512 * (t + 1)].rearrange("(kt p) n -> p kt n", p=128)),
                        f"ada{s}", writes=[b_slots[s]])
                    if seg in (2, 5):
                        items = [(psb[1][0:1, :], cact[:, kt:kt + 1], slots[s][:, kt, :], kt == 0, kt == NCH - 1) for kt in range(NCH)]
                        _mm(P, items, [b_cact, b_slots[s]], [pb[1]])
                        P.op("dve", lambda e, gi=gi: e.tensor_tensor(out=grow[0:1, gi * 512:(gi + 1) * 512], in0=psb[1][0:1, :],
                                                                     in1=badag[0:1, gi * 512:(gi + 1) * 512], op=ALU.add),
                             [pb[1], b_badag], [b_grow[gi]])
                        gi += 1
                    else:
                        items = []
                        for blk in range(4):
                            j = 4 * t + blk
                            for kt in range(NCH):
                                items.append((psb[0][:, j:j + 1], slots[s][:, kt, blk * 128:(blk + 1) * 128], cact[:, kt:kt + 1],
                                              kt == 0, kt == NCH - 1))
                        _mm(P, items, [b_cact, b_slots[s]], [pb[0]])
                P.op("dve", lambda e: e.tensor_tensor(out=mod[:, 0:32], in0=psb[0][:, 0:32], in1=b_adaT[:, 0:32], op=ALU.add),
                     [pb[0], b_badaT], [b_mod])
                P.op("dve", lambda e: e.tensor_tensor(out=mod[:, 48:80], in0=psb[0][:, 48:80], in1=b_adaT[:, 48:80], op=ALU.add),
                     [pb[0], b_badaT, b_mod], [b_mod])
                P.op("dve", lambda e: e.scalar_tensor_tensor(out=A1[:, :], in0=mod[:, 16:32], scalar=1.0, in1=n1T[:, :], op0=ALU.add, op1=ALU.mult),
                     [b_mod, b_n1], [b_A1])
                P.op("dve", lambda e: e.scalar_tensor_tensor(out=A2[:, :], in0=mod[:, 64:80], scalar=1.0, in1=n2T[:, :], op0=ALU.add, op1=ALU.mult),
                     [b_mod, b_n2], [b_A2])
                for gi in range(8):
                    bk = 2 + gi % 2
                    _mm(P, [(psb[bk][:, :], ones_row[0:1, :], grow[0:1, gi * 512:(gi + 1) * 512], True, True)], [b_onesr, b_grow[gi]], [pb[bk]])
                    P.op("act", lambda e, gi=gi, bk=bk: e.copy(out=gbc[:, gi * 512:(gi + 1) * 512], in_=psb[bk][:, :]), [pb[bk]], [b_gbc[gi]])
                P.barrier()
            _phase_E0()
        B1 = mod[:, 0:16]
        B2 = mod[:, 48:64]
        if "mod" in debug:
            d = nc.dram_tensor("dbg_mod", [128, 96], F32, kind="ExternalOutput").ap()
            fin.append(P.dma("sp", lambda e, d=d: e.dma_start(out=d[:, :], in_=mod[:, :]), "st_mod", reads=[b_mod]))
            d = nc.dram_tensor("dbg_gbc", [128, 2 * D], F32, kind="ExternalOutput").ap()
            fin.append(P.dma("sp", lambda e, d=d: e.dma_start(out=d[:, :], in_=gbc[:, :]), "st_gbc", reads=b_gbc))
        if "stop0" in debug:
            P.emit(final_wait_ops=fin)
            return nc

        def make_hT(src_ap, b_src, Aap, Bap, b_AB, hT, b_hT_t, t, xs_slots, b_xs, junk, b_junk, k):
            s = k % 2
            P.op("act", lambda e: e.activation(out=junk[:, :], in_=src_ap, func=AF.Square, accum_out=sst[:, s:s + 1]),
                 b_src, [b_junk, b_ss[s]])
            P.op("dve", lambda e: e.tensor_scalar(out=sst[:, 2 + s:3 + s], in0=sst[:, s:s + 1], scalar1=1.0 / D, scalar2=EPS,
                                                  op0=ALU.mult, op1=ALU.add), [b_ss[s]], [b_t1[s]])
            P.op("act", lambda e: e.activation(out=sst[:, 2 + s:3 + s], in_=sst[:, 2 + s:3 + s], func=AF.Sqrt), [b_t1[s]], [b_t1[s]])
            P.op("dve", lambda e: e.reciprocal(out=rstd[:, s:s + 1], in_=sst[:, 2 + s:3 + s]), [b_t1[s]], [b_rstd[s]])
            xs = xs_slots[s]
            P.op("act", lambda e: e.activation(out=xs[:, :], in_=src_ap, func=AF.Copy, scale=rstd[:, s:s + 1]),
                 b_src + [b_rstd[s]], [b_xs[s]])
            for g in range(4):
                bk = g % 2

                def tr(e, g=g, bk=bk):
                    ins = None
                    for j in range(4):
                        c = 4 * g + j
                        ins = e.transpose(out=psb[bk][:, j * 128:(j + 1) * 128], in_=xs[:, c * 128:(c + 1) * 128], identity=ident[:, :])
                    return ins
                P.op("pe", tr, [b_xs[s], b_ident], [pb[bk]])
                if g % 2 == 0:
                    def ev(e, g=g, bk=bk):
                        ins = None
                        for j in range(4):
                            c = 4 * g + j
                            ins = e.activation(out=hT[:, c, t * 128:(t + 1) * 128], in_=psb[bk][:, j * 128:(j + 1) * 128],
                                               func=AF.Identity, scale=Aap[:, c:c + 1], bias=Bap[:, c:c + 1])
                        return ins
                    P.op("act", ev, [pb[bk], b_AB, b_mod], [b_hT_t[g]])
                else:
                    def ev(e, g=g, bk=bk):
                        ins = None
                        for j in range(4):
                            c = 4 * g + j
                            ins = e.tensor_scalar(out=hT[:, c, t * 128:(t + 1) * 128], in0=psb[bk][:, j * 128:(j + 1) * 128],
                                                  scalar1=Aap[:, c:c + 1], scalar2=Bap[:, c:c + 1], op0=ALU.mult, op1=ALU.add)
                        return ins
                    P.op("dve", ev, [pb[bk], b_AB, b_mod], [b_hT_t[g]])

        def rotary(src_ps, cs, sn, t, m1, m2, b_m, dst, b_dst, b_src, b_tab):
            P.op("dve", lambda e: e.tensor_tensor(out=m1[:, :], in0=src_ps, in1=cs[:, t, :], op=ALU.mult), [b_src] + b_tab, [b_m[0]])
            P.op("dve", lambda e: e.tensor_tensor(out=m2[:, :], in0=src_ps, in1=sn[:, t, :], op=ALU.mult), [b_src] + b_tab, [b_m[1]])
            P.op("dve", lambda e: e.tensor_tensor(out=dst[:, 0:64], in0=m1[:, 0:64], in1=m2[:, 64:128], op=ALU.subtract),
                 [b_m[0], b_m[1]], [b_dst[0]])
            P.op("dve", lambda e: e.tensor_tensor(out=dst[:, 64:128], in0=m2[:, 0:64], in1=m1[:, 64:128], op=ALU.add),
                 [b_m[0], b_m[1]], [b_dst[1]])

        hT = S("hT", [128, NCH, TOK], BF16)
        b_hT = [P.bufs(f"hT{t}_", 4) for t in range(NT)]
        mergedT = S("mergedT", [128, NCH, TOK], BF16)
        b_mg = [P.bufs(f"mg{d}_", 2) for d in range(NCH)]

        with ExitStack() as E12:
            Sst = S("Sst", [128, 8, 256], es=E12); b_S = P.bufs("S", 8)
            P.op("dve", lambda e: e.memset(Sst[:, :, :], 0.0), [], b_S)

            with ExitStack() as E1:
                def _phase_E1():
                    xt = [S(f"xt{i}", [128, D], es=E1) for i in range(2)]; b_xt = P.bufs("xt", 2)
                    xs_slots = [S(f"xs{i}", [128, D], es=E1) for i in range(2)]; b_xs = P.bufs("xs", 2)
                    junk = S("junk", [128, D], BF16, E1); b_junk = P.buf("junk")
                    csp = S("csp", [128, NT, 128], es=E1); snp = S("snp", [128, NT, 128], es=E1); b_tabp = P.bufs("tabp", 2)
                    ld(csp[:, :, :], cs_prev_d[:, :, :], b_tabp[0], "ld1")
                    ld(snp[:, :, :], sn_prev_d[:, :, :], b_tabp[1], "ld1")
                    kvp = [S(f"kvp{i}", [128, NCH, 384], BF16, E1) for i in range(2)]
                    b_kvp = [P.bufs(f"kvp{i}_", 2) for i in range(2)]
                    m1_ = [S(f"m1{i}", [128, 128], es=E1) for i in range(2)]; m2_ = [S(f"m2{i}", [128, 128], es=E1) for i in range(2)]
                    b_m_ = [P.bufs(f"m{i}_", 2) for i in range(2)]
                    krot_ = [S(f"krot{i}", [128, 128], es=E1) for i in range(2)]; b_krot_ = [P.bufs(f"krot{i}_", 2) for i in range(2)]
                    kd_ = [S(f"kd{i}", [128, 128], BF16, E1) for i in range(2)]; b_kd_ = P.bufs("kd", 2)
                    vb_ = [S(f"vb{i}", [128, 256], BF16, E1) for i in range(2)]; b_vb_ = P.bufs("vb", 2)
                    for t in range(NT):
                        s = t % 2
                        P.dma("sp", lambda e, t=t, s=s: e.dma_start(out=xt[s][:, :], in_=x_prev[t * 128:(t + 1) * 128, :]), f"xt{s}", writes=[b_xt[s]])
                        make_hT(xt[s][:, :], [b_xt[s]], A1, B1, b_A1, hT, b_hT[t], t, xs_slots, b_xs, junk, b_junk, t)
                    dump("hTp", hT[:, :, :].rearrange("p c t -> p (c t)"), [b for t in range(NT) for b in b_hT[t]], BF16)
                    stop("stop1a")
                    P.op("act", lambda e: e.copy(out=htail[:, :, :], in_=hT[:, :, TOK - 2:TOK]), b_hT[NT - 1], [b_htail])
                    stop("stop1b")
                    def p1A(h, t):
                        s = h % 2
                        if t == 0:
                            P.dma("pool", lambda e: e.dma_start(
                                out=kvp[s][:, :, 0:128], in_=w_in[:, 4096 + 128 * h:4096 + 128 * (h + 1)].rearrange("(kt p) n -> p kt n", p=128)),
                                f"kvp{s}", writes=[b_kvp[s][0]])
                            P.dma("pool", lambda e: e.dma_start(
                                out=kvp[s][:, :, 128:384], in_=w_in[:, 5120 + 256 * h:5120 + 256 * (h + 1)].rearrange("(kt p) n -> p kt n", p=128)),
                                f"kvp{s}", writes=[b_kvp[s][1]])
                        bk = 2 + t % 2
                        ip = t % 2
                        m1 = m1_[ip]; m2 = m2_[ip]; b_m = b_m_[ip]; krot = krot_[ip]; b_krot = b_krot_[ip]
                        kd = kd_[ip]; b_kd = b_kd_[ip]; vb = vb_[ip]; b_vb = b_vb_[ip]
                        items = [(psb[bk][:, 0:384], hT[:, c, t * 128:(t + 1) * 128], kvp[s][:, c, :], c == 0, c == NCH - 1) for c in range(NCH)]
                        _mm(P, items, b_hT[t] + b_kvp[s], [pb[bk]])
                        rotary(psb[bk][:, 0:128], csp, snp, t, m1, m2, b_m, krot, b_krot, pb[bk], b_tabp)
                        P.op("act", lambda e: e.copy(out=vb[:, :], in_=psb[bk][:, 128:384]), [pb[bk]], [b_vb])
                        P.op("act", lambda e: e.activation(out=kd[:, :], in_=krot[:, :], func=AF.Copy, scale=kdecp[:, h:h + 1]),
                             b_krot + [b_kdecp], [b_kd])

                    def p1B(h, t):
                        ip = t % 2
                        kd = kd_[ip]; b_kd = b_kd_[ip]; vb = vb_[ip]; b_vb = b_vb_[ip]
                        bk2 = 4 + t % 2
                        _mm(P, [(psb[bk2][:, 0:256], kd[:, :], vb[:, :], True, True)], [b_kd, b_vb], [pb[bk2]])
                        P.op("dve", lambda e: e.scalar_tensor_tensor(
                            out=Sst[:, h, :], in0=Sst[:, h, :], scalar=cdec[h], in1=psb[bk2][:, 0:256], op0=ALU.mult, op1=ALU.add),
                            [pb[bk2], b_S[h]], [b_S[h]])

                    seq1 = [(h, t) for h in range(8) for t in range(NT)]
                    for i in range(len(seq1) + 1):
                        if i < len(seq1):
                            p1A(*seq1[i])
                        if i >= 1:
                            p1B(*seq1[i - 1])
                    P.barrier()
                _phase_E1()

            if "stop1" in debug:
                dump("S", Sst[:, :, :].rearrange("p h e -> p (h e)"), b_S)
                stop("stop1")
            if "S" in debug:
                d = nc.dram_tensor("dbg_S", [128, 8 * 256], F32, kind="ExternalOutput").ap()
                fin.append(P.dma("sp", lambda e, d=d: e.dma_start(out=d[:, :], in_=Sst[:, :, :].rearrange("p h e -> p (h e)")), "st_S", reads=b_S))

            with ExitStack() as E2a:
                def _phase_E2a():
                    xt = [S(f"xt{i}", [128, D], es=E2a) for i in range(2)]; b_xt = P.bufs("xt", 2)
                    xs_slots = [S(f"xs{i}", [128, D], es=E2a) for i in range(2)]; b_xs = P.bufs("xs", 2)
                    junk = S("junk", [128, D], BF16, E2a); b_junk = P.buf("junk")
                    for t in range(NT):
                        s = t % 2
                        P.dma("sp", lambda e, t=t, s=s: e.dma_start(out=xt[s][:, :], in_=x_own[t * 128:(t + 1) * 128, :]), f"xt{s}", writes=[b_xt[s]])
                        make_hT(xt[s][:, :], [b_xt[s]], A1, B1, b_A1, hT, b_hT[t], t, xs_slots, b_xs, junk, b_junk, t)
                    P.barrier()
                _phase_E2a()
            all_hT = [b for t in range(NT) for b in b_hT[t]]

            if "hT" in debug:
                d = nc.dram_tensor("dbg_hT", [128, NCH * TOK], BF16, kind="ExternalOutput").ap()
                fin.append(P.dma("sp", lambda e, d=d: e.dma_start(out=d[:, :], in_=hT[:, :, :].rearrange("p c t -> p (c t)")), "st_hT", reads=all_hT))

            stop("stop2a")
            zbT = S("zbT", [128, NCH, TOK], BF16, E12); b_zb = [P.bufs(f"zb{c}_", NT) for c in range(NCH)]

            with ExitStack() as E2b:
                def _phase_E2b():
                    cso = S("cso", [128, NT, 128], es=E2b); sno = S("sno", [128, NT, 128], es=E2b); b_tabo = P.bufs("tabo", 2)
                    maskT = S("maskT", [128, 8, 128], es=E2b); qdec = S("qdec", [128, 8, 128], es=E2b); b_msk = P.buf("msk"); b_qdec = P.buf("qdecb")
                    ld(cso[:, :, :], cs_own_d[:, :, :], b_tabo[0], "ld2")
                    ld(sno[:, :, :], sn_own_d[:, :, :], b_tabo[1], "ld2")
                    ld(maskT[:, :, :], maskT_d[:, :, :], b_msk, "ld2")
                    ld(qdec[:, :, :], qdec_d[:, :, :], b_qdec, "ld2")
                    qkvp = [S(f"qkvp{i}", [128, NCH, 512], BF16, E2b) for i in range(2)]
                    b_qkvp = [P.bufs(f"qkvp{i}_", 3) for i in range(2)]
                    rgp = S("rgp", [128, NCH, 256], BF16, E2b); b_rgp = P.buf("rgp")
                    srg_ = [S(f"srg{i}", [128, 2, TOK], BF16, E2b) for i in range(2)]; b_srg_ = [P.bufs(f"srg{i}_", 4) for i in range(2)]
                    def two(name, shape, dt=F32):
                        return [S(f"{name}{i}", shape, dt, E2b) for i in range(2)]
                    mq1_ = two("mq1", [128, 128]); mq2_ = two("mq2", [128, 128]); b_mq_ = [P.bufs(f"mq{i}_", 2) for i in range(2)]
                    mk1_ = two("mk1", [128, 128]); mk2_ = two("mk2", [128, 128]); b_mk_ = [P.bufs(f"mk{i}_", 2) for i in range(2)]
                    qrot_ = two("qrot", [128, 128]); b_qrot_ = [P.bufs(f"qrot{i}_", 2) for i in range(2)]
                    krot_ = two("krot", [128, 128]); b_krot_ = [P.bufs(f"krot{i}_", 2) for i in range(2)]
                    kd_ = two("kd", [128, 128], BF16); b_kd_ = P.bufs("kd", 2)
                    vb_ = two("vb", [128, 256], BF16); b_vb_ = P.bufs("vb", 2)
                    qT_ = two("qT", [128, 128], BF16); qTd_ = two("qTd", [128, 128], BF16); kT_ = two("kT", [128, 128], BF16)
                    b_qT_ = P.bufs("qT", 2); b_qTd_ = P.bufs("qTd", 2); b_kT_ = P.bufs("kT", 2)
                    AT_ = two("AT", [128, 128], BF16); b_AT_ = P.bufs("AT", 2)
                    Sb = S("Sb", [128, 256], BF16, E2b); b_Sb = P.buf("Sb")
                    sq_ = two("sq", [128, 256], BF16); b_sq_ = P.bufs("sq", 2)
                    rbc_ = two("rbc", [128, 128]); b_rbc_ = P.bufs("rbc", 2)
                    ztmp_ = two("ztmp", [128, 2, 128]); b_ztmp_ = P.bufs("ztmp", 2)

                    def chunk(h, t, s, ip, stage):
                        srg = srg_[h % 2]; b_srg = b_srg_[h % 2]
                        pT_ = psb[ip]; r_T = pb[ip]
                        bsc = 6 if ip == 0 else 4
                        pSC = psb[bsc]; r_sc = pb[bsc]
                        brt = 7 if ip == 0 else 5
                        pRET = psb[brt]; r_ret = pb[brt]
                        mq1 = mq1_[ip]; mq2 = mq2_[ip]; b_mq = b_mq_[ip]; mk1 = mk1_[ip]; mk2 = mk2_[ip]; b_mk = b_mk_[ip]
                        qrot = qrot_[ip]; b_qrot = b_qrot_[ip]; krot = krot_[ip]; b_krot = b_krot_[ip]
                        kd = kd_[ip]; b_kd = b_kd_[ip]; vb = vb_[ip]; b_vb = b_vb_[ip]
                        qT = qT_[ip]; qTd = qTd_[ip]; kT = kT_[ip]; b_qT = b_qT_[ip]; b_qTd = b_qTd_[ip]; b_kT = b_kT_[ip]
                        AT = AT_[ip]; b_AT = b_AT_[ip]; sq = sq_[ip]; b_sq = b_sq_[ip]; rbc = rbc_[ip]; b_rbc = b_rbc_[ip]
                        ztmp = ztmp_[ip]; b_ztmp = b_ztmp_[ip]
                        bk = 2 + t % 2
                        if stage == "B":
                            return chunkB(locals())
                        items = [(psb[bk][:, :], hT[:, c, t * 128:(t + 1) * 128], qkvp[s][:, c, :], c == 0, c == NCH - 1) for c in range(NCH)]
                        _mm(P, items, b_hT[t] + b_qkvp[s], [pb[bk]])
                        rotary(psb[bk][:, 0:128], cso, sno, t, mq1, mq2, b_mq, qrot, b_qrot, pb[bk], b_tabo)
                        rotary(psb[bk][:, 128:256], cso, sno, t, mk1, mk2, b_mk, krot, b_krot, pb[bk], b_tabo)
                        P.op("act", lambda e: e.copy(out=vb[:, :], in_=psb[bk][:, 256:512]), [pb[bk]], [b_vb])
                        P.op("act", lambda e: e.activation(out=kd[:, :], in_=krot[:, :], func=AF.Copy, scale=kdec[:, h:h + 1]),
                             b_krot + [b_kdec], [b_kd])

                        def trq(e):
                            e.transpose(out=pT_[:, 0:128], in_=qrot[:, :], identity=ident[:, :])
                            return e.transpose(out=pT_[:, 128:256], in_=krot[:, :], identity=ident[:, :])
                        P.op("pe", trq, b_qrot + b_krot + [b_ident], [r_T])
                        P.op("act", lambda e: e.copy(out=qT[:, :], in_=pT_[:, 0:128]), [r_T], [b_qT])
                        P.op("act", lambda e: e.copy(out=kT[:, :], in_=pT_[:, 128:256]), [r_T], [b_kT])
                        P.op("dve", lambda e: e.tensor_tensor(out=qTd[:, :], in0=pT_[:, 0:128], in1=qdec[:, h, :], op=ALU.mult),
                             [r_T, b_qdec], [b_qTd])
                        _mm(P, [(pSC[:, 0:128], kT[:, :], qT[:, :], True, True)], [b_kT, b_qT], [r_sc])
                        P.op("dve", lambda e: e.tensor_tensor(out=AT[:, :], in0=pSC[:, 0:128], in1=maskT[:, h, :], op=ALU.mult),
                             [r_sc, b_msk], [b_AT])

                    def chunkB(L):
                        h = L["h"]; t = L["t"]; srg = L["srg"]; b_srg = L["b_srg"]
                        pSC = L["pSC"]; r_sc = L["r_sc"]; pRET = L["pRET"]; r_ret = L["r_ret"]
                        kd = L["kd"]; b_kd = L["b_kd"]; vb = L["vb"]; b_vb = L["b_vb"]; qTd = L["qTd"]; b_qTd = L["b_qTd"]
                        AT = L["AT"]; b_AT = L["b_AT"]; sq = L["sq"]; b_sq = L["b_sq"]; rbc = L["rbc"]; b_rbc = L["b_rbc"]
                        ztmp = L["ztmp"]; b_ztmp = L["b_ztmp"]
                        if t == 0:
                            P.op("act", lambda e: e.copy(out=Sb[:, :], in_=Sst[:, h, :]), [b_S[h]], [b_Sb])
                        items = []
                        for eb in range(2):
                            items.append((pRET[:, eb * 128:(eb + 1) * 128], vb[:, eb * 128:(eb + 1) * 128], AT[:, :], True, False))
                            items.append((pRET[:, eb * 128:(eb + 1) * 128], Sb[:, eb * 128:(eb + 1) * 128], qTd[:, :], False, True))
                        _mm(P, items, [b_vb, b_AT, b_Sb, b_qTd], [r_ret])
                        P.op("act", lambda e: e.activation(out=sq[:, :], in_=pRET[:, 0:256], func=AF.Square), [r_ret], [b_sq])
                        _mm(P, [(pSC[:, 128:256], ones_bf[:, :], sq[:, 0:128], True, False),
                                (pSC[:, 128:256], ones_bf[:, :], sq[:, 128:256], False, True)], [b_ones, b_sq], [r_sc])
                        P.op("dve", lambda e: e.tensor_scalar(out=rbc[:, :], in0=pSC[:, 128:256], scalar1=1.0 / 256, scalar2=EPS,
                                                              op0=ALU.mult, op1=ALU.add), [r_sc], [b_rbc])
                        P.op("act", lambda e: e.activation(out=rbc[:, :], in_=rbc[:, :], func=AF.Sqrt), [b_rbc], [b_rbc])
                        P.op("dve", lambda e: e.reciprocal(out=rbc[:, :], in_=rbc[:, :]), [b_rbc], [b_rbc])
                        P.op("dve", lambda e: e.tensor_tensor(out=ztmp[:, :, :], in0=pRET[:, 0:256].rearrange("p (a i) -> p a i", a=2),
                                                              in1=rbc[:, :].unsqueeze(1).to_broadcast([128, 2, 128]), op=ALU.mult),
                             [r_ret, b_rbc], [b_ztmp])
                        P.op("dve", lambda e: e.tensor_tensor(out=zbT[:, 2 * h:2 * h + 2, t * 128:(t + 1) * 128], in0=ztmp[:, :, :],
                                                              in1=srg[:, :, t * 128:(t + 1) * 128], op=ALU.mult),
                             [b_ztmp] + b_srg, [b_zb[2 * h][t], b_zb[2 * h + 1][t]])
                        if t < NT - 1:
                            _mm(P, [(pRET[:, 256:512], kd[:, :], vb[:, :], True, True)], [b_kd, b_vb], [r_ret])
                            P.op("dve", lambda e: e.scalar_tensor_tensor(
                                out=Sst[:, h, :], in0=Sst[:, h, :], scalar=cdec[h], in1=pRET[:, 256:512], op0=ALU.mult, op1=ALU.add),
                                [r_ret, b_S[h]], [b_S[h]])
                            P.op("act", lambda e: e.copy(out=Sb[:, :], in_=Sst[:, h, :]), [b_S[h]], [b_Sb])

                    def prologue(h):
                        s = h % 2
                        srg = srg_[h % 2]; b_srg = b_srg_[h % 2]
                        for i, (c0, n, off) in enumerate([(3072 + 128 * h, 128, 0), (4096 + 128 * h, 128, 128), (5120 + 256 * h, 256, 256)]):
                            P.dma("pool", lambda e, s=s, c0=c0, n=n, off=off: e.dma_start(
                                out=qkvp[s][:, :, off:off + n], in_=w_in[:, c0:c0 + n].rearrange("(kt p) n -> p kt n", p=128)),
                                f"qkvp{s}", writes=[b_qkvp[s][i]])
                        P.dma("pool", lambda e, h=h: e.dma_start(
                            out=rgp[:, :, :], in_=w_in[:, 7168 + 256 * h:7168 + 256 * (h + 1)].rearrange("(kt p) n -> p kt n", p=128)),
                            "rgp", writes=[b_rgp])
                        for eb in range(2):
                            for th in range(2):
                                bk = 4 + th
                                items = [(psb[bk][:, :], rgp[:, c, eb * 128:(eb + 1) * 128], hT[:, c, th * 512:(th + 1) * 512], c == 0, c == NCH - 1)
                                         for c in range(NCH)]
                                _mm(P, items, all_hT + [b_rgp], [pb[bk]])
                                P.op("act", lambda e, eb=eb, th=th, bk=bk: e.activation(out=srg[:, eb, th * 512:(th + 1) * 512], in_=psb[bk][:, :], func=AF.Silu),
                                     [pb[bk]], [b_srg[eb * 2 + th]])

                    seq2 = [(h, t) for h in range(8) for t in range(NT)]
                    for i in range(len(seq2) + 1):
                        if i < len(seq2):
                            h, t = seq2[i]
                            if t == 0:
                                prologue(h)
                            chunk(h, t, h % 2, t % 2, "A")
                        if i >= 1:
                            h, t = seq2[i - 1]
                            chunk(h, t, h % 2, t % 2, "B")
                    P.barrier()
                _phase_E2b()
            all_zb = [b for c in range(NCH) for b in b_zb[c]]
            if "zbT" in debug:
                d = nc.dram_tensor("dbg_zbT", [128, NCH * TOK], BF16, kind="ExternalOutput").ap()
                fin.append(P.dma("sp", lambda e, d=d: e.dma_start(out=d[:, :], in_=zbT[:, :, :].rearrange("p c t -> p (c t)")), "st_zb", reads=all_zb))

            stop("stop2b")
            zaT = S("zaT", [128, 8, TOK], BF16, E12); b_za = P.bufs("za", 8)

            with ExitStack() as E2c:
                def _phase_E2c():
                    cvp = [S(f"cvp{i}", [128, NCH, 384], BF16, E2c) for i in range(2)]
                    b_cvp = [P.bufs(f"cvp{i}_", 3) for i in range(2)]
                    ccs = S("ccs", [128, TOK], es=E2c); b_ccs = P.bufs("ccs", 2)
                    tt = S("tt", [128, TOK + 2], es=E2c); b_tt = P.bufs("tt", 3)
                    hcc = S("hcc", [128, 2], es=E2c); b_hcc = P.buf("hcc")
                    acc = S("acc", [128, TOK], es=E2c); b_acc = P.buf("acc")
                    r_h = pb[6]
                    for cb in range(8):
                        s = cb % 2
                        for i in range(3):
                            P.dma("pool", lambda e, s=s, i=i, cb=cb: e.dma_start(
                                out=cvp[s][:, :, i * 128:(i + 1) * 128],
                                in_=w_in[:, 1024 * i + 128 * cb:1024 * i + 128 * (cb + 1)].rearrange("(kt p) n -> p kt n", p=128)),
                                f"cvp{s}", writes=[b_cvp[s][i]])
                        items = [(psb[6][:, 0:2], cvp[s][:, c, 128:256], htail[:, c, :], c == 0, c == NCH - 1) for c in range(NCH)]
                        items += [(psb[6][:, 2:4], cvp[s][:, c, 256:384], htail[:, c, :], c == 0, c == NCH - 1) for c in range(NCH)]
                        _mm(P, items, [b_htail] + b_cvp[s], [r_h])
                        P.op("act", lambda e: e.activation(out=hcc[:, :], in_=psb[6][:, 0:2], func=AF.Copy, scale=flag[:, 0:1]), [r_h, b_flag], [b_hcc])
                        P.op("dve", lambda e: e.tensor_tensor(out=tt[:, 0:2], in0=hcc[:, :], in1=psb[6][:, 2:4], op=ALU.mult), [r_h, b_hcc], [b_tt[2]])
                        for th in range(2):
                            bk = 2 + th
                            items = [(psb[bk][:, :], cvp[s][:, c, 128:256], hT[:, c, th * 512:(th + 1) * 512], c == 0, c == NCH - 1) for c in range(NCH)]
                            _mm(P, items, all_hT + b_cvp[s], [pb[bk]])
                            P.op("act", lambda e, th=th, bk=bk: e.copy(out=ccs[:, th * 512:(th + 1) * 512], in_=psb[bk][:, :]), [pb[bk]], [b_ccs[th]])
                        for th in range(2):
                            bk = 4 + th
                            items = [(psb[bk][:, :], cvp[s][:, c, 256:384], hT[:, c, th * 512:(th + 1) * 512], c == 0, c == NCH - 1) for c in range(NCH)]
                            _mm(P, items, all_hT + b_cvp[s], [pb[bk]])
                            P.op("dve", lambda e, th=th, bk=bk: e.tensor_tensor(out=tt[:, 2 + th * 512:2 + (th + 1) * 512], in0=ccs[:, th * 512:(th + 1) * 512],
                                                                                in1=psb[bk][:, :], op=ALU.mult), [pb[bk], b_ccs[th]], [b_tt[th]])
                        P.op("dve", lambda e, cb=cb: e.tensor_scalar(out=acc[:, :], in0=tt[:, 0:TOK], scalar1=convw[:, cb, 0:1], scalar2=None, op0=ALU.mult),
                             b_tt + [b_convw], [b_acc])
                        P.op("dve", lambda e, cb=cb: e.scalar_tensor_tensor(out=acc[:, :], in0=tt[:, 1:TOK + 1], scalar=convw[:, cb, 1:2], in1=acc[:, :],
                                                                            op0=ALU.mult, op1=ALU.add), b_tt + [b_convw, b_acc], [b_acc])
                        P.op("dve", lambda e, cb=cb: e.scalar_tensor_tensor(out=acc[:, :], in0=tt[:, 2:TOK + 2], scalar=convw[:, cb, 2:3], in1=acc[:, :],
                                                                            op0=ALU.mult, op1=ALU.add), b_tt + [b_convw, b_acc], [b_acc])
                        for th in range(2):
                            bk = 2 + th
                            items = [(psb[bk][:, :], cvp[s][:, c, 0:128], hT[:, c, th * 512:(th + 1) * 512], c == 0, c == NCH - 1) for c in range(NCH)]
                            _mm(P, items, all_hT + b_cvp[s], [pb[bk]])
                            P.op("dve", lambda e, th=th, bk=bk, cb=cb: e.tensor_tensor(out=zaT[:, cb, th * 512:(th + 1) * 512], in0=acc[:, th * 512:(th + 1) * 512],
                                                                                       in1=psb[bk][:, :], op=ALU.mult), [pb[bk], b_acc], [b_za[cb]])
                    P.barrier()
                _phase_E2c()
            if "zaT" in debug:
                d = nc.dram_tensor("dbg_zaT", [128, 8 * TOK], BF16, kind="ExternalOutput").ap()
                fin.append(P.dma("sp", lambda e, d=d: e.dma_start(out=d[:, :], in_=zaT[:, :, :].rearrange("p c t -> p (c t)")), "st_za", reads=b_za))

            stop("stop2c")
            with ExitStack() as E2d:
                def _phase_E2d():
                    opp = [S(f"opp{i}", [128, 56, 128], BF16, E2d) for i in range(2)]
                    b_opp = [P.bufs(f"opp{i}_", 4) for i in range(2)]
                    sga = [S(f"sga{i}", [128, 512], es=E2d) for i in range(2)]; b_sga = P.bufs("sga", 2)
                    sgb = [S(f"sgb{i}", [128, 512], es=E2d) for i in range(2)]; b_sgb = P.bufs("sgb", 2)
                    ma = [S(f"ma{i}", [128, 512], es=E2d) for i in range(2)]; b_ma = P.bufs("ma", 2)
                    mb = [S(f"mb{i}", [128, 512], es=E2d) for i in range(2)]; b_mb = P.bufs("mb", 2)
                    banks = [(2, 3, 4, 5), (0, 1, 6, 7)]
                    for db in range(NCH):
                        s = db % 2
                        srcs = [(w_co[:, db * 128:(db + 1) * 128], 0, 8), (w_ro[:, db * 128:(db + 1) * 128], 8, 16),
                                (w_in[:, 9216 + db * 128:9216 + (db + 1) * 128], 24, 16), (w_in[:, 11264 + db * 128:11264 + (db + 1) * 128], 40, 16)]
                        for i, (src, off, n) in enumerate(srcs):
                            P.dma("pool", lambda e, s=s, src=src, off=off, n=n: e.dma_start(
                                out=opp[s][:, off:off + n, :], in_=src.rearrange("(kt p) n -> p kt n", p=128)), f"opp{s}", writes=[b_opp[s][i]])
                        for th in range(2):
                            ba, bb, bga, bgb = banks[th]
                            tsl = slice(th * 512, (th + 1) * 512)
                            _mm(P, [(psb[ba][:, :], opp[s][:, c, :], zaT[:, c, tsl], c == 0, c == 7) for c in range(8)], b_za + [b_opp[s][0]], [pb[ba]])
                            _mm(P, [(psb[bb][:, :], opp[s][:, 8 + c, :], zbT[:, c, tsl], c == 0, c == 15) for c in range(16)], all_zb + [b_opp[s][1]], [pb[bb]])
                            _mm(P, [(psb[bga][:, :], opp[s][:, 24 + c, :], hT[:, c, tsl], c == 0, c == 15) for c in range(16)], all_hT + [b_opp[s][2]], [pb[bga]])
                            _mm(P, [(psb[bgb][:, :], opp[s][:, 40 + c, :], hT[:, c, tsl], c == 0, c == 15) for c in range(16)], all_hT + [b_opp[s][3]], [pb[bgb]])
                            P.op("act", lambda e, th=th, bga=bga: e.activation(out=sga[th][:, :], in_=psb[bga][:, :], func=AF.Sigmoid), [pb[bga]], [b_sga[th]])
                            P.op("act", lambda e, th=th, bgb=bgb: e.activation(out=sgb[th][:, :], in_=psb[bgb][:, :], func=AF.Sigmoid), [pb[bgb]], [b_sgb[th]])
                            P.op("dve", lambda e, th=th, ba=ba: e.tensor_tensor(out=ma[th][:, :], in0=sga[th][:, :], in1=psb[ba][:, :], op=ALU.mult),
                                 [pb[ba], b_sga[th]], [b_ma[th]])
                            P.op("dve", lambda e, th=th, bb=bb: e.tensor_tensor(out=mb[th][:, :], in0=sgb[th][:, :], in1=psb[bb][:, :], op=ALU.mult),
                                 [pb[bb], b_sgb[th]], [b_mb[th]])
                            P.op("dve", lambda e, th=th, db=db, tsl=tsl: e.tensor_tensor(out=mergedT[:, db, tsl], in0=ma[th][:, :], in1=mb[th][:, :], op=ALU.add),
                                 [b_ma[th], b_mb[th]], [b_mg[db][th]])
                    P.barrier()
                _phase_E2d()
        all_mg = [b for d_ in range(NCH) for b in b_mg[d_]]
        if "mergedT" in debug:
            d = nc.dram_tensor("dbg_mergedT", [128, NCH * TOK], BF16, kind="ExternalOutput").ap()
            fin.append(P.dma("sp", lambda e, d=d: e.dma_start(out=d[:, :], in_=mergedT[:, :, :].rearrange("p c t -> p (c t)")), "st_mg", reads=all_mg))

        stop("stop2d")
        desti = [S(f"desti{k}", [128, NT], I32) for k in range(2)]; b_desti = P.buf("desti")
        cc = S("cc", [128, NT, 2]); b_cc = P.bufs("cc", NT)
        idxw = S("idxw", [128, 16 * 8], I32); b_idxw = P.buf("idxw")
        ident_bf = S("ident_bf", [128, 128], BF16); b_identb = P.buf("identb")
        P.op("act", lambda e: e.copy(out=ident_bf[:, :], in_=ident[:, :]), [b_ident], [b_identb])
        b_x1buf = P.bufs("x1buf", NT)
        with ExitStack() as EX1:
            x1 = S("x1", [128, NT, D], es=EX1); b_x1 = [P.bufs(f"x1_{t}_", 4) for t in range(NT)]
            P.wait_all.add("x1ld")
            for t in range(NT):
                P.dma("sp", lambda e, t=t: e.dma_start(out=x1[:, t, :], in_=x_own[t * 128:(t + 1) * 128, :]), "x1ld", writes=b_x1[t])
            with ExitStack() as E3:
                def _phase_E3():
                    wop = [S(f"wop{i}", [128, NCH, 256], BF16, E3) for i in range(2)]; b_wop = P.bufs("wop", 2)
                    tmp = [S(f"tmp{i}", [128, 256], es=E3) for i in range(2)]; b_tmp = P.bufs("tmp", 2)
                    xs_slots = [S(f"xs{i}", [128, D], es=E3) for i in range(2)]; b_xs = P.bufs("xs", 2)
                    junk = S("junk", [128, D], BF16, E3); b_junk = P.buf("junk")
                    ltri_f = S("ltri_f", [128, 128], es=E3); ltri = S("ltri", [128, 128], BF16, E3); b_ltf = P.buf("ltf"); b_ltri = P.buf("ltri")
                    jrow = S("jrow", [128, 48], es=E3); p8 = S("p8", [128, 1], es=E3); j8 = S("j8", [128, 8], es=E3)
                    b_jrow = P.buf("jrow"); b_p8 = P.buf("p8"); b_j8 = P.buf("j8")
                    ld(ltri_f[:, :], ltri_d[:, :], b_ltf, "ld3"); ld(jrow[:, :], jrow_d[:, :], b_jrow, "ld3")
                    ld(p8[:, :], p8_d[:, :], b_p8, "ld3"); ld(j8[:, :], j8_d[:, :], b_j8, "ld3")
                    erow = S("erow", [128, NEXP], es=E3); b_erow = P.buf("erow")
                    ld(erow[:, :], erow_d[:, :], b_erow, "ld3")
                    P.op("act", lambda e: e.copy(out=ltri[:, :], in_=ltri_f[:, :]), [b_ltf], [b_ltri])
                    k = 0
                    for nb in range(8):
                        s = nb % 2
                        P.dma("pool", lambda e, s=s, nb=nb: e.dma_start(
                            out=wop[s][:, :, :], in_=w_o[:, nb * 256:(nb + 1) * 256].rearrange("(kt p) n -> p kt n", p=128)), f"wop{s}", writes=[b_wop[s]])
                        for t in range(NT):
                            bk = 2 + k % 2
                            q = nb // 2
                            _mm(P, [(psb[bk][:, 0:256], mergedT[:, c, t * 128:(t + 1) * 128], wop[s][:, c, :], c == 0, c == NCH - 1) for c in range(NCH)],
                                all_mg + [b_wop[s]], [pb[bk]])
                            P.op("dve", lambda e, bk=bk, nb=nb, k=k: e.tensor_tensor(out=tmp[k % 2][:, :], in0=psb[bk][:, 0:256], in1=gbc[:, nb * 256:(nb + 1) * 256], op=ALU.mult),
                                 [pb[bk], b_gbc[nb // 2]], [b_tmp[k % 2]])
                            P.op("dve", lambda e, t=t, nb=nb, k=k: e.tensor_tensor(out=x1[:, t, nb * 256:(nb + 1) * 256], in0=x1[:, t, nb * 256:(nb + 1) * 256],
                                                                                   in1=tmp[k % 2][:, :], op=ALU.add), [b_tmp[k % 2], b_x1[t][q]], [b_x1[t][q]])
                            k += 1
                    if "x1" in debug:
                        d = nc.dram_tensor("dbg_x1", [TOK, D], F32, kind="ExternalOutput").ap()
                        for t in range(NT):
                            fin.append(P.dma("sp", lambda e, t=t, d=d: e.dma_start(out=d[t * 128:(t + 1) * 128, :], in_=x1[:, t, :]), f"st_x1{t}", reads=b_x1[t]))
                    for t in range(NT):
                        P.dma("sp", lambda e, t=t: e.dma_start(out=x1buf[t * 128:(t + 1) * 128, :], in_=x1[:, t, :]), "x1st", reads=b_x1[t], writes=[b_x1buf[t]])
                    lg = S("lg", [128, 36], es=E3); b_lg = P.buf("lg")
                    rs = S("rs", [128, 16], es=E3); b_rs = P.bufs("rs", 16)
                    ohg = S("ohg", [128, 4], es=E3); b_ohg = P.buf("ohg")
                    egj = S("egj", [128, 4], es=E3); b_egj = P.buf("egj")
                    selm = S("selm", [128, 4, 8], es=E3); b_selm = P.buf("selm")
                    sel = S("sel", [128, 8], es=E3); sel2 = S("sel2", [128, 8], es=E3); b_sel = P.buf("sel"); b_sel2 = P.buf("sel2")
                    oh1 = S("oh1", [128, 8], es=E3); oh2 = S("oh2", [128, 8], es=E3); b_oh1 = P.buf("oh1"); b_oh2 = P.buf("oh2")
                    Mk = S("Mk", [128, NT, 2, NEXP], es=E3); b_Mk = [P.bufs(f"Mk{t}_", 2) for t in range(NT)]
                    M12 = S("M12", [128, NT, NEXP], BF16, E3); b_M12 = P.bufs("M12", NT)
                    for t in range(NT):
                        make_hT(x1[:, t, :], b_x1[t], A2, B2, b_A2, hT, b_hT[t], t, xs_slots, b_xs, junk, b_junk, t)
                        _mm(P, [(psb[4][:, 0:36], hT[:, c, t * 128:(t + 1) * 128], wr[:, c, :], c == 0, c == NCH - 1) for c in range(NCH)],
                            b_hT[t] + [b_wr], [pb[4]])
                        P.op("dve", lambda e: e.tensor_tensor(out=lg[:, :], in0=psb[4][:, 0:36], in1=b_rt[:, :], op=ALU.add), [pb[4], b_br], [b_lg])
                        P.op("dve", lambda e: e.reduce_max(out=rs[:, 0:1], in_=lg[:, 0:4], axis=AX.X), [b_lg], [b_rs[0]])
                        P.op("dve", lambda e: e.tensor_scalar(out=ohg[:, :], in0=lg[:, 0:4], scalar1=rs[:, 0:1], scalar2=None, op0=ALU.is_equal), [b_lg, b_rs[0]], [b_ohg])
                        P.op("dve", lambda e: e.tensor_scalar(out=rs[:, 1:2], in0=rs[:, 0:1], scalar1=-1.0, scalar2=None, op0=ALU.mult), [b_rs[0]], [b_rs[1]])
                        P.op("act", lambda e: e.activation(out=egj[:, :], in_=lg[:, 0:4], func=AF.Exp, bias=rs[:, 1:2], scale=1.0, accum_out=rs[:, 2:3]),
                             [b_lg, b_rs[1]], [b_egj, b_rs[2]])
                        P.op("dve", lambda e: e.reciprocal(out=rs[:, 3:4], in_=rs[:, 2:3]), [b_rs[2]], [b_rs[3]])
                        P.op("dve", lambda e: e.tensor_tensor(out=selm[:, :, :], in0=lg[:, 4:36].rearrange("p (g j) -> p g j", g=4),
                                                              in1=ohg[:, :].unsqueeze(2).to_broadcast([128, 4, 8]), op=ALU.mult), [b_lg, b_ohg], [b_selm])
                        P.op("dve", lambda e: e.tensor_reduce(out=sel[:, :], in_=selm[:, :, :].rearrange("p g j -> p j g"), axis=AX.X, op=ALU.add), [b_selm], [b_sel])
                        P.op("dve", lambda e: e.reduce_max(out=rs[:, 4:5], in_=sel[:, :], axis=AX.X), [b_sel], [b_rs[4]])
                        P.op("dve", lambda e: e.tensor_scalar(out=oh1[:, :], in0=sel[:, :], scalar1=rs[:, 4:5], scalar2=None, op0=ALU.is_equal), [b_sel, b_rs[4]], [b_oh1])
                        P.op("dve", lambda e: e.scalar_tensor_tensor(out=sel2[:, :], in0=oh1[:, :], scalar=-1e30, in1=sel[:, :], op0=ALU.mult, op1=ALU.add),
                             [b_oh1, b_sel], [b_sel2])
                        P.op("dve", lambda e: e.reduce_max(out=rs[:, 5:6], in_=sel2[:, :], axis=AX.X), [b_sel2], [b_rs[5]])
                        P.op("dve", lambda e: e.tensor_scalar(out=oh2[:, :], in0=sel2[:, :], scalar1=rs[:, 5:6], scalar2=None, op0=ALU.is_equal), [b_sel2, b_rs[5]], [b_oh2])
                        P.op("dve", lambda e: e.tensor_tensor(out=rs[:, 6:7], in0=rs[:, 5:6], in1=rs[:, 4:5], op=ALU.subtract), [b_rs[4], b_rs[5]], [b_rs[6]])
                        P.op("act", lambda e: e.activation(out=rs[:, 7:8], in_=rs[:, 6:7], func=AF.Exp), [b_rs[6]], [b_rs[7]])
                        P.op("dve", lambda e: e.tensor_scalar(out=rs[:, 8:9], in0=rs[:, 7:8], scalar1=1.0, scalar2=None, op0=ALU.add), [b_rs[7]], [b_rs[8]])
                        P.op("dve", lambda e: e.reciprocal(out=rs[:, 9:10], in_=rs[:, 8:9]), [b_rs[8]], [b_rs[9]])
                        P.op("dve", lambda e: e.tensor_tensor(out=rs[:, 10:11], in0=rs[:, 9:10], in1=rs[:, 3:4], op=ALU.mult), [b_rs[9], b_rs[3]], [b_rs[10]])
                        P.op("dve", lambda e: e.tensor_tensor(out=rs[:, 11:12], in0=rs[:, 10:11], in1=rs[:, 7:8], op=ALU.mult), [b_rs[10], b_rs[7]], [b_rs[11]])
                        P.op("act", lambda e, t=t: e.copy(out=cc[:, t, :], in_=rs[:, 10:12]), [b_rs[10], b_rs[11]], [b_cc[t]])
                        P.op("dve", lambda e, t=t: e.tensor_tensor(out=Mk[:, t, 0, :].rearrange("p (g j) -> p g j", g=4),
                                                                   in0=ohg[:, :].unsqueeze(2).to_broadcast([128, 4, 8]),
                                                                   in1=oh1[:, :].unsqueeze(1).to_broadcast([128, 4, 8]), op=ALU.mult), [b_ohg, b_oh1], [b_Mk[t][0]])
                        P.op("dve", lambda e, t=t: e.tensor_tensor(out=Mk[:, t, 1, :].rearrange("p (g j) -> p g j", g=4),
                                                                   in0=ohg[:, :].unsqueeze(2).to_broadcast([128, 4, 8]),
                                                                   in1=oh2[:, :].unsqueeze(1).to_broadcast([128, 4, 8]), op=ALU.mult), [b_ohg, b_oh2], [b_Mk[t][1]])
                        P.op("dve", lambda e, t=t: e.tensor_tensor(out=M12[:, t, :], in0=Mk[:, t, 0, :], in1=Mk[:, t, 1, :], op=ALU.add), b_Mk[t], [b_M12[t]])
                    rank = S("rank", [128, NT, NEXP], es=E3); b_rank = P.buf("rank")
                    cnt = S("cnt", [128, NEXP], es=E3); b_cnt = P.buf("cnt")
                    nblk = S("nblk", [128, NEXP], es=E3); b_nblk = P.buf("nblk")
                    sa = S("sa", [128, NEXP], es=E3); sb_ = S("sb", [128, NEXP], es=E3); b_sa = P.buf("sa"); b_sb = P.buf("sb")
                    pst = S("pst", [128, NEXP], es=E3); b_pst = P.buf("pst")
                    base = S("base", [128, NT, NEXP], es=E3); b_base = P.buf("base")
                    prod = S("prod", [128, NT, NEXP], es=E3); b_prod = P.buf("prod")
                    destf = [S(f"destf{k}", [128, NT], es=E3) for k in range(2)]; b_destf = P.bufs("destf", 2)
                    cmp_ = S("cmp", [128, 48, NEXP], es=E3); b_cmp = P.buf("cmp")
                    blke = S("blke", [128, 48], es=E3); b_blke = P.buf("blke")
                    valid = S("valid", [128, 48], es=E3); b_valid = P.buf("valid")
                    tmpi = S("tmpi", [128, 48], es=E3); b_tmpi = P.buf("tmpi")
                    idxf = S("idxf", [128, 48, 8], es=E3); b_idxf = P.buf("idxf")
                    items = []
                    for t in range(NT):
                        for t2 in range(t):
                            items.append((psb[2][:, t * 32:(t + 1) * 32], ones_bf[:, :], M12[:, t2, :], t2 == 0, False))
                        items.append((psb[2][:, t * 32:(t + 1) * 32], ltri[:, :], M12[:, t, :], t == 0, True))
                    _mm(P, items, b_M12 + [b_ones, b_ltri], [pb[2]])
                    P.op("dve", lambda e: e.tensor_copy(out=rank[:, :, :].rearrange("p t e -> p (t e)"), in_=psb[2][:, 0:256]), [pb[2]], [b_rank])
                    _mm(P, [(psb[3][:, 0:32], ones_bf[:, :], M12[:, t, :], t == 0, t == NT - 1) for t in range(NT)], b_M12 + [b_ones], [pb[3]])
                    P.op("dve", lambda e: e.tensor_copy(out=cnt[:, :], in_=psb[3][:, 0:32]), [pb[3]], [b_cnt])
                    P.op("dve", lambda e: e.tensor_scalar(out=nblk[:, :], in0=cnt[:, :], scalar1=0.0, scalar2=None, op0=ALU.is_gt), [b_cnt], [b_nblk])
                    for kk in range(1, 8):
                        P.op("dve", lambda e, kk=kk: e.scalar_tensor_tensor(out=nblk[:, :], in0=cnt[:, :], scalar=128.0 * kk, in1=nblk[:, :],
                                                                            op0=ALU.is_gt, op1=ALU.add), [b_cnt, b_nblk], [b_nblk])
                    ovb = S("ovb", [128, NEXP], es=E3); b_ovb = P.buf("ovb")
                    P.op("dve", lambda e: e.tensor_scalar(out=ovb[:, :], in0=nblk[:, :], scalar1=-1.0, scalar2=0.0, op0=ALU.add, op1=ALU.max), [b_nblk], [b_ovb])
                    P.op("dve", lambda e: e.tensor_copy(out=sa[:, :], in_=ovb[:, :]), [b_ovb], [b_sa])
                    cur, nxt, bc_, bn_ = sa, sb_, b_sa, b_sb
                    for sh in (1, 2, 4, 8, 16):
                        P.op("dve", lambda e, cur=cur, nxt=nxt, sh=sh: e.tensor_copy(out=nxt[:, 0:sh], in_=cur[:, 0:sh]), [bc_], [bn_])
                        P.op("dve", lambda e, cur=cur, nxt=nxt, sh=sh: e.tensor_tensor(out=nxt[:, sh:NEXP], in0=cur[:, sh:NEXP], in1=cur[:, 0:NEXP - sh], op=ALU.add),
                             [bc_, bn_], [bn_])
                        cur, nxt, bc_, bn_ = nxt, cur, bn_, bc_
                    incl, b_incl = cur, bc_
                    P.op("dve", lambda e: e.tensor_tensor(out=pst[:, :], in0=incl[:, :], in1=ovb[:, :], op=ALU.subtract), [b_incl, b_ovb], [b_pst])
                    P.op("dve", lambda e: e.tensor_scalar(out=pst[:, :], in0=pst[:, :], scalar1=128.0, scalar2=3968.0, op0=ALU.mult, op1=ALU.add), [b_pst], [b_pst])
                    P.op("dve", lambda e: e.tensor_tensor(out=pst[:, :], in0=pst[:, :], in1=erow[:, :], op=ALU.subtract), [b_pst, b_erow], [b_pst])
                    P.op("dve", lambda e: e.tensor_scalar(out=prod[:, :, :], in0=rank[:, :, :], scalar1=128.0, scalar2=None, op0=ALU.is_ge), [b_rank], [b_prod])
                    P.op("dve", lambda e: e.tensor_tensor(out=prod[:, :, :], in0=prod[:, :, :], in1=pst[:, :].unsqueeze(1).to_broadcast([128, NT, NEXP]), op=ALU.mult),
                         [b_prod, b_pst], [b_prod])
                    P.op("dve", lambda e: e.tensor_tensor(out=base[:, :, :], in0=rank[:, :, :], in1=erow[:, :].unsqueeze(1).to_broadcast([128, NT, NEXP]), op=ALU.add),
                         [b_rank, b_erow], [b_base])
                    P.op("dve", lambda e: e.tensor_tensor(out=base[:, :, :], in0=base[:, :, :], in1=prod[:, :, :], op=ALU.add), [b_base, b_prod], [b_base])
                    for kq in range(2):
                        P.op("dve", lambda e, kq=kq: e.tensor_tensor(out=prod[:, :, :], in0=Mk[:, :, kq, :], in1=base[:, :, :], op=ALU.mult),
                             [b_base] + [b_Mk[t][kq] for t in range(NT)], [b_prod])
                        P.op("dve", lambda e, kq=kq: e.tensor_reduce(out=destf[kq][:, :], in_=prod[:, :, :], axis=AX.X, op=ALU.add), [b_prod], [b_destf[kq]])
                        P.op("dve", lambda e, kq=kq: e.tensor_copy(out=desti[kq][:, :], in_=destf[kq][:, :]), [b_destf[kq]], [b_desti])
                    P.op("dve", lambda e: e.tensor_tensor(out=cmp_[:, 0:16, :], in0=incl[:, :].unsqueeze(1).to_broadcast([128, 16, NEXP]),
                                                          in1=jrow[:, 0:16].unsqueeze(2).to_broadcast([128, 16, NEXP]), op=ALU.is_le), [b_incl, b_jrow], [b_cmp])
                    P.op("dve", lambda e: e.tensor_reduce(out=blke[:, 0:16], in_=cmp_[:, 0:16, :], axis=AX.X, op=ALU.add), [b_cmp], [b_blke])
                    P.op("dve", lambda e: e.tensor_scalar(out=blke[:, 0:16], in0=blke[:, 0:16], scalar1=31.0, scalar2=None, op0=ALU.min), [b_blke], [b_blke])
                    P.op("dve", lambda e: e.tensor_scalar(out=valid[:, 0:16], in0=jrow[:, 0:16], scalar1=incl[:, NEXP - 1:NEXP], scalar2=None, op0=ALU.is_lt),
                         [b_jrow, b_incl], [b_valid])
                    P.op("dve", lambda e: e.tensor_scalar(out=valid[:, 0:16], in0=valid[:, 0:16], scalar1=-1.0e6, scalar2=1.0e6, op0=ALU.mult, op1=ALU.add), [b_valid], [b_valid])
                    P.op("dve", lambda e: e.scalar_tensor_tensor(out=tmpi[:, 0:16], in0=blke[:, 0:16], scalar=1024.0, in1=valid[:, 0:16], op0=ALU.mult, op1=ALU.add),
                         [b_blke, b_valid], [b_tmpi])
                    P.op("dve", lambda e: e.tensor_scalar(out=tmpi[:, 0:16], in0=tmpi[:, 0:16], scalar1=p8[:, 0:1], scalar2=None, op0=ALU.add), [b_tmpi, b_p8], [b_tmpi])
                    P.op("dve", lambda e: e.tensor_tensor(out=idxf[:, 0:16, :], in0=tmpi[:, 0:16].unsqueeze(2).to_broadcast([128, 16, 8]),
                                                          in1=j8[:, :].unsqueeze(1).to_broadcast([128, 16, 8]), op=ALU.add), [b_tmpi, b_j8], [b_idxf])
                    P.op("dve", lambda e: e.tensor_copy(out=idxw[:, :], in_=idxf[:, 0:16, :].rearrange("p a b -> p (a b)")), [b_idxf], [b_idxw])
                    if "route" in debug:
                        dump("destf0", destf[0][:, :], [b_destf[0]]); dump("destf1", destf[1][:, :], [b_destf[1]])
                        dump("blke", blke[:, 0:16], [b_blke])
                    P.barrier()
                _phase_E3()
        all_h2 = [b for t in range(NT) for b in b_hT[t]]
        stop("stop3")

        NBLK = 48
        with ExitStack() as E4:
            def _phase_E4():
                zt = S("zt", [128, D], BF16, E4); b_zt = P.buf("zt")
                P.op("dve", lambda e: e.memset(zt[:, :], 0.0), [], [b_zt])
                b_xz = P.bufs("xz", NBLK)
                for j in range(NBLK):
                    P.dma("sp", lambda e, j=j: e.dma_start(out=xbuf[j * 128:(j + 1) * 128, :], in_=zt[:, :]), "xz", reads=[b_zt], writes=[b_xz[j]])
                h2tm = [S(f"h2tm{i}", [128, D], BF16, E4) for i in range(2)]; b_h2tm = P.bufs("h2tm", 2)
                b_xsc = P.bufs("xsc", 2 * NT)
                for t in range(NT):
                    s = t % 2
                    for g in range(4):
                        _mm(P, [(psb[g][:, j * 128:(j + 1) * 128], hT[:, 4 * g + j, t * 128:(t + 1) * 128], ident_bf[:, :], True, True) for j in range(4)],
                            b_hT[t] + [b_identb], [pb[g]])
                        if g % 2 == 0:
                            P.op("act", lambda e, g=g, s=s: e.copy(out=h2tm[s][:, g * 512:(g + 1) * 512], in_=psb[g][:, :]), [pb[g]], [b_h2tm[s]])
                        else:
                            P.op("dve", lambda e, g=g, s=s: e.tensor_copy(out=h2tm[s][:, g * 512:(g + 1) * 512], in_=psb[g][:, :]), [pb[g]], [b_h2tm[s]])
                    for kq in range(2):
                        P.dma("pool", lambda e, t=t, s=s, kq=kq: e.indirect_dma_start(
                            out=xbuf[:, :], out_offset=bass.IndirectOffsetOnAxis(ap=desti[kq][:, t:t + 1], axis=0),
                            in_=h2tm[s][:, :], in_offset=None, bounds_check=breg(e, NBLK * 128 - 1), oob_is_err=False),
                            "xsc", reads=[b_h2tm[s], b_desti] + b_xz, writes=[b_xsc[2 * t + kq]])
                P.barrier()
                wgp = hT
                wup = S("wup", [128, NCH, 1024], BF16, E4)
                b_wg = P.bufs("wg", 8); b_wu = P.bufs("wu", 8); b_wd = P.bufs("wd", 8)
                xb = [S(f"xb{i}", [128, D], BF16, E4) for i in range(2)]; b_xb = P.bufs("xb", 2)
                xTb = [S(f"xTb{i}", [128, NCH, 128], BF16, E4) for i in range(2)]; b_xTb = [P.bufs(f"xTb{i}_", 4) for i in range(2)]
                sg = [S(f"sg{i}", [128, 512], es=E4) for i in range(2)]; b_sg = P.bufs("sg", 2)
                hb = [S(f"hb{i}", [128, 1024], BF16, E4) for i in range(2)]; b_hb = [P.bufs(f"hb{i}_", 2) for i in range(2)]
                hTb = [S(f"hTb{i}", [128, 8, 128], BF16, E4) for i in range(2)]; b_hTb = [P.bufs(f"hTb{i}_", 2) for i in range(2)]
                yb = [S(f"yb{i}", [128, D], es=E4) for i in range(2)]; b_yb = [P.bufs(f"yb{i}_", 4) for i in range(2)]
                b_yst = P.bufs("yst", NBLK)

                def block(j):
                    s = j % 2
                    P.dma("sp", lambda e: e.dma_start(out=xb[s][:, :], in_=xbuf[j * 128:(j + 1) * 128, :]), f"xb{s}", reads=b_xsc + b_xz, writes=[b_xb[s]])
                    if j < NEXP:
                        for (dst, srcw, bufs, sem) in ((wgp, wg_r, b_wg, "wg"), (wup, wu_r, b_wu, "wu"), (mergedT, wd_r, b_wd, "wd")):
                            P.dma("pool", lambda e, dst=dst, srcw=srcw: e.dma_start(
                                out=dst[:, :, :].rearrange("p a n -> p (a n)"),
                                in_=srcw[j * 1024:(j + 1) * 1024, :].rearrange("(p q) n -> p (q n)", q=8)), sem, writes=bufs)
                    else:
                        jo = j - NEXP
                        for (dst, srcw, bufs, sem) in ((wgp, wg_r, b_wg, "wg"), (wup, wu_r, b_wu, "wu"), (mergedT, wd_r, b_wd, "wd")):
                            for jj in range(8):
                                P.dma("pool", lambda e, jj=jj, dst=dst, srcw=srcw: e.indirect_dma_start(
                                    out=dst[:, 2 * jj:2 * jj + 2, :].rearrange("p a n -> p (a n)"), out_offset=None, in_=srcw[:, :],
                                    in_offset=bass.IndirectOffsetOnAxis(ap=idxw[:, jo * 8 + jj:jo * 8 + jj + 1], axis=0),
                                    bounds_check=breg(e, NEXP * 1024 - 1), oob_is_err=False),
                                    sem, reads=[b_idxw], writes=[bufs[jj]])
                    for g in range(4):
                        _mm(P, [(psb[g][:, q * 128:(q + 1) * 128], xb[s][:, (4 * g + q) * 128:(4 * g + q + 1) * 128], ident_bf[:, :], True, True) for q in range(4)],
                            [b_xb[s], b_identb], [pb[g]])
                        if g % 2 == 0:
                            P.op("act", lambda e, g=g: e.copy(out=xTb[s][:, 4 * g:4 * g + 4, :].rearrange("p a n -> p (a n)"), in_=psb[g][:, :]), [pb[g]], [b_xTb[s][g]])
                        else:
                            P.op("dve", lambda e, g=g: e.tensor_copy(out=xTb[s][:, 4 * g:4 * g + 4, :].rearrange("p a n -> p (a n)"), in_=psb[g][:, :]), [pb[g]], [b_xTb[s][g]])
                    for fh in range(2):
                        bg = 4 + fh
                        bu = 6 + fh
                        _mm(P, [(psb[bg][:, :], xTb[s][:, c, :], wgp[:, c, fh * 512:(fh + 1) * 512], c == 0, c == NCH - 1) for c in range(NCH)],
                            b_xTb[s] + b_wg, [pb[bg]])
                        _mm(P, [(psb[bu][:, :], xTb[s][:, c, :], wup[:, c, fh * 512:(fh + 1) * 512], c == 0, c == NCH - 1) for c in range(NCH)],
                            b_xTb[s] + b_wu, [pb[bu]])
                        P.op("act", lambda e, fh=fh, bg=bg: e.activation(out=sg[fh][:, :], in_=psb[bg][:, :], func=AF.Silu), [pb[bg]], [b_sg[fh]])
                        P.op("dve", lambda e, fh=fh, bu=bu: e.tensor_tensor(out=hb[s][:, fh * 512:(fh + 1) * 512], in0=sg[fh][:, :], in1=psb[bu][:, :], op=ALU.mult),
                             [pb[bu], b_sg[fh]], [b_hb[s][fh]])
                    for g in range(2):
                        bk = 4 + g
                        _mm(P, [(psb[bk][:, q * 128:(q + 1) * 128], hb[s][:, (4 * g + q) * 128:(4 * g + q + 1) * 128], ident_bf[:, :], True, True) for q in range(4)],
                            b_hb[s] + [b_identb], [pb[bk]])
                        if g == 0:
                            P.op("act", lambda e, g=g, bk=bk: e.copy(out=hTb[s][:, 4 * g:4 * g + 4, :].rearrange("p a n -> p (a n)"), in_=psb[bk][:, :]), [pb[bk]], [b_hTb[s][g]])
                        else:
                            P.op("dve", lambda e, g=g, bk=bk: e.tensor_copy(out=hTb[s][:, 4 * g:4 * g + 4, :].rearrange("p a n -> p (a n)"), in_=psb[bk][:, :]), [pb[bk]], [b_hTb[s][g]])
                    for nb in range(4):
                        bk = (6, 7, 0, 1)[nb]
                        _mm(P, [(psb[bk][:, :], hTb[s][:, fc, :], mergedT[:, 2 * fc + nb // 2, (nb % 2) * 512:(nb % 2) * 512 + 512], fc == 0, fc == 7) for fc in range(8)],
                            b_hTb[s] + b_wd, [pb[bk]])
                        if nb % 2 == 0:
                            P.op("act", lambda e, nb=nb, bk=bk: e.copy(out=yb[s][:, nb * 512:(nb + 1) * 512], in_=psb[bk][:, :]), [pb[bk]], [b_yb[s][nb]])
                        else:
                            P.op("dve", lambda e, nb=nb, bk=bk: e.tensor_copy(out=yb[s][:, nb * 512:(nb + 1) * 512], in_=psb[bk][:, :]), [pb[bk]], [b_yb[s][nb]])
                    P.dma("sp", lambda e: e.dma_start(out=ybuf[j * 128:(j + 1) * 128, :], in_=yb[s][:, :]), f"yst{s}", reads=b_yb[s], writes=[b_yst[j]])

                if "nomoe" not in debug:
                    for j in range(NBLK):
                        block(j)
                P.barrier()
                return b_yst
            b_yst = _phase_E4()

        with ExitStack() as E5:
            def _phase_E5():
                nfb = S("nfb", [128, D], es=E5); b_nfb = P.buf("nfb")
                P.dma("sp", lambda e: e.dma_start(out=nfb[:, :], in_=nf_d[:, :]), "nfb", writes=[b_nfb])
                x1t = [S(f"x1t{i}", [128, D], es=E5) for i in range(2)]; b_x1t = P.bufs("x1t", 2)
                y1 = [S(f"y1_{i}", [128, D], es=E5) for i in range(2)]; b_y1 = P.bufs("y1", 2)
                y2 = [S(f"y2_{i}", [128, D], es=E5) for i in range(2)]; b_y2 = P.bufs("y2", 2)
                acc = [S(f"acc{i}", [128, D], es=E5) for i in range(2)]; b_acc = P.bufs("acc", 2)
                ot = [S(f"ot{i}", [128, D], es=E5) for i in range(2)]; b_ot = P.bufs("ot", 2)
                junk = S("junk", [128, D], BF16, E5); b_junk = P.buf("junk")
                for t in range(NT):
                    s = t % 2
                    P.dma("sp", lambda e, t=t, s=s: e.dma_start(out=x1t[s][:, :], in_=x1buf[t * 128:(t + 1) * 128, :]), f"x1t{s}", reads=[b_x1buf[t]], writes=[b_x1t[s]])
                    if "nomoe" not in debug:
                        P.dma("pool", lambda e, t=t, s=s: e.indirect_dma_start(
                            out=y1[s][:, :], out_offset=None, in_=ybuf[:, :],
                            in_offset=bass.IndirectOffsetOnAxis(ap=desti[0][:, t:t + 1], axis=0)), f"y1_{s}", reads=b_yst + [b_desti], writes=[b_y1[s]])
                        P.dma("pool", lambda e, t=t, s=s: e.indirect_dma_start(
                            out=y2[s][:, :], out_offset=None, in_=ybuf[:, :],
                            in_offset=bass.IndirectOffsetOnAxis(ap=desti[1][:, t:t + 1], axis=0)), f"y2_{s}", reads=b_yst + [b_desti], writes=[b_y2[s]])
                        P.op("act", lambda e, t=t, s=s: e.activation(out=acc[s][:, :], in_=y1[s][:, :], func=AF.Copy, scale=cc[:, t, 0:1]), [b_y1[s], b_cc[t]], [b_acc[s]])
                        P.op("dve", lambda e, t=t, s=s: e.scalar_tensor_tensor(out=acc[s][:, :], in0=y2[s][:, :], scalar=cc[:, t, 1:2], in1=acc[s][:, :],
                                                                               op0=ALU.mult, op1=ALU.add), [b_y2[s], b_cc[t], b_acc[s]], [b_acc[s]])
                        P.op("dve", lambda e, s=s: e.tensor_tensor(out=acc[s][:, :], in0=acc[s][:, :], in1=gbc[:, D:2 * D], op=ALU.mult), [b_acc[s]] + b_gbc[4:8], [b_acc[s]])
                        P.op("dve", lambda e, s=s: e.tensor_tensor(out=x1t[s][:, :], in0=x1t[s][:, :], in1=acc[s][:, :], op=ALU.add), [b_acc[s], b_x1t[s]], [b_x1t[s]])
                    P.op("act", lambda e, s=s: e.activation(out=junk[:, :], in_=x1t[s][:, :], func=AF.Square, accum_out=sst[:, s:s + 1]),
                         [b_x1t[s]], [b_junk, b_ss[s]])
                    P.op("dve", lambda e, s=s: e.tensor_scalar(out=sst[:, 2 + s:3 + s], in0=sst[:, s:s + 1], scalar1=1.0 / D, scalar2=EPS,
                                                               op0=ALU.mult, op1=ALU.add), [b_ss[s]], [b_t1[s]])
                    P.op("act", lambda e, s=s: e.activation(out=sst[:, 2 + s:3 + s], in_=sst[:, 2 + s:3 + s], func=AF.Sqrt), [b_t1[s]], [b_t1[s]])
                    P.op("dve", lambda e, s=s: e.reciprocal(out=rstd[:, s:s + 1], in_=sst[:, 2 + s:3 + s]), [b_t1[s]], [b_rstd[s]])
                    P.op("dve", lambda e, s=s: e.scalar_tensor_tensor(out=ot[s][:, :], in0=x1t[s][:, :], scalar=rstd[:, s:s + 1], in1=nfb[:, :],
                                                                      op0=ALU.mult, op1=ALU.mult), [b_x1t[s], b_rstd[s], b_nfb], [b_ot[s]])
                    fin.append(P.dma("sp", lambda e, t=t, s=s: e.dma_start(out=y_out[t * 128:(t + 1) * 128, :], in_=ot[s][:, :]), f"ot{s}", reads=[b_ot[s]]))
                P.emit(final_wait_ops=fin)
            _phase_E5()
    return nc


def _const_tables(half):
    inv = (np.float32(10000.0) ** (-np.arange(0, 128, 2, dtype=np.float32) / np.float32(128))).astype(np.float32)

    def tabs(pos0):
        pos = (pos0 + np.arange(TOK, dtype=np.float32)).astype(np.float32)
        ang = (pos[:, None] * inv[None, :]).astype(np.float32)
        cos = np.cos(ang).astype(np.float32); sin = np.sin(ang).astype(np.float32)
        cs = np.concatenate([cos, cos], axis=1).reshape(NT, 128, 128).transpose(1, 0, 2)
        sn = np.concatenate([sin, sin], axis=1).reshape(NT, 128, 128).transpose(1, 0, 2)
        return np.ascontiguousarray(cs), np.ascontiguousarray(sn)

    cs_own, sn_own = tabs(np.float32(half * TOK))
    cs_prev, sn_prev = tabs(np.float32(0))
    log_g = np.log1p(-(2.0 ** (-5.0 - np.arange(8, dtype=np.float32)))).astype(np.float32)
    i = np.arange(128, dtype=np.float32)
    scale = np.float32(128.0 ** -0.5)
    diff = i[None, :] - i[:, None]
    maskT = np.where(diff[None] >= 0, np.exp(log_g[:, None, None] * np.maximum(diff[None], 0.0)), 0.0).astype(np.float32) * scale
    maskT = np.ascontiguousarray(maskT.transpose(1, 0, 2))
    qd = np.exp(log_g[:, None] * (i + 1.0)).astype(np.float32)
    qdec = np.ascontiguousarray(np.broadcast_to(qd[None], (128, 8, 128))).astype(np.float32)
    kdec = np.ascontiguousarray((np.exp(log_g[:, None] * (127.0 - i)).astype(np.float32) * scale).T)
    return dict(cs_own=cs_own, sn_own=sn_own, cs_prev=cs_prev, sn_prev=sn_prev, maskT=maskT, qdec=qdec, kdec=kdec)


_PROG_CACHE = {}


def _make_in_maps(inputs):
    f = lambda a: np.ascontiguousarray(np.asarray(a, dtype=np.float32))
    x = f(inputs["x"]); c = f(inputs["c"])
    b_ada = f(inputs["b_ada"])[0]
    shared = dict(
        w_ada=f(inputs["w_ada"])[0],
        b_adaT=np.ascontiguousarray(b_ada.reshape(96, 128).T),
        b_adag=np.ascontiguousarray(np.concatenate([b_ada[2 * D:3 * D], b_ada[5 * D:6 * D]])[None, :]),
        n1T=np.ascontiguousarray(f(inputs["norm1_g"])[0].reshape(NCH, 128).T),
        n2T=np.ascontiguousarray(f(inputs["norm2_g"])[0].reshape(NCH, 128).T),
        nf_bc=np.ascontiguousarray(np.broadcast_to(f(inputs["norm_f_g"])[None, :], (128, D))),
        w_in=f(inputs["w_in"])[0],
        conv_wT=np.ascontiguousarray(f(inputs["conv_w"])[0].reshape(3, 8, 128).transpose(2, 1, 0)),
        w_conv_out=f(inputs["w_conv_out"])[0], w_ret_out=f(inputs["w_ret_out"])[0], w_o=f(inputs["w_o"])[0],
        w_r=np.ascontiguousarray(np.concatenate([f(inputs["w_router_group"])[0], f(inputs["w_router_expert"])[0]], axis=1)),
        b_r=np.ascontiguousarray(np.broadcast_to(np.concatenate([f(inputs["b_router_group"])[0], f(inputs["b_router_expert"])[0]])[None, :], (128, 36))),
        wg_r=np.ascontiguousarray(f(inputs["w_gate"])[0].reshape(NEXP, 8, 2, 128, 1024).transpose(0, 3, 1, 2, 4)).reshape(NEXP * 1024, D),
        wu_r=np.ascontiguousarray(f(inputs["w_up"])[0].reshape(NEXP, 8, 2, 128, 1024).transpose(0, 3, 1, 2, 4)).reshape(NEXP * 1024, D),
        wd_r=np.ascontiguousarray(f(inputs["w_down"])[0].reshape(NEXP, 8, 128, D).transpose(0, 2, 1, 3)).reshape(NEXP * 1024, D),
        ident=np.eye(128, dtype=np.float32),
        ltri=np.triu(np.ones((128, 128), dtype=np.float32), k=1),
        erow128=np.ascontiguousarray(np.broadcast_to((np.arange(NEXP, dtype=np.float32) * 128.0)[None, :], (128, NEXP))),
        jrow=np.ascontiguousarray(np.broadcast_to(np.arange(48, dtype=np.float32)[None, :], (128, 48))),
        p8=(np.arange(128, dtype=np.float32) * 8.0)[:, None].copy(),
        j8=np.ascontiguousarray(np.broadcast_to(np.arange(8, dtype=np.float32)[None, :], (128, 8))),
    )
    tabs = [_const_tables(0), _const_tables(1)]
    in_maps = []
    for core in range(NCORES):
        b, half = core // 2, core % 2
        m = dict(shared)
        m.update(tabs[half])
        m["x_own"] = np.ascontiguousarray(x[b, half * TOK:(half + 1) * TOK])
        m["x_prev"] = np.ascontiguousarray(x[b, 0:TOK])
        m["cT"] = np.ascontiguousarray(c[b].reshape(NCH, 128).T)
        m["flag"] = np.full((128, 1), float(half), dtype=np.float32)
        in_maps.append(m)
    return in_maps


def kernel(**inputs):
    if "prog" not in _PROG_CACHE:
        _PROG_CACHE["prog"] = build_program()
    nc = _PROG_CACHE["prog"]
    in_maps = _make_in_maps(inputs)
    res = run_bass_kernel_spmd(nc, in_maps, core_ids=list(range(NCORES)))
    out = np.empty((4, 2048, D), dtype=np.float32)
    for core in range(NCORES):
        b, half = core // 2, core % 2
        out[b, half * TOK:(half + 1) * TOK] = res.results[core]["y"]
    return out
```
